# Optimizing a Trainium2 kernel written in Bass

```python
import jax, jax.numpy as jnp
from jax import lax
import numpy as np

D_MODEL = 1024
BATCH = 2
SEQ = 16384
DEPTH = 4

N_BRANCH = 4
HEAD_DIM = 64
BRANCH_WIDTH = D_MODEL // N_BRANCH
N_HEADS = BRANCH_WIDTH // HEAD_DIM
NORM_EPS = 1e-6
RW_DECAY_LORA = 64
RW_AAA_LORA = 64
RW_GATE_LORA = 128
RW_DECAY_SCALE = 0.606531
RW_LN_EPS = 64e-5
SB_BLOCK = 128
RET_CHUNK = 128
RET_GN_EPS = 1e-5
ROPE_BASE = 10000.0
CONV_WIDTH = 31
CONV_LN_EPS = 1e-5
D_FF = 2816
N_EXPERTS = 8
TOP_K = 2
D_FF_EXPERT = 1408
MOE_BLOCK = 256

RW_IN = 3 * BRANCH_WIDTH + RW_DECAY_LORA + RW_AAA_LORA + RW_GATE_LORA
SB_IN = 3 * BRANCH_WIDTH
RET_IN = 4 * BRANCH_WIDTH
CONV_IN = 2 * BRANCH_WIDTH
N_IN = RW_IN + SB_IN + RET_IN + CONV_IN
MIX_SPLITS = [RW_IN, RW_IN + SB_IN, RW_IN + SB_IN + RET_IN]
RW_SPLITS = [BRANCH_WIDTH, 2 * BRANCH_WIDTH, 3 * BRANCH_WIDTH,
             3 * BRANCH_WIDTH + RW_DECAY_LORA, 3 * BRANCH_WIDTH + RW_DECAY_LORA + RW_AAA_LORA]

kernel_name = "hybrid_rwkv7_stickbreak_retnet_conformer_moe"


def _rmsnorm(x, gain, eps=NORM_EPS):
    x32 = x.astype(jnp.float32)
    y = x32 * lax.rsqrt(jnp.mean(x32 * x32, axis=-1, keepdims=True) + eps)
    return (y * gain).astype(x.dtype)


def _layernorm(x, gain, bias, eps):
    x32 = x.astype(jnp.float32)
    xc = x32 - jnp.mean(x32, axis=-1, keepdims=True)
    var = jnp.mean(xc * xc, axis=-1, keepdims=True)
    return (xc * lax.rsqrt(var + eps) * gain + bias).astype(x.dtype)


def _token_shift(x):
    return jnp.pad(x, ((0, 0), (1, 0), (0, 0)))[:, :-1]


def _split_heads(t):
    return t.reshape(t.shape[0], t.shape[1], N_HEADS, HEAD_DIM)


def _rwkv7_mixer(p, mu, w0, w2, a0, a2, g2, k_k, k_a, r_k, ln_w, ln_b):
    B, S, _ = p.shape
    p = p + (_token_shift(p) - p) * mu
    r, k, v, pw, pa, pg = jnp.split(p, RW_SPLITS, axis=-1)
    w = jnp.exp(-RW_DECAY_SCALE * jax.nn.sigmoid((w0 + jnp.tanh(pw) @ w2).astype(jnp.float32)))
    a = jax.nn.sigmoid(a0 + pa @ a2)
    g = jax.nn.sigmoid(pg) @ g2
    r, k, v, w, a = (_split_heads(t) for t in (r, k, v, w, a))
    kk32 = (k * k_k.reshape(N_HEADS, HEAD_DIM)).astype(jnp.float32)
    kk = kk32 / jnp.maximum(jnp.sqrt(jnp.sum(kk32 * kk32, axis=-1, keepdims=True)), 1e-12)
    k = k * (1.0 + (a - 1.0) * k_a.reshape(N_HEADS, HEAD_DIM))

    def step(state, inp):
        r_t, w_t, k_t, v_t, kk_t, a_t = inp
        sa = jnp.einsum('bhvk,bhk->bhv', state, -kk_t)
        state = (state * w_t[:, :, None, :] + sa[..., None] * (kk_t * a_t)[:, :, None, :]
                 + v_t[..., None] * k_t[:, :, None, :])
        return state, jnp.einsum('bhvk,bhk->bhv', state, r_t)

    seq_first = lambda t: jnp.moveaxis(t.astype(jnp.float32), 1, 0)
    state0 = jnp.zeros((B, N_HEADS, HEAD_DIM, HEAD_DIM), jnp.float32)
    _, y = lax.scan(step, state0, tuple(seq_first(t) for t in (r, w, k, v, kk, a)))
    y = jnp.moveaxis(y, 0, 1)
    y = _layernorm(y, ln_w.reshape(N_HEADS, HEAD_DIM), ln_b.reshape(N_HEADS, HEAD_DIM), RW_LN_EPS)
    y = y + jnp.sum(r * k * r_k, axis=-1, keepdims=True) * v
    return (y.reshape(B, S, BRANCH_WIDTH) * g).astype(p.dtype)


def _stick_breaking_mixer(p, q_norm, k_norm):
    B, S, _ = p.shape
    nb = S // SB_BLOCK
    blocks = lambda t: _split_heads(t).reshape(B, nb, SB_BLOCK, N_HEADS, HEAD_DIM).transpose(0, 3, 1, 2, 4)
    q, k, v = (blocks(t) for t in jnp.split(p, 3, axis=-1))
    q = _rmsnorm(q, q_norm) * (HEAD_DIM ** -0.5)
    k = _rmsnorm(k, k_norm)
    idx = jnp.arange(SB_BLOCK)
    rev_incl = (idx[:, None] >= idx[None, :]).astype(jnp.float32)
    diag_mask = idx[None, :] < idx[:, None]
    acc = jnp.zeros((B, N_HEADS, nb, SB_BLOCK), jnp.float32)
    o = jnp.zeros((B, N_HEADS, nb, SB_BLOCK, HEAD_DIM), jnp.float32)
    for d in range(nb):
        n = nb - d
        z = jnp.einsum('bhnqd,bhnkd->bhnqk', q[:, :, d:], k[:, :, :n]).astype(jnp.float32)
        ls = jax.nn.log_sigmoid(-z)
        if d == 0:
            ls = jnp.where(diag_mask, ls, 0.0)
        cum = jnp.einsum('bhnqk,kj->bhnqj', ls, rev_incl)
        log_a = z + cum + acc[:, :, d:, :, None]
        if d == 0:
            log_a = jnp.where(diag_mask, log_a, -jnp.inf)
        attn = jnp.exp(log_a)
        o = o.at[:, :, d:].add(jnp.einsum('bhnqk,bhnkd->bhnqd', attn.astype(v.dtype), v[:, :, :n]).astype(jnp.float32))
        acc = acc.at[:, :, d:].add(cum[..., 0])
    return o.transpose(0, 2, 3, 1, 4).reshape(B, S, BRANCH_WIDTH).astype(p.dtype)


def _rotary(t):
    S = t.shape[1]
    inv_freq = ROPE_BASE ** (-jnp.arange(0, HEAD_DIM, 2, dtype=jnp.float32) / HEAD_DIM)
    ang = jnp.arange(S, dtype=jnp.float32)[:, None] * inv_freq[None, :]
    cos = jnp.cos(ang)[None, :, None, :].astype(t.dtype)
    sin = jnp.sin(ang)[None, :, None, :].astype(t.dtype)
    t1, t2 = jnp.split(t, 2, axis=-1)
    return jnp.concatenate([t1 * cos - t2 * sin, t1 * sin + t2 * cos], axis=-1)


def _retention_mixer(p, gn_w):
    B, S, _ = p.shape
    dt = p.dtype
    q, k, v, gt = jnp.split(p, 4, axis=-1)
    q = _rotary(_split_heads(q))
    k = _rotary(_split_heads(k)) * (HEAD_DIM ** -0.5)
    v = _split_heads(v)
    nc = S // RET_CHUNK
    chunk = lambda t: t.reshape(B, nc, RET_CHUNK, N_HEADS, HEAD_DIM).transpose(0, 3, 1, 2, 4)
    qc, kc, vc = chunk(q), chunk(k), chunk(v)
    log_gamma = jnp.log(1.0 - 2.0 ** (-5.0 - jnp.arange(N_HEADS, dtype=jnp.float32)))
    idx = jnp.arange(RET_CHUNK, dtype=jnp.float32)
    rel = idx[:, None] - idx[None, :]
    intra_decay = jnp.where(rel >= 0, jnp.exp(jnp.maximum(rel, 0.0) * log_gamma[:, None, None]), 0.0)
    k_decay = jnp.exp((RET_CHUNK - 1 - idx)[None, :] * log_gamma[:, None])
    q_decay = jnp.exp((idx + 1.0)[None, :] * log_gamma[:, None])
    chunk_decay = jnp.exp(RET_CHUNK * log_gamma)
    scores = jnp.einsum('bhcqd,bhckd->bhcqk', qc, kc) * intra_decay[None, :, None].astype(dt)
    intra = jnp.einsum('bhcqk,bhckd->bhcqd', scores, vc)
    kv = jnp.einsum('bhckd,bhcke->cbhde', kc * k_decay[None, :, None, :, None].astype(dt), vc)

    def step(state, kv_c):
        return chunk_decay[None, :, None, None] * state + kv_c, state

    _, state_prev = lax.scan(step, jnp.zeros(kv.shape[1:], jnp.float32), kv.astype(jnp.float32))
    inter = jnp.einsum('bhcqd,cbhde->bhcqe', qc * q_decay[None, :, None, :, None].astype(dt),
                       state_prev.astype(dt))
    o = (intra + inter).transpose(0, 2, 3, 1, 4).reshape(B, S, N_HEADS, HEAD_DIM)
    o = _layernorm(o, gn_w.reshape(N_HEADS, HEAD_DIM), 0.0, RET_GN_EPS)
    return o.reshape(B, S, BRANCH_WIDTH) * jax.nn.silu(gt)


def _conformer_conv_mixer(p, dw, db, ln_w, ln_b):
    u_a, u_b = jnp.split(p, 2, axis=-1)
    u = u_a * jax.nn.sigmoid(u_b)
    u = lax.conv_general_dilated(u, dw[:, None, :].astype(u.dtype), window_strides=(1,),
                                 padding=[(CONV_WIDTH - 1, 0)],
                                 dimension_numbers=('NWC', 'WIO', 'NWC'),
                                 feature_group_count=BRANCH_WIDTH) + db
    return jax.nn.silu(_layernorm(u, ln_w, ln_b, CONV_LN_EPS))


def _swiglu(h, w1, w3, w2):
    return (jax.nn.silu(h @ w1) * (h @ w3)) @ w2


def _moe_ffn(h, router, w1, w3, w2):
    B, S, D = h.shape
    T = B * S
    xt = h.reshape(T, D)
    logits = (xt @ router).astype(jnp.float32)
    top_logit, top_idx = lax.top_k(logits, TOP_K)
    gates = jax.nn.softmax(top_logit, axis=-1)
    n_assign = T * TOP_K
    expert = top_idx.reshape(-1)
    token = jnp.arange(n_assign) // TOP_K
    order = jnp.argsort(expert)
    expert_s, token_s, weight_s = expert[order], token[order], gates.reshape(-1)[order]
    x_s = xt[token_s]
    counts = jnp.bincount(expert, length=N_EXPERTS)
    n_blk = n_assign // MOE_BLOCK
    seg_start = jnp.sort(jnp.concatenate([jnp.arange(n_blk) * MOE_BLOCK, jnp.cumsum(counts)[:-1]]))
    seg_end = jnp.concatenate([seg_start[1:], jnp.array([n_assign], seg_start.dtype)])
    seg_blk = jnp.minimum(seg_start // MOE_BLOCK, n_blk - 1)
    seg_exp = expert_s[jnp.minimum(seg_start, n_assign - 1)]

    def segment(args):
        start, end, blk, e = args
        rows = blk * MOE_BLOCK + jnp.arange(MOE_BLOCK)
        xb = lax.dynamic_slice_in_dim(x_s, blk * MOE_BLOCK, MOE_BLOCK, axis=0)
        yb = _swiglu(xb, w1[e], w3[e], w2[e])
        return jnp.where(((rows >= start) & (rows < end))[:, None], yb, 0.0)

    y_seg = lax.map(segment, (seg_start, seg_end, seg_blk, seg_exp))
    y_s = jax.ops.segment_sum(y_seg, seg_blk, num_segments=n_blk).reshape(n_assign, D)
    out = jax.ops.segment_sum(y_s * weight_s[:, None].astype(y_s.dtype), token_s, num_segments=T)
    return out.reshape(B, S, D)


def setup_inputs(seed: int = 0) -> dict:
    key = jax.random.key(seed)
    ks = iter(jax.random.split(key, 40))
    nrm = lambda shape, scale: scale * jax.random.normal(next(ks), shape, jnp.float32)
    L, C = DEPTH, BRANCH_WIDTH
    nd, nm = (DEPTH + 1) // 2, DEPTH // 2
    return {
        "x": nrm((BATCH, SEQ, D_MODEL), 1.0),
        "norm_mix": 1.0 + nrm((L, D_MODEL), 0.1),
        "w_in": nrm((L, D_MODEL, N_IN), D_MODEL ** -0.5),
        "rw_mu": jax.random.uniform(next(ks), (L, RW_IN), jnp.float32),
        "rw_w0": -1.5 + nrm((L, C), 0.5),
        "rw_w2": nrm((L, RW_DECAY_LORA, C), 0.5 * RW_DECAY_LORA ** -0.5),
        "rw_a0": nrm((L, C), 0.5),
        "rw_a2": nrm((L, RW_AAA_LORA, C), RW_AAA_LORA ** -0.5),
        "rw_g2": nrm((L, RW_GATE_LORA, C), RW_GATE_LORA ** -0.5),
        "rw_k_k": 0.85 + nrm((L, C), 0.1),
        "rw_k_a": 1.0 + nrm((L, C), 0.1),
        "rw_r_k": nrm((L, N_HEADS, HEAD_DIM), 0.1),
        "rw_ln_w": 1.0 + nrm((L, C), 0.1),
        "rw_ln_b": nrm((L, C), 0.01),
        "sb_q_norm": 1.0 + nrm((L, HEAD_DIM), 0.1),
        "sb_k_norm": 1.0 + nrm((L, HEAD_DIM), 0.1),
        "ret_gn": 1.0 + nrm((L, C), 0.1),
        "conv_dw": nrm((L, CONV_WIDTH, C), CONV_WIDTH ** -0.5),
        "conv_b": nrm((L, C), 0.01),
        "conv_ln_w": 1.0 + nrm((L, C), 0.1),
        "conv_ln_b": nrm((L, C), 0.01),
        "w_gate": nrm((L, N_BRANCH, D_MODEL, D_MODEL), D_MODEL ** -0.5),
        "w_branch": nrm((L, N_BRANCH, C, D_MODEL), C ** -0.5),
        "w_out": nrm((L, D_MODEL, D_MODEL), 0.5 * D_MODEL ** -0.5),
        "norm_ffn": 1.0 + nrm((L, D_MODEL), 0.1),
        "ffn_w1": nrm((nd, D_MODEL, D_FF), D_MODEL ** -0.5),
        "ffn_w3": nrm((nd, D_MODEL, D_FF), D_MODEL ** -0.5),
        "ffn_w2": nrm((nd, D_FF, D_MODEL), 0.5 * D_FF ** -0.5),
        "router": nrm((nm, D_MODEL, N_EXPERTS), D_MODEL ** -0.5),
        "moe_w1": nrm((nm, N_EXPERTS, D_MODEL, D_FF_EXPERT), D_MODEL ** -0.5),
        "moe_w3": nrm((nm, N_EXPERTS, D_MODEL, D_FF_EXPERT), D_MODEL ** -0.5),
        "moe_w2": nrm((nm, N_EXPERTS, D_FF_EXPERT, D_MODEL), 0.5 * D_FF_EXPERT ** -0.5),
    }


def reference(x, norm_mix, w_in, rw_mu, rw_w0, rw_w2, rw_a0, rw_a2, rw_g2, rw_k_k, rw_k_a, rw_r_k,
              rw_ln_w, rw_ln_b, sb_q_norm, sb_k_norm, ret_gn, conv_dw, conv_b, conv_ln_w, conv_ln_b,
              w_gate, w_branch, w_out, norm_ffn, ffn_w1, ffn_w3, ffn_w2, router, moe_w1, moe_w3, moe_w2):
    for l in range(DEPTH):
        h = _rmsnorm(x, norm_mix[l])
        p_rw, p_sb, p_ret, p_conv = jnp.split(h @ w_in[l], MIX_SPLITS, axis=-1)
        branches = (
            _rwkv7_mixer(p_rw, rw_mu[l], rw_w0[l], rw_w2[l], rw_a0[l], rw_a2[l], rw_g2[l],
                         rw_k_k[l], rw_k_a[l], rw_r_k[l], rw_ln_w[l], rw_ln_b[l]),
            _stick_breaking_mixer(p_sb, sb_q_norm[l], sb_k_norm[l]),
            _retention_mixer(p_ret, ret_gn[l]),
            _conformer_conv_mixer(p_conv, conv_dw[l], conv_b[l], conv_ln_w[l], conv_ln_b[l]),
        )
        merged = jax.nn.sigmoid(h @ w_gate[l, 0]) * (branches[0] @ w_branch[l, 0])
        for i in range(1, N_BRANCH):
            merged = merged + jax.nn.sigmoid(h @ w_gate[l, i]) * (branches[i] @ w_branch[l, i])
        x = x + merged @ w_out[l]
        h = _rmsnorm(x, norm_ffn[l])
        if l % 2 == 0:
            x = x + _swiglu(h, ffn_w1[l // 2], ffn_w3[l // 2], ffn_w2[l // 2])
        else:
            x = x + _moe_ffn(h, router[l // 2], moe_w1[l // 2], moe_w3[l // 2], moe_w2[l // 2])
    return x
```

```python
import numpy as np
import concourse.bass as bass
import concourse.mybir as mybir

F32 = mybir.dt.float32
BF16 = mybir.dt.bfloat16
AF = mybir.ActivationFunctionType
ALU = mybir.AluOpType
AX = mybir.AxisListType

N_DMA_SEMS = 40
PSUM_NAMES = set()


def _region(ap):
    t = ap.tensor
    name = ap.name
    dims = ap.ap
    off = ap.offset
    space = str(ap.space)
    if 'DRAM' in space.upper() or 'HBM' in space.upper() or not hasattr(ap, 'base_partition') or len(dims) == 0:
        lo = off
        hi = off + sum((c - 1) * abs(s) for s, c in dims) + 1
        return (name, 0, 1, lo, hi)
    pstride = dims[0][0]
    pcount = dims[0][1]
    if pstride == 0:
        pstride = 1 << 30
    p0 = off // pstride if pstride < (1 << 30) else 0
    f0 = off - p0 * pstride if pstride < (1 << 30) else off
    f1 = f0 + sum((c - 1) * abs(s) for s, c in dims[1:]) + 1
    if name in PSUM_NAMES:
        return (name, (p0 // 32) * 32, ((p0 + pcount + 31) // 32) * 32, 0, 1 << 30)
    return (name, p0, p0 + pcount, f0, f1)


class Prog:
    def __init__(self, nc):
        self.nc = nc
        self.ops = {e: [] for e in ('pe', 'act', 'dve', 'pool', 'sp')}
        self.cnt = {e: 0 for e in ('pe', 'act', 'dve', 'pool')}
        self.recs = {}
        self.events = []
        self.known = {e: {} for e in self.ops}
        self.dma_uses = [0] * N_DMA_SEMS
        self.dma_next = 0
        self.nops = 0
        self.out_events = []

    def _is_dram(self, ap):
        s = str(ap.space).upper()
        return 'DRAM' in s or 'HBM' in s

    def add(self, eng, fn, reads=(), writes=(), dma=False):
        waits = {}

        def need(ev):
            semkey, val, _, _ = ev
            if waits.get(semkey, 0) < val:
                waits[semkey] = val

        rregs = [_region(a) for a in reads]
        wregs = [_region(a) for a in writes]
        idx = len(self.events)
        for r in rregs:
            for rec in self.recs.get(r[0], ()):
                if rec[5] != 'w':
                    continue
                if rec[1] < r[2] and r[1] < rec[2] and rec[3] < r[4] and r[3] < rec[4]:
                    ev = self.events[rec[6]]
                    if ev[2] == eng and not ev[3] and not dma and eng == 'pe':
                        continue
                    need(ev)
        for r in wregs:
            for rec in self.recs.get(r[0], ()):
                if rec[1] < r[2] and r[1] < rec[2] and rec[3] < r[4] and r[3] < rec[4]:
                    ev = self.events[rec[6]]
                    if ev[2] == eng and not ev[3] and not dma:
                        if eng == 'pe':
                            continue
                    need(ev)
        if dma:
            j = self.dma_next
            self.dma_next = (j + 1) % N_DMA_SEMS
            prev = self.dma_uses[j] * 16
            self.dma_uses[j] += 1
            val = prev + 16
            semkey = ('dma', j)
            if prev > 0:
                if waits.get(semkey, 0) < prev:
                    waits[semkey] = prev
            ev = (semkey, val, eng, True)
        else:
            self.cnt[eng] += 1
            ev = ((eng,), self.cnt[eng], eng, False)
        self.events.append(ev)
        kn = self.known[eng]
        wl = []
        for sk, v in waits.items():
            if kn.get(sk, 0) >= v:
                continue
            kn[sk] = v
            wl.append((sk, v))
        self.ops[eng].append((wl, fn, ev))
        evs = self.events
        for r in wregs:
            lst = self.recs.setdefault(r[0], [])
            lst[:] = [rec for rec in lst
                      if not (r[1] <= rec[1] and rec[2] <= r[2] and r[3] <= rec[3] and rec[4] <= r[4])]
            lst.append((r[0], r[1], r[2], r[3], r[4], 'w', idx))
        for r in rregs:
            lst = self.recs.setdefault(r[0], [])
            if not dma:
                lst[:] = [rec for rec in lst
                          if not (rec[5] == 'r' and evs[rec[6]][2] == eng and not evs[rec[6]][3]
                                  and r[1] <= rec[1] and rec[2] <= r[2] and r[3] <= rec[3] and rec[4] <= r[4])]
            lst.append((r[0], r[1], r[2], r[3], r[4], 'r', idx))
        self.nops += 1
        return ev

    def dma(self, out, in_, eng='sp', **kw):
        ev = self.add(eng, lambda e: e.dma_start(out=out, in_=in_, **kw), reads=[in_], writes=[out], dma=True)
        if self._is_dram(out):
            self.out_events.append(ev)
        return ev

    def mm(self, out, lhsT, rhs, start=True, stop=True, **kw):
        return self.add('pe', lambda e: e.matmul(out, lhsT, rhs, start=start, stop=stop, **kw),
                        reads=[lhsT, rhs], writes=[out])

    def transpose(self, out, in_, ident):
        return self.add('pe', lambda e: e.transpose(out, in_, ident), reads=[in_, ident], writes=[out])

    def act(self, out, in_, func, bias=None, scale=None, accum_out=None, eng='act'):
        kw = {}
        reads = [in_]
        writes = [out]
        if bias is not None:
            kw['bias'] = bias
            if not isinstance(bias, (int, float)):
                reads.append(bias)
        if scale is not None:
            kw['scale'] = scale
            if not isinstance(scale, (int, float)):
                reads.append(scale)
        if accum_out is not None:
            kw['accum_out'] = accum_out
            writes.append(accum_out)
        return self.add('act', lambda e: e.activation(out, in_, func, **kw), reads=reads, writes=writes)

    def tt(self, out, in0, in1, op, eng='dve'):
        return self.add(eng, lambda e: e.tensor_tensor(out, in0, in1, op), reads=[in0, in1], writes=[out])

    def ts(self, out, in0, s1, s2, op0, op1=None, eng='dve', accum_out=None):
        reads = [in0] + [s for s in (s1, s2) if s is not None and not isinstance(s, (int, float))]
        writes = [out] + ([accum_out] if accum_out is not None else [])
        kw = {}
        if op1 is not None:
            kw['op1'] = op1
        if accum_out is not None:
            kw['accum_out'] = accum_out
        return self.add(eng, lambda e: e.tensor_scalar(out, in0, s1, s2, op0, **kw), reads=reads, writes=writes)

    def stt(self, out, in0, scalar, in1, op0, op1, eng='dve', accum_out=None):
        reads = [in0, in1] + ([scalar] if not isinstance(scalar, (int, float)) else [])
        writes = [out] + ([accum_out] if accum_out is not None else [])
        kw = {}
        eng = 'dve'
        if accum_out is not None:
            kw['accum_out'] = accum_out
        return self.add(eng, lambda e: e.scalar_tensor_tensor(out, in0, scalar, in1, op0, op1, **kw),
                        reads=reads, writes=writes)

    def copy(self, out, in_, eng='dve'):
        if eng == 'act':
            return self.add('act', lambda e: e.copy(out, in_), reads=[in_], writes=[out])
        return self.add(eng, lambda e: e.tensor_copy(out, in_), reads=[in_], writes=[out])

    def memset(self, out, val, eng='pool'):
        return self.add(eng, lambda e: e.memset(out, val), reads=[], writes=[out])

    def reduce(self, out, in_, op, axis=AX.X, eng='dve'):
        return self.add(eng, lambda e: e.tensor_reduce(out, in_, axis, op), reads=[in_], writes=[out])

    def emit(self):
        nc = self.nc
        import contextlib
        with contextlib.ExitStack() as st:
            sems = {}
            for e in ('pe', 'act', 'dve', 'pool'):
                sems[(e,)] = st.enter_context(nc.semaphore("s_" + e))
            for j in range(N_DMA_SEMS):
                sems[('dma', j)] = st.enter_context(nc.semaphore("s_dma%d" % j))
            final = {}
            for ev in self.out_events:
                if final.get(ev[0], 0) < ev[1]:
                    final[ev[0]] = ev[1]
            for e in ('pe', 'act', 'dve', 'pool'):
                if self.cnt[e] > 0:
                    final[(e,)] = self.cnt[e]
            for j in range(N_DMA_SEMS):
                if self.dma_uses[j] > 0:
                    final[('dma', j)] = self.dma_uses[j] * 16
            block = st.enter_context(nc.Block())
            ops = self.ops

            def run(engname):
                def f(eng):
                    for wl, fn, ev in ops[engname]:
                        for sk, v in wl:
                            eng.wait_ge(sems[sk], v)
                        ins = fn(eng)
                        ins.then_inc(sems[ev[0]], 16 if ev[3] else 1)
                    if engname == 'sp':
                        for sk, v in final.items():
                            eng.wait_ge(sems[sk], v)
                return f

            block.sync(run('sp'))
            block.tensor(run('pe'))
            block.scalar(run('act'))
            block.vector(run('dve'))
            block.gpsimd(run('pool'))


import contextlib
import ml_dtypes
from concourse.bass_utils import run_bass_kernel_spmd

D_MODEL = 1024
NH = 4
HD = 64
RW_SCALE = 0.606531
C_R, C_K, C_V, C_PW, C_PA, C_PG, C_SQ, C_SK, C_SV, C_RQ, C_RK, C_RV, C_RG = \
    0, 64, 128, 192, 256, 320, 448, 512, 576, 640, 704, 768, 832
NCOL_B = 896
B_PARTS = ('rw', 'ret', 'sb')
RET_DBG = 3
(V_MUR, V_MUK, V_MUV, V_MUPW, V_MUPA, V_W0, V_A0, V_KK, V_KA, V_RK, V_LNW, V_LNB,
 V_SBQ, V_SBK, V_GN) = range(15)
CST = [1e-6, 64e-5, 1e-5, 1.0, 1e-12, 0.0]
K_EPS6, K_EPSRW, K_EPS5, K_ONE, K_TINY, K_ZERO = range(6)


class Alloc:
    def __init__(self, nc, st):
        self.nc = nc
        self.st = st
        self.bytes = 0

    def sb(self, name, shape, dt=F32):
        n = 1
        for s in shape[1:]:
            n *= s
        self.bytes += n * (4 if dt == F32 else 2)
        return self.st.enter_context(self.nc.sbuf_tensor(name, list(shape), dt))

    def ps(self, name, shape, dt=F32):
        PSUM_NAMES.add(name)
        return self.st.enter_context(self.nc.psum_tensor(name, list(shape), dt))


def rms_tile(P, xt, sq, ones_bf, pn, rstd, cst, hT, gain, tmpf=None):
    nsq = sq.shape[1]
    if nsq == 8:
        P.act(sq, xt, AF.Square)
    for c in range(8):
        if nsq < 8:
            P.act(sq[:, c % nsq, :], xt[:, c, :], AF.Square)
        P.mm(pn, ones_bf, sq[:, c % nsq, :], start=(c == 0), stop=(c == 7))
    P.act(rstd, pn, AF.Sqrt, scale=1.0 / D_MODEL, bias=cst[:, K_EPS6:K_EPS6 + 1])
    P.add('dve', lambda e: e.reciprocal(rstd, rstd), reads=[rstd], writes=[rstd])
    for c in range(8):
        P.stt(hT[:, c, :], xt[:, c, :], gain[:, c:c + 1], rstd, ALU.mult, ALU.mult,
              eng=('dve' if c % 2 == 0 else 'pool'))


def ln_feat(P, A, y, pn, ones_s, eps_col, tmp1, tmp2, sbf, npart=64, N=512):
    P.copy(sbf, y, eng='pool')
    P.mm(pn[0:npart, 0:N], ones_s, sbf, start=True, stop=True)
    P.tt(y, y, pn[0:npart, 0:N], ALU.subtract)
    P.act(sbf, y, AF.Square)
    P.mm(pn[0:npart, 0:N], ones_s, sbf, start=True, stop=True)
    P.act(tmp2, pn[0:npart, 0:N], AF.Sqrt, bias=eps_col, scale=1.0)
    P.add('dve', lambda e: e.reciprocal(tmp2, tmp2), reads=[tmp2], writes=[tmp2])
    P.tt(y, y, tmp2, ALU.mult)


def build_B(S):
    nc = bass.Bass("TRN2", target_bir_lowering=False)
    NT = S // 512
    NB = S // 128
    din = lambda n, s, d=F32: nc.dram_tensor(n, list(s), d, kind="ExternalInput").ap()
    xT = din("xT", [1024, S])
    gmix_d = din("gmix", [128, 8])
    wh_d = din("wh", [1024, NCOL_B])
    vec64_d = din("vec64", [64, 16])
    mupg_d = din("mupg", [128, 1])
    w2_d = din("w2h", [64, 64])
    a2_d = din("a2h", [64, 64])
    g2_d = din("g2h", [128, 64])
    cos_d = din("cosT", [64, S])
    sin_d = din("sinT", [64, S])
    cst_d = din("cst", [128, 8])
    tab_d = din("tab", [128, 6, 128])
    tab64_d = din("tab64", [64, 5, 512], BF16)
    qdec_d = din("qdec4", [64, 512])
    kdec_d = din("kdec", [128, 2])
    y_d = nc.dram_tensor("yT", [3, 64, S], BF16, kind="ExternalOutput").ap()

    with contextlib.ExitStack() as st:
        A = Alloc(nc, st)
        P = Prog(nc)
        gmix = A.sb("gmix_s", [128, 8]); P.dma(gmix[:], gmix_d)
        vec = A.sb("vec_s", [64, 16]); P.dma(vec[:], vec64_d)
        mupg = A.sb("mupg_s", [128, 1]); P.dma(mupg[:], mupg_d)
        w2f = A.sb("w2_s", [64, 64]); P.dma(w2f[:], w2_d)
        a2f = A.sb("a2_s", [64, 64]); P.dma(a2f[:], a2_d)
        g2f = A.sb("g2_s", [128, 64]); P.dma(g2f[:], g2_d)
        w2 = A.sb("w2_b", [64, 64], BF16); P.copy(w2[:], w2f[:])
        a2 = A.sb("a2_b", [64, 64], BF16); P.copy(a2[:], a2f[:])
        g2 = A.sb("g2_b", [128, 64], BF16); P.copy(g2[:], g2f[:])
        cst = A.sb("cst_s", [128, 8]); P.dma(cst[:], cst_d)
        tab = A.sb("tab_s", [128, 6, 128]); P.dma(tab[:], tab_d)
        tab64 = A.sb("tab64_s", [64, 5, 512], BF16); P.dma(tab64[:], tab64_d)
        qdec4t = A.sb("qdec4_s", [64, 512]); P.dma(qdec4t[:], qdec_d)
        kdec = A.sb("kdec_s", [128, 2]); P.dma(kdec[:], kdec_d)
        ident = A.sb("identb", [128, 128], BF16); P.copy(ident[:], tab[:, 0, :])
        ident = ident[:]
        mask_lt = A.sb("mask_lt", [128, 128], BF16); P.copy(mask_lt[:], tab[:, 1, :])
        negrev = A.sb("negrev", [128, 128], BF16); P.copy(negrev[:], tab[:, 2, :])
        decayT = tab[:, 3, :]
        maskU8, maskL8, maskUI8, I8, rstm = (tab64[:, i, :] for i in range(5))
        qdec4 = qdec4t[:]
        ones_bf = A.sb("ones_bf", [128, 128], BF16); P.memset(ones_bf[:], 1.0)
        negones = A.sb("negones", [128, 128], BF16); P.memset(negones[:], -1.0)
        ones64 = A.sb("ones64", [64, 64], BF16); P.memset(ones64[:], 1.0)
        ones64s = A.sb("ones64s", [64, 64], BF16); P.memset(ones64s[:], 1.0 / 64)
        qn8 = A.sb("qn8", [64, 1]); P.ts(qn8[:], vec[:, V_SBQ:V_SBQ + 1], 0.125, None, ALU.mult)
        V = lambda i: vec[:, i:i + 1]
        C = lambda i, n=64: cst[0:n, i:i + 1]

        xts = [A.sb("xt0", [128, 8, 512])] * 2
        wbf = A.sb("wbf", [128, 8, NCOL_B], BF16)
        sq = A.sb("sq", [128, 2, 512], BF16)
        hT = A.sb("hT", [128, 8, 512], BF16)
        rstd = A.sb("rstd", [128, 512])
        kT_all = A.sb("kT_all", [64, S], BF16)
        v_all = A.sb("v_all", [128, NB, 64], BF16)
        pj = A.ps("pj", [128, 512]); pn = A.ps("pn", [128, 512])
        px0 = A.ps("px0", [128, 512]); px1 = A.ps("px1", [128, 512]); py = A.ps("py", [128, 512])
        pz = A.ps("pz", [128, 512]); pla = A.ps("pla", [128, 512]); po = A.ps("po", [128, 512])
        pzs = [pz, pj]; plas = [pla, py]

        whv = wh_d.rearrange("(c p) n -> p c n", p=128)
        for half in range(2):
            stg = xts[half][:].rearrange("p c t -> p (c t)")[:, 0:4 * NCOL_B].rearrange("p (c n) -> p c n", c=4)
            P.dma(stg, whv[:, 4 * half:4 * half + 4, :])
            P.copy(wbf[:, 4 * half:4 * half + 4, :], stg, eng=('dve' if half == 0 else 'act'))

        names = ["raw_r", "raw_k", "raw_v", "raw_pw", "raw_pa"]
        raws = [A.sb(n, [64, 513]) for n in names]
        rawg = A.sb("raw_pg", [128, 513])
        for r_ in raws:
            P.memset(r_[:, 0:1], 0.0)
            P.memset(r_[:, 512:513], 0.0)
        P.memset(rawg[:, 0:1], 0.0); P.memset(rawg[:, 512:513], 0.0)
        e1 = A.sb("sb_e1", [128, 512]); spb = A.sb("sb_spb", [128, 512], BF16)
        LS = A.sb("sb_LS", [128, 512])
        T = {}
        for n in ["r", "k", "v", "pw", "pa", "d", "logw", "lp", "ep", "em", "epm", "a", "g", "kk", "t1",
                  "kp", "al", "be", "kt", "rb", "bon", "ysb", "tm1", "tm2"]:
            T[n] = A.sb("rw_" + n, [64, 512], BF16 if n in ("al", "be", "kt", "rb") else F32)
        sA = A.sb("rw_sA", [64, 512], BF16); sB = A.sb("rw_sB", [64, 512], BF16); vb = A.sb("rw_vb", [64, 512], BF16)
        dgb = spb
        pgm = LS; dg = e1
        M_ = [A.sb("rw_M%d" % i, [64, 8, 64], BF16) for i in range(2)]
        L_ = [A.sb("rw_L%d" % i, [64, 8, 64], BF16) for i in range(2)]
        Pt = A.sb("rw_Pt", [64, 8, 64], BF16); Pt32 = A.sb("rw_Pt32", [64, 8, 64])
        AakT = A.sb("rw_AakT", [64, 8, 64], BF16); ArbT = A.sb("rw_ArbT", [64, 8, 64], BF16)
        ArkT = A.sb("rw_ArkT", [64, 8, 64], BF16); Vt = A.sb("rw_Vt", [64, 8, 64], BF16); Bt = A.sb("rw_Bt", [64, 8, 64], BF16)
        Kt = A.sb("rw_Kt", [64, 8, 64], BF16); Us = A.sb("rw_Us", [64, 8, 64], BF16); Xs = A.sb("rw_Xs", [64, 64], BF16)
        H32 = A.sb("rw_H32", [64, 64]); H = A.sb("rw_H", [64, 64], BF16); Hd = A.sb("rw_Hd", [64, 64])
        P.memset(H32[:], 0.0); P.memset(H[:], 0.0)
        yout = [A.sb("yout%d" % i, [64, 512], BF16) for i in range(3)]
        cosb = A.sb("cosb", [64, 512]); sinb = A.sb("sinb", [64, 512])
        tq = T["d"]; t1 = T["tm1"]; t2 = T["tm2"]
        qr = A.sb("rt_qr", [64, 512], BF16); kr = A.sb("rt_kr", [64, 512], BF16); qd = A.sb("rt_qd", [64, 512], BF16)
        rv = A.sb("rt_rvb", [64, 512], BF16); rg = T["pa"]
        sc = A.sb("rt_sc", [128, 128], BF16); vtok = A.sb("rt_vtok", [128, 64], BF16); ktok = A.sb("rt_ktok", [128, 64], BF16)
        rst = A.sb("rt_st", [64, 64]); rst_bf = A.sb("rt_stbf", [64, 64], BF16)
        P.memset(rst[:], 0.0); P.memset(rst_bf[:], 0.0)
        ro = A.sb("rt_ro", [64, 512])
        sq_t = T["logw"]; sq_s = T["lp"]; sq_r = T["em"]
        qT = A.sb("sb_qT", [64, 512], BF16); svT = sB
        attn = A.sb("sb_attn", [128, 512], BF16)
        oacc = A.sb("sb_oacc", [64, 512])
        e1s = [e1, A.sb("sb_e1b", [128, 512])]; spbs = [spb, A.sb("sb_spb2", [128, 512], BF16)]
        attns = [attn, A.sb("sb_attn2", [128, 512], BF16)]
        LSb = A.sb("sb_LSb", [128, 512], BF16)

        xTv = xT.rearrange("(c p) t -> p c t", p=128)

        def proj(col0, M, ps):
            for c in range(8):
                P.mm(ps, wbf[:, c, col0:col0 + M], hT[:, c, :], start=(c == 0), stop=(c == 7))

        for g in range(NT):
            t0 = g * 512
            gens = []
            xt = xts[g % 2]
            P.dma(xt[:], xTv[:, :, t0:t0 + 512])
            P.dma(cosb[:], cos_d[:, t0:t0 + 512])
            P.dma(sinb[:], sin_d[:, t0:t0 + 512])
            rms_tile(P, xt[:], sq[:], ones_bf[:], pj[:], rstd[:], cst, hT, gmix)

            if 'rw' in B_PARTS:
                mixed = [T["r"], T["k"], T["v"], T["pw"], T["pa"]]
                for i, (col, mu) in enumerate([(C_R, V_MUR), (C_K, V_MUK), (C_V, V_MUV), (C_PW, V_MUPW), (C_PA, V_MUPA)]):
                    raw = raws[i]
                    if g > 0:
                        P.copy(raw[:, 0:1], raw[:, 512:513], eng='pool')
                    proj(col, 64, pj[0:64, :])
                    P.copy(raw[:, 1:513], pj[0:64, :], eng='act')
                    P.tt(T["d"][:], raw[:, 0:512], raw[:, 1:513], ALU.subtract, eng='pool')
                    P.stt(mixed[i][:], T["d"][:], V(mu), raw[:, 1:513], ALU.mult, ALU.add)
                if g > 0:
                    P.copy(rawg[:, 0:1], rawg[:, 512:513], eng='pool')
                proj(C_PG, 128, pj[:, :])
                P.copy(rawg[:, 1:513], pj[:, :], eng='act')
                P.tt(dg[:], rawg[:, 0:512], rawg[:, 1:513], ALU.subtract, eng='pool')
                P.stt(pgm[:], dg[:], mupg[:, 0:1], rawg[:, 1:513], ALU.mult, ALU.add)

                r_, k_, v_ = T["r"], T["k"], T["v"]
                P.act(sA[:], T["pw"][:], AF.Tanh)
                P.mm(pn[0:64, :], w2[:], sA[:])
                P.act(T["logw"][:], pn[0:64, :], AF.Sigmoid, bias=V(V_W0))
                P.ts(T["logw"][:], T["logw"][:], -RW_SCALE, None, ALU.mult)
                P.add('dve', lambda e: e.tensor_tensor_scan(T["lp"][:], rstm, T["logw"][:], 0.0, ALU.mult, ALU.add),
                      reads=[rstm, T["logw"][:]], writes=[T["lp"][:]])
                P.act(T["ep"][:], T["lp"][:], AF.Exp)
                P.act(T["em"][:], T["lp"][:], AF.Exp, scale=-1.0)
                P.tt(T["tm1"][:], T["lp"][:], T["logw"][:], ALU.subtract, eng='pool')
                P.act(T["epm"][:], T["tm1"][:], AF.Exp)
                P.copy(sB[:], T["pa"][:], eng='pool')
                P.mm(pn[0:64, :], a2[:], sB[:])
                P.act(T["a"][:], pn[0:64, :], AF.Sigmoid, bias=V(V_A0))
                P.act(dgb[:], pgm[:], AF.Sigmoid)
                P.mm(pn[0:64, :], g2[:], dgb[:])
                P.copy(T["g"][:], pn[0:64, :], eng='act')
                P.ts(T["kk"][:], k_[:], V(V_KK), None, ALU.mult)
                P.act(sA[:], T["kk"][:], AF.Square)
                P.mm(pn[0:64, :], ones64[:], sA[:])
                P.act(T["tm2"][:], pn[0:64, :], AF.Sqrt)
                P.ts(T["tm2"][:], T["tm2"][:], 1e-12, None, ALU.max)
                P.add('dve', lambda e: e.reciprocal(T["tm2"][:], T["tm2"][:]), reads=[T["tm2"][:]], writes=[T["tm2"][:]])
                P.tt(T["kk"][:], T["kk"][:], T["tm2"][:], ALU.mult)
                P.ts(T["t1"][:], T["a"][:], 1.0, V(V_KA), ALU.subtract, ALU.mult)
                P.stt(T["kp"][:], T["t1"][:], 1.0, k_[:], ALU.add, ALU.mult)
                P.stt(T["al"][:], T["kk"][:], -1.0, T["epm"][:], ALU.mult, ALU.mult)
                P.tt(T["t1"][:], T["kk"][:], T["a"][:], ALU.mult, eng='pool')
                P.tt(T["be"][:], T["t1"][:], T["em"][:], ALU.mult, eng='pool')
                P.tt(T["kt"][:], T["kp"][:], T["em"][:], ALU.mult)
                P.tt(T["rb"][:], r_[:], T["ep"][:], ALU.mult, eng='pool')
                P.stt(sB[:], r_[:], V(V_RK), T["kp"][:], ALU.mult, ALU.mult)
                P.mm(pn[0:64, :], ones64[:], sB[:])
                P.copy(vb[:], v_[:], eng='pool')
                P.tt(T["bon"][:], pn[0:64, :], v_[:], ALU.mult)

                al, be, kt, rb = T["al"], T["be"], T["kt"], T["rb"]
                X0 = px0[0:64, :].rearrange("p (c n) -> p c n", c=8)
                X1 = px1[0:64, :].rearrange("p (c n) -> p c n", c=8)
                X2 = pn[0:64, :].rearrange("p (c n) -> p c n", c=8)
                f3 = lambda ap: ap
                fl = lambda t3: t3.rearrange("p c n -> p (c n)")
                cs = lambda c: slice(64 * c, 64 * c + 64)
                for c in range(8):
                    P.mm(X0[:, c, :], be[:, cs(c)], al[:, cs(c)])
                    P.mm(X1[:, c, :], al[:, cs(c)], be[:, cs(c)])
                P.tt(fl(Pt32[:]), px0[0:64, :], maskU8, ALU.mult)
                P.copy(fl(M_[0][:]), fl(Pt32[:]), eng='pool')
                P.tt(fl(L_[0][:]), px1[0:64, :], maskL8, ALU.mult, eng='pool' if False else 'dve')
                for c in range(8):
                    P.mm(X2[:, c, :], kt[:, cs(c)], al[:, cs(c)])
                P.tt(fl(AakT[:]), pn[0:64, :], maskU8, ALU.mult)
                for c in range(8):
                    P.mm(X0[:, c, :], be[:, cs(c)], rb[:, cs(c)])
                    P.mm(X1[:, c, :], kt[:, cs(c)], rb[:, cs(c)])
                P.tt(fl(ArbT[:]), px0[0:64, :], maskUI8, ALU.mult)
                P.tt(fl(ArkT[:]), px1[0:64, :], maskUI8, ALU.mult)
                for c in range(8):
                    P.mm(X2[:, c, :], vb[:, cs(c)], ident[0:64, 0:64])
                    P.mm(X0[:, c, :], be[:, cs(c)], ident[0:64, 0:64])
                    P.mm(X1[:, c, :], kt[:, cs(c)], ident[0:64, 0:64])
                P.copy(fl(Vt[:]), pn[0:64, :], eng='act')
                P.copy(fl(Bt[:]), px0[0:64, :], eng='dve')
                P.copy(fl(Kt[:]), px1[0:64, :], eng='act')
                P.tt(fl(Pt32[:]), fl(Pt32[:]), I8, ALU.add)
                P.copy(fl(Pt[:]), fl(Pt32[:]), eng='pool')
                for n in range(1, 6):
                    Mp, Lp = M_[(n - 1) % 2], L_[(n - 1) % 2]
                    Mn, Ln = M_[n % 2], L_[n % 2]
                    for c in range(8):
                        if n < 5:
                            P.mm(X0[:, c, :], Lp[:, c, :], Mp[:, c, :])
                        P.mm(X1[:, c, :], Mp[:, c, :], Lp[:, c, :])
                    if n < 5:
                        P.copy(fl(Mn[:]), px0[0:64, :], eng='act')
                    P.copy(fl(Ln[:]), px1[0:64, :], eng='dve')
                    for c in range(8):
                        P.mm(X2[:, c, :], Ln[:, c, :], Pt[:, c, :])
                    P.tt(fl(Pt32[:]), fl(Pt32[:]), pn[0:64, :], ALU.add)
                    P.copy(fl(Pt[:]), fl(Pt32[:]), eng='pool')
                def rw_gen():
                    ysb = T["ysb"]
                    for c in range(8):
                        epC = T["ep"][:, 64 * c + 63:64 * c + 64]
                        P.mm(X0[:, c, :], al[:, cs(c)], H[:], start=True, stop=False)
                        P.mm(X0[:, c, :], AakT[:, c, :], Vt[:, c, :], start=False, stop=True)
                        P.copy(Xs[:], X0[:, c, :], eng='act')
                        P.mm(X1[:, c, :], Pt[:, c, :], Xs[:])
                        P.copy(Us[:, c, :], X1[:, c, :], eng='dve')
                        yield
                        P.mm(X0[:, c, :], H[:], rb[:, cs(c)], start=True, stop=False)
                        P.mm(X0[:, c, :], Us[:, c, :], ArbT[:, c, :], start=False, stop=False)
                        P.mm(X0[:, c, :], Vt[:, c, :], ArkT[:, c, :], start=False, stop=True)
                        P.copy(ysb[:, cs(c)], X0[:, c, :], eng='act')
                        P.ts(Hd[:], H32[:], epC, None, ALU.mult, eng='pool')
                        P.mm(X2[:, c, :], Bt[:, c, :], Us[:, c, :], start=True, stop=False)
                        P.mm(X2[:, c, :], Kt[:, c, :], Vt[:, c, :], start=False, stop=True)
                        P.stt(H32[:], X2[:, c, :], epC, Hd[:], ALU.mult, ALU.add)
                        P.copy(H[:], H32[:], eng='pool')
                        yield
                    ln_feat(P, A, ysb[:], pn, ones64s[:], C(K_EPSRW), T["tm1"][:], T["tm2"][:], sA[:])
                    P.ts(ysb[:], ysb[:], V(V_LNW), V(V_LNB), ALU.mult, ALU.add)
                    P.tt(ysb[:], ysb[:], T["bon"][:], ALU.add)
                    P.tt(yout[0][:], ysb[:], T["g"][:], ALU.mult)
                    P.dma(y_d[0, :, t0:t0 + 512], yout[0][:])
                gens.append(rw_gen())

            if 'ret' in B_PARTS:
                def rotary(src_ps, dst, scale):
                    P.copy(tq[:], src_ps, eng='act')
                    P.stt(t1[0:32, :], tq[32:64, :], scale, sinb[32:64, :], ALU.mult, ALU.mult, eng='pool')
                    P.stt(t1[32:64, :], tq[0:32, :], scale, sinb[0:32, :], ALU.mult, ALU.mult, eng='pool')
                    P.stt(t2[:], tq[:], scale, cosb[:], ALU.mult, ALU.mult)
                    P.tt(dst[0:32, :], t2[0:32, :], t1[0:32, :], ALU.subtract)
                    P.tt(dst[32:64, :], t2[32:64, :], t1[32:64, :], ALU.add)
                proj(C_RQ, 64, pj[0:64, :]); rotary(pj[0:64, :], qr, 1.0)
                proj(C_RK, 64, pj[0:64, :]); rotary(pj[0:64, :], kr, 0.125)
                P.tt(qd[:], qr[:], qdec4, ALU.mult, eng='pool')
                proj(C_RV, 64, pj[0:64, :]); P.copy(rv[:], pj[0:64, :], eng='act')
                proj(C_RG, 64, pj[0:64, :]); P.act(rg[:], pj[0:64, :], AF.Silu)
                def ret_gen():
                    for c in range(4):
                        c4 = slice(128 * c, 128 * c + 128)
                        P.mm(px0[:, 0:128], kr[:, c4], qr[:, c4])
                        P.tt(sc[:], px0[:, 0:128], decayT, ALU.mult)
                        P.mm(px1[:, 0:64], rv[:, c4], ident[0:64, 0:64])
                        P.copy(vtok[:], px1[:, 0:64], eng='act')
                        P.mm(px1[:, 64:128], kr[:, c4], ident[0:64, 0:64])
                        P.ts(ktok[:], px1[:, 64:128], kdec[:, 0:1], None, ALU.mult)
                        yield
                        P.mm(px0[0:64, 0:128], vtok[:], sc[:], start=True, stop=False)
                        P.mm(px0[0:64, 0:128], rst_bf[:], qd[:, c4], start=False, stop=True)
                        P.copy(ro[:, c4], px0[0:64, 0:128], eng='act')
                        P.mm(px1[0:64, 128:192], ktok[:], vtok[:])
                        P.stt(rst[:], rst[:], kdec[0:64, 1:2], px1[0:64, 128:192], ALU.mult, ALU.add)
                        P.copy(rst_bf[:], rst[:], eng='pool')
                        yield
                    ln_feat(P, A, ro[:], pn, ones64s[:], C(K_EPS5), t1[:], t2[:], sA[:])
                    P.stt(yout[2][:], ro[:], V(V_GN), rg[:], ALU.mult, ALU.mult)
                    P.dma(y_d[2, :, t0:t0 + 512], yout[2][:])
                gens.append(ret_gen())

            if 'sb' in B_PARTS:
                def qknorm(col, dst, gcol):
                    proj(col, 64, pj[0:64, :])
                    P.copy(sq_t[:], pj[0:64, :], eng='act')
                    P.act(sA[:], sq_t[:], AF.Square)
                    P.mm(pn[0:64, :], ones64s[:], sA[:])
                    P.act(sq_r[:], pn[0:64, :], AF.Sqrt, bias=C(K_EPS6), scale=1.0)
                    P.add('dve', lambda e: e.reciprocal(sq_r[:], sq_r[:]), reads=[sq_r[:]], writes=[sq_r[:]])
                    P.stt(dst, sq_t[:], gcol, sq_r[:], ALU.mult, ALU.mult)
                qknorm(C_SQ, qT[:], qn8[:, 0:1])
                qknorm(C_SK, kT_all[:, t0:t0 + 512], V(V_SBK))
                proj(C_SV, 64, pj[0:64, :]); P.copy(svT[:], pj[0:64, :], eng='act')
                for i in range(4):
                    P.mm(px1[:, 0:64], svT[:, 128 * i:128 * i + 128], ident[0:64, 0:64])
                    P.copy(v_all[:, 4 * g + i, :], px1[:, 0:64], eng='act')
                P.memset(LS[:], 0.0); P.memset(LSb[:], 0.0)
                def sb_gen():
                    js = list(range(4 * g + 3, -1, -1))

                    def geom(j):
                        diag = j >= 4 * g
                        c0 = (j - 4 * g) * 128 if diag else 0
                        return diag, c0, slice(c0, 512), kT_all[:, 128 * j:128 * j + 128]

                    def stage1(i):
                        j = js[i]
                        diag, c0, cols, kTj = geom(j)
                        pz_, e1_, spb_ = pzs[i % 2], e1s[i % 2], spbs[i % 2]
                        P.mm(pz_[:, cols], kTj, qT[:, cols])
                        P.act(e1_[:, cols], pz_[:, cols], AF.Exp)
                        P.act(spb_[:, cols], e1_[:, cols], AF.Ln, bias=cst[:, K_ONE:K_ONE + 1], scale=1.0)
                        if diag:
                            P.tt(spb_[:, c0:c0 + 128], spb_[:, c0:c0 + 128], mask_lt[:], ALU.mult, eng='pool')

                    def stage2(i):
                        j = js[i]
                        diag, c0, cols, kTj = geom(j)
                        pla_, spb_, attn_ = plas[i % 2], spbs[i % 2], attns[i % 2]
                        P.mm(pla_[:, cols], kTj, qT[:, cols], start=True, stop=False)
                        P.mm(pla_[:, cols], negrev[:], spb_[:, cols], start=False, stop=False)
                        P.mm(pla_[:, cols], negones[:], LSb[:, cols], start=False, stop=True)
                        P.act(attn_[:, cols], pla_[:, cols], AF.Exp)
                        if diag:
                            P.tt(attn_[:, c0:c0 + 128], attn_[:, c0:c0 + 128], mask_lt[:], ALU.mult, eng='pool')
                        P.mm(po[0:64, cols], v_all[:, j, :], attn_[:, cols], start=True, stop=True)
                        if diag:
                            P.copy(oacc[:, c0:c0 + 128], po[0:64, c0:c0 + 128], eng='dve')
                            if c0 + 128 < 512:
                                P.tt(oacc[:, c0 + 128:512], oacc[:, c0 + 128:512], po[0:64, c0 + 128:512], ALU.add)
                        else:
                            P.tt(oacc[:, :], oacc[:, :], po[0:64, :], ALU.add)
                        if j > 0:
                            P.tt(LS[:, cols], LS[:, cols], spb_[:, cols], ALU.add)
                            P.copy(LSb[:, cols], LS[:, cols], eng='pool')

                    stage1(0)
                    for i in range(len(js)):
                        if i + 1 < len(js):
                            stage1(i + 1)
                        stage2(i)
                        yield
                    P.copy(yout[1][:], oacc[:], eng='act')
                    P.dma(y_d[1, :, t0:t0 + 512], yout[1][:])
                gens.append(sb_gen())

            while gens:
                for gen in list(gens):
                    try:
                        next(gen)
                    except StopIteration:
                        gens.remove(gen)
        P.emit()
        print("build_B: sbuf bytes/partition", A.bytes, "ops", P.nops)
    return nc


def _const_tables_B(S, h):
    idx = np.arange(128)
    tab = np.zeros((128, 6, 128), np.float32)
    tab[:, 0, :] = np.eye(128, dtype=np.float32)
    tab[:, 1, :] = (idx[:, None] < idx[None, :]).astype(np.float32)
    tab[:, 2, :] = -(idx[:, None] >= idx[None, :]).astype(np.float32)
    lg = np.log(np.float32(1.0) - np.float32(2.0) ** np.float32(-5.0 - h)).astype(np.float32)
    rel = (idx[None, :] - idx[:, None]).astype(np.float32)
    tab[:, 3, :] = np.where(rel >= 0, np.exp(np.maximum(rel, 0) * lg), 0.0).astype(np.float32)
    i64 = np.arange(64)
    t64 = np.zeros((64, 5, 512), np.float32)
    mU = (i64[:, None] < i64[None, :]).astype(np.float32)
    mL = (i64[None, :] < i64[:, None]).astype(np.float32)
    mUI = (i64[:, None] <= i64[None, :]).astype(np.float32)
    t64[:, 0, :] = np.tile(mU, (1, 8))
    t64[:, 1, :] = np.tile(mL, (1, 8))
    t64[:, 2, :] = np.tile(mUI, (1, 8))
    t64[:, 3, :] = np.tile(np.eye(64, dtype=np.float32), (1, 8))
    rs = np.ones(512, np.float32); rs[::64] = 0.0
    t64[:, 4, :] = rs[None, :]
    qdec = np.exp((idx.astype(np.float32) + 1.0) * lg).astype(np.float32)
    qdec4 = np.ascontiguousarray(np.tile(qdec[None, :], (64, 4)).astype(np.float32))
    kdec = np.zeros((128, 2), np.float32)
    kdec[:, 0] = np.exp((127.0 - idx.astype(np.float32)) * lg)
    kdec[:, 1] = np.exp(np.float32(128.0) * lg)
    inv_freq = (np.float32(10000.0) ** (-np.arange(0, 64, 2, dtype=np.float32) / np.float32(64))).astype(np.float32)
    ang = np.arange(S, dtype=np.float32)[:, None] * inv_freq[None, :]
    cosT = np.concatenate([np.cos(ang).T, np.cos(ang).T], axis=0).astype(np.float32)
    sinT = np.concatenate([np.sin(ang).T, np.sin(ang).T], axis=0).astype(np.float32)
    cst = np.zeros((128, 8), np.float32)
    for i, v in enumerate(CST):
        cst[:, i] = v
    return dict(tab=tab, tab64=t64.astype(ml_dtypes.bfloat16), qdec4=qdec4, kdec=kdec, cosT=np.ascontiguousarray(cosT), sinT=np.ascontiguousarray(sinT), cst=cst)


def prep_B(inp, l, b, h, xT_b, tables):
    hs = slice(64 * h, 64 * h + 64)
    w_in = inp["w_in"][l]
    RW, SB0, RT0 = 0, 1024, 1024 + 768
    cols = [w_in[:, RW + 0 + 64 * h: RW + 0 + 64 * h + 64], w_in[:, RW + 256 + 64 * h: RW + 256 + 64 * h + 64],
            w_in[:, RW + 512 + 64 * h: RW + 512 + 64 * h + 64], w_in[:, RW + 768: RW + 832], w_in[:, RW + 832: RW + 896],
            w_in[:, RW + 896: RW + 1024],
            w_in[:, SB0 + 64 * h: SB0 + 64 * h + 64], w_in[:, SB0 + 256 + 64 * h: SB0 + 256 + 64 * h + 64],
            w_in[:, SB0 + 512 + 64 * h: SB0 + 512 + 64 * h + 64],
            w_in[:, RT0 + 64 * h: RT0 + 64 * h + 64], w_in[:, RT0 + 256 + 64 * h: RT0 + 256 + 64 * h + 64],
            w_in[:, RT0 + 512 + 64 * h: RT0 + 512 + 64 * h + 64], w_in[:, RT0 + 768 + 64 * h: RT0 + 768 + 64 * h + 64]]
    wh = np.ascontiguousarray(np.concatenate(cols, axis=1))
    mu = inp["rw_mu"][l]
    vec = np.zeros((64, 16), np.float32)
    vec[:, V_MUR] = mu[0 + 64 * h: 64 * h + 64]
    vec[:, V_MUK] = mu[256 + 64 * h: 256 + 64 * h + 64]
    vec[:, V_MUV] = mu[512 + 64 * h: 512 + 64 * h + 64]
    vec[:, V_MUPW] = mu[768:832]
    vec[:, V_MUPA] = mu[832:896]
    vec[:, V_W0] = inp["rw_w0"][l][hs]
    vec[:, V_A0] = inp["rw_a0"][l][hs]
    vec[:, V_KK] = inp["rw_k_k"][l][hs]
    vec[:, V_KA] = inp["rw_k_a"][l][hs]
    vec[:, V_RK] = inp["rw_r_k"][l][h]
    vec[:, V_LNW] = inp["rw_ln_w"][l][hs]
    vec[:, V_LNB] = inp["rw_ln_b"][l][hs]
    vec[:, V_SBQ] = inp["sb_q_norm"][l]
    vec[:, V_SBK] = inp["sb_k_norm"][l]
    vec[:, V_GN] = inp["ret_gn"][l][hs]
    m = dict(xT=xT_b, gmix=np.ascontiguousarray(inp["norm_mix"][l].reshape(8, 128).T), wh=wh, vec64=vec,
             mupg=np.ascontiguousarray(mu[896:1024].reshape(128, 1)),
             w2h=np.ascontiguousarray(inp["rw_w2"][l][:, hs]), a2h=np.ascontiguousarray(inp["rw_a2"][l][:, hs]),
             g2h=np.ascontiguousarray(inp["rw_g2"][l][:, hs]))
    m.update(tables[h])
    return m


def load_cast(P, dst_bf, src_view, stg, engs=('dve', 'act')):
    C_, N_ = dst_bf.shape[1], dst_bf.shape[2]
    cap = stg.shape[1] // N_
    i = 0
    c = 0
    while c < C_:
        k = min(cap, C_ - c)
        sv = stg[:, 0:k * N_].rearrange("p (c n) -> p c n", c=k)
        P.dma(sv, src_view[:, c:c + k, :])
        P.copy(dst_bf[:, c:c + k, :], sv, eng=engs[i % len(engs)])
        c += k
        i += 1


def build_C1(NTOK):
    nc = bass.Bass("TRN2", target_bir_lowering=False)
    NT = NTOK // 512
    din = lambda n, s, d=F32: nc.dram_tensor(n, list(s), d, kind="ExternalInput").ap()
    xTh = din("xTh", [1024, 32 + NTOK])
    gmix_d = din("gmix", [128, 8])
    wconv_d = din("wconv", [1024, 512])
    cvec_d = din("cvec", [128, 2, 34])
    yT_d = din("yT", [3, 256, NTOK], BF16)
    wg_d = din("wg", [4, 1024, 1024])
    wb_d = din("wb", [4, 256, 1024])
    wout_d = din("wout", [1024, 1024])
    cst_d = din("cst", [128, 8])
    x1_d = nc.dram_tensor("x1T", [1024, NTOK], F32, kind="ExternalOutput").ap()
    with contextlib.ExitStack() as st:
        A = Alloc(nc, st); P = Prog(nc)
        gmix = A.sb("gmix_s", [128, 8]); P.dma(gmix[:], gmix_d)
        cvec = A.sb("cvec_s", [128, 2, 34]); P.dma(cvec[:], cvec_d)
        cst = A.sb("cst_s", [128, 8]); P.dma(cst[:], cst_d)
        ones_bf = A.sb("ones_bf", [128, 128], BF16); P.memset(ones_bf[:], 1.0)
        ones_s = A.sb("ones_s", [128, 128], BF16); P.memset(ones_s[:], 1.0 / 256)
        stg = A.sb("stg", [128, 4096])
        wg = A.sb("wg_b", [128, 32, 1024], BF16)
        wb = A.sb("wb_b", [128, 8, 1024], BF16)
        wout = A.sb("wout_b", [128, 8, 1024], BF16)
        wconv = A.sb("wconv_b", [128, 8, 512], BF16)
        load_cast(P, wconv[:], wconv_d.rearrange("(c p) n -> p c n", p=128), stg)
        load_cast(P, wg[:], wg_d.rearrange("i (c p) n -> p (i c) n", p=128), stg)
        load_cast(P, wb[:], wb_d.rearrange("i (c p) n -> p (i c) n", p=128), stg)
        load_cast(P, wout[:], wout_d.rearrange("(c p) n -> p c n", p=128), stg)
        xt = A.sb("xt", [128, 8, 512]); sq = A.sb("sq", [128, 8, 512], BF16); hT = A.sb("hT", [128, 8, 512], BF16)
        rstd = A.sb("rstd", [128, 512])
        xh = A.sb("xh", [128, 8, 32]); sqh = A.sb("sqh", [128, 8, 32], BF16); hTh = A.sb("hTh", [128, 8, 32], BF16)
        rstdh = A.sb("rstdh", [128, 32])
        ua = A.sb("ua", [128, 512]); sg = A.sb("sg", [128, 512])
        u = A.sb("u", [128, 2, 544])
        acc = A.sb("acc", [128, 2, 512]); accb = A.sb("accb", [128, 2, 512], BF16); tmp2 = A.sb("tmp2", [128, 512])
        ycT = A.sb("ycT", [128, 2, 512], BF16)
        ybr = A.sb("ybr", [128, 6, 512], BF16)
        merged = A.sb("merged", [128, 8, 512], BF16); macc = A.sb("macc", [128, 512]); mtmp = A.sb("mtmp", [128, 512])
        pj = [A.ps("pj%d" % i, [128, 512]) for i in range(2)]
        pg = [A.ps("pg%d" % i, [128, 512]) for i in range(2)]
        pb = [A.ps("pb%d" % i, [128, 512]) for i in range(2)]
        pn = A.ps("pn", [128, 512]); po = A.ps("po", [128, 512])
        xv = xTh.rearrange("(c p) t -> p c t", p=128)
        yv = yT_d.rearrange("i (c p) t -> p i c t", p=128)
        x1v = x1_d.rearrange("(c p) t -> p c t", p=128)

        def conv_u(hT_, N, ucol0):
            for cc in range(2):
                pa_, pb_ = pj[0], pj[1]
                for c in range(8):
                    P.mm(pa_[:, 0:N], wconv[:, c, 128 * cc:128 * cc + 128], hT_[:, c, :], start=(c == 0), stop=(c == 7))
                for c in range(8):
                    P.mm(pb_[:, 0:N], wconv[:, c, 256 + 128 * cc:256 + 128 * cc + 128], hT_[:, c, :], start=(c == 0), stop=(c == 7))
                P.act(sg[:, 0:N], pb_[:, 0:N], AF.Sigmoid)
                P.tt(u[:, cc, ucol0:ucol0 + N], sg[:, 0:N], pa_[:, 0:N], ALU.mult)

        P.dma(xh[:], xv[:, :, 0:32])
        rms_tile(P, xh[:], sqh[:], ones_bf[:], pn[:, 0:32], rstdh[:], cst, hTh, gmix)
        conv_u(hTh, 32, 0)
        for g in range(NT):
            t0 = g * 512
            P.dma(xt[:], xv[:, :, 32 + t0:32 + t0 + 512])
            for i in range(3):
                P.dma(ybr[:, 2 * i:2 * i + 2, :], yv[:, i, :, t0:t0 + 512])
            rms_tile(P, xt[:], sq[:], ones_bf[:], pn[:], rstd[:], cst, hT, gmix)
            if g > 0:
                P.copy(u[:, :, 0:32], u[:, :, 512:544], eng='pool')
            conv_u(hT, 512, 32)
            for cc in range(2):
                P.ts(acc[:, cc, :], u[:, cc, 2:514], cvec[:, cc, 0:1], cvec[:, cc, 31:32], ALU.mult, ALU.add)
                for j in range(1, 31):
                    P.stt(acc[:, cc, :], u[:, cc, 2 + j:514 + j], cvec[:, cc, j:j + 1], acc[:, cc, :], ALU.mult, ALU.add)
            P.copy(accb[:], acc[:], eng='pool')
            for cc in range(2):
                P.mm(pn[:], ones_s[:], accb[:, cc, :], start=(cc == 0), stop=(cc == 1))
            for cc in range(2):
                P.tt(acc[:, cc, :], acc[:, cc, :], pn[:], ALU.subtract)
            P.act(accb[:], acc[:], AF.Square)
            for cc in range(2):
                P.mm(pn[:], ones_s[:], accb[:, cc, :], start=(cc == 0), stop=(cc == 1))
            P.act(tmp2[:], pn[:], AF.Sqrt, bias=cst[:, K_EPS5:K_EPS5 + 1], scale=1.0)
            P.add('dve', lambda e: e.reciprocal(tmp2[:], tmp2[:]), reads=[tmp2[:]], writes=[tmp2[:]])
            for cc in range(2):
                P.tt(acc[:, cc, :], acc[:, cc, :], tmp2[:], ALU.mult)
                P.ts(acc[:, cc, :], acc[:, cc, :], cvec[:, cc, 32:33], cvec[:, cc, 33:34], ALU.mult, ALU.add)
                P.act(ycT[:, cc, :], acc[:, cc, :], AF.Silu)
            k = 0
            for m in range(8):
                mc = slice(128 * m, 128 * m + 128)
                for i in range(4):
                    pg_, pb_ = pg[k % 2], pb[k % 2]
                    k += 1
                    for c in range(8):
                        P.mm(pg_[:], wg[:, 8 * i + c, mc], hT[:, c, :], start=(c == 0), stop=(c == 7))
                    for c2 in range(2):
                        src = ybr[:, 2 * i + c2, :] if i < 3 else ycT[:, c2, :]
                        P.mm(pb_[:], wb[:, 2 * i + c2, mc], src, start=(c2 == 0), stop=(c2 == 1))
                    P.act(sg[:], pg_[:], AF.Sigmoid)
                    if i == 0:
                        P.tt(macc[:], sg[:], pb_[:], ALU.mult)
                    elif i < 3:
                        P.tt(mtmp[:], sg[:], pb_[:], ALU.mult)
                        P.tt(macc[:], macc[:], mtmp[:], ALU.add, eng='pool')
                    else:
                        P.tt(mtmp[:], sg[:], pb_[:], ALU.mult)
                        P.tt(merged[:, m, :], macc[:], mtmp[:], ALU.add, eng='pool')
            for m in range(8):
                mc = slice(128 * m, 128 * m + 128)
                for c in range(8):
                    P.mm(po[:], wout[:, c, mc], merged[:, c, :], start=(c == 0), stop=(c == 7))
                P.tt(xt[:, m, :], xt[:, m, :], po[:], ALU.add)
            P.dma(x1v[:, :, t0:t0 + 512], xt[:])
        P.emit()
        print("build_C1: sbuf bytes/partition", A.bytes, "ops", P.nops)
    return nc


def build_C2(NTOK, NF, moe):
    nc = bass.Bass("TRN2", target_bir_lowering=False)
    NT = NTOK // 512
    DFF = NF * 128
    din = lambda n, s, d=F32: nc.dram_tensor(n, list(s), d, kind="ExternalInput").ap()
    x1_d = din("x1T", [1024, NTOK])
    xa_d = din("xaT", [1024, NTOK])
    gffn_d = din("gffn", [128, 8])
    w1_d = din("w1", [1024, DFF]); w3_d = din("w3", [1024, DFF]); w2_d = din("w2", [DFF, 1024])
    cst_d = din("cst", [128, 8])
    if moe:
        rt_d = din("router", [1024, 8])
        esel_d = din("esel", [128, 8])
        idn_d = din("ident", [128, 128])
    x2_d = nc.dram_tensor("x2T", [1024, NTOK], F32, kind="ExternalOutput").ap()
    with contextlib.ExitStack() as st:
        A = Alloc(nc, st); P = Prog(nc)
        gffn = A.sb("gffn_s", [128, 8]); P.dma(gffn[:], gffn_d)
        cst = A.sb("cst_s", [128, 8]); P.dma(cst[:], cst_d)
        ones_bf = A.sb("ones_bf", [128, 128], BF16); P.memset(ones_bf[:], 1.0)
        stg = A.sb("stg", [128, 2816])
        w1 = A.sb("w1_b", [128, 8, DFF], BF16); w3 = A.sb("w3_b", [128, 8, DFF], BF16); w2 = A.sb("w2_b", [128, NF, 1024], BF16)
        load_cast(P, w1[:], w1_d.rearrange("(c p) n -> p c n", p=128), stg)
        load_cast(P, w3[:], w3_d.rearrange("(c p) n -> p c n", p=128), stg)
        load_cast(P, w2[:], w2_d.rearrange("(c p) n -> p c n", p=128), stg)
        xt = A.sb("xt", [128, 8, 512]); xa = A.sb("xa", [128, 8, 512]) if moe else xt
        sq = A.sb("sq", [128, 8, 512], BF16); hT = A.sb("hT", [128, 8, 512], BF16)
        rstd = A.sb("rstd", [128, 512])
        actT = A.sb("actT", [128, NF, 512], BF16)
        sil = [A.sb("sil%d" % i, [128, 512]) for i in range(2)]
        pa = [A.ps("pa%d" % i, [128, 512]) for i in range(2)]
        pb = [A.ps("pb%d" % i, [128, 512]) for i in range(2)]
        pn = A.ps("pn", [128, 512]); po = A.ps("po", [128, 512]); pr = A.ps("pr", [128, 512])
        if moe:
            rtf = A.sb("rtf", [128, 8, 8]); P.dma(rtf[:], rt_d.rearrange("(c p) n -> p c n", p=128))
            rtb = A.sb("rtb", [128, 8, 8], BF16); P.copy(rtb[:], rtf[:])
            esel = A.sb("esel_s", [128, 8]); P.dma(esel[:], esel_d)
            idf = A.sb("idf", [128, 128]); P.dma(idf[:], idn_d)
            idb = A.sb("idb", [128, 128], BF16); P.copy(idb[:], idf[:])
            lg = A.sb("lg", [128, 8]); mx = A.sb("mx", [128, 8]); ex = A.sb("ex", [128, 8]); msk = A.sb("msk", [128, 8])
            s1 = A.sb("s1", [128, 4]); wrep = A.sb("wrep", [128, 128], BF16); wbc = A.sb("wbc", [128, 512])
        x1v = x1_d.rearrange("(c p) t -> p c t", p=128)
        xav = xa_d.rearrange("(c p) t -> p c t", p=128)
        x2v = x2_d.rearrange("(c p) t -> p c t", p=128)
        for g in range(NT):
            t0 = g * 512
            P.dma(xt[:], x1v[:, :, t0:t0 + 512])
            if moe:
                P.dma(xa[:], xav[:, :, t0:t0 + 512])
            rms_tile(P, xt[:], sq[:], ones_bf[:], pn[:], rstd[:], cst, hT, gffn)
            if moe:
                for blk in range(4):
                    bc = slice(128 * blk, 128 * blk + 128)
                    for c in range(8):
                        P.mm(pr[:, 0:8], hT[:, c, bc], rtb[:, c, :], start=(c == 0), stop=(c == 7))
                    P.copy(lg[:], pr[:, 0:8], eng='act')
                    P.add('dve', lambda e: e.max(mx[:], lg[:]), reads=[lg[:]], writes=[mx[:]])
                    P.tt(s1[:, 0:1], mx[:, 1:2], mx[:, 0:1], ALU.subtract)
                    P.act(s1[:, 0:1], s1[:, 0:1], AF.Exp)
                    P.ts(s1[:, 0:1], s1[:, 0:1], 1.0, None, ALU.add)
                    P.add('dve', lambda e: e.reciprocal(s1[:, 1:2], s1[:, 0:1]), reads=[s1[:, 0:1]], writes=[s1[:, 1:2]])
                    P.ts(s1[:, 2:3], mx[:, 0:1], -1.0, None, ALU.mult)
                    P.act(ex[:], lg[:], AF.Exp, bias=s1[:, 2:3], scale=1.0)
                    P.ts(msk[:], lg[:], mx[:, 1:2], None, ALU.is_ge)
                    P.stt(ex[:], ex[:], s1[:, 1:2], msk[:], ALU.mult, ALU.mult)
                    P.tt(ex[:], ex[:], esel[:], ALU.mult)
                    P.reduce(s1[:, 3:4], ex[:], ALU.add)
                    P.copy(wrep[:], s1[:, 3:4].to_broadcast([128, 128]), eng='dve')
                    P.mm(pr[:, 128:256], wrep[:], idb[:])
                    P.copy(wbc[:, bc], pr[:, 128:256], eng='act')
            for f in range(NF):
                fc = slice(128 * f, 128 * f + 128)
                pa_, pb_ = pa[f % 2], pb[f % 2]
                for c in range(8):
                    P.mm(pa_[:], w1[:, c, fc], hT[:, c, :], start=(c == 0), stop=(c == 7))
                for c in range(8):
                    P.mm(pb_[:], w3[:, c, fc], hT[:, c, :], start=(c == 0), stop=(c == 7))
                P.act(sil[f % 2][:], pa_[:], AF.Silu)
                if moe:
                    P.tt(sil[f % 2][:], sil[f % 2][:], pb_[:], ALU.mult)
                    P.tt(actT[:, f, :], sil[f % 2][:], wbc[:], ALU.mult, eng='pool')
                else:
                    P.tt(actT[:, f, :], sil[f % 2][:], pb_[:], ALU.mult)
            for m in range(8):
                mc = slice(128 * m, 128 * m + 128)
                for f in range(NF):
                    P.mm(po[:], w2[:, f, mc], actT[:, f, :], start=(f == 0), stop=(f == NF - 1))
                P.tt(xa[:, m, :], xa[:, m, :], po[:], ALU.add)
            P.dma(x2v[:, :, t0:t0 + 512], xa[:])
        P.emit()
        print("build_C2: sbuf bytes/partition", A.bytes, "ops", P.nops)
    return nc


_NC_CACHE = {}


def _get(key, fn):
    if key not in _NC_CACHE:
        _NC_CACHE[key] = fn()
    return _NC_CACHE[key]


def kernel(**inp):
    inp = {k: np.asarray(v) for k, v in inp.items()}
    x = inp["x"]
    B, S, D = x.shape
    NTOK = B * S // 8
    SH = S // NTOK
    cstv = np.zeros((128, 8), np.float32)
    for i, v in enumerate(CST):
        cstv[:, i] = v
    tables = [_const_tables_B(S, h) for h in range(4)]
    xT = np.ascontiguousarray(np.transpose(x, (0, 2, 1)))
    L = inp["w_in"].shape[0]
    ident = np.eye(128, dtype=np.float32)
    for l in range(L):
        ncB = _get(("B", S), lambda: build_B(S))
        maps = [prep_B(inp, l, c // 4, c % 4, xT[c // 4], tables) for c in range(8)]
        res = run_bass_kernel_spmd(ncB, maps, core_ids=list(range(8)))
        yT = np.stack([np.asarray(res.results[c]["yT"]) for c in range(8)])
        yT = yT.reshape(B, 4, 3, 64, S).transpose(0, 2, 1, 3, 4).reshape(B, 3, 256, S)
        ncC1 = _get(("C1", NTOK), lambda: build_C1(NTOK))
        xTp = np.concatenate([np.zeros((B, D, 32), np.float32), xT], axis=2)
        cv = np.zeros((128, 2, 34), np.float32)
        for cc in range(2):
            sl = slice(128 * cc, 128 * cc + 128)
            cv[:, cc, 0:31] = inp["conv_dw"][l][:, sl].T
            cv[:, cc, 31] = inp["conv_b"][l][sl]
            cv[:, cc, 32] = inp["conv_ln_w"][l][sl]
            cv[:, cc, 33] = inp["conv_ln_b"][l][sl]
        gmix = np.ascontiguousarray(inp["norm_mix"][l].reshape(8, 128).T)
        wconv = np.ascontiguousarray(inp["w_in"][l][:, 2816:3328])
        maps = []
        for c in range(8):
            b, s0 = c // SH, (c % SH) * NTOK
            maps.append(dict(xTh=np.ascontiguousarray(xTp[b, :, s0:s0 + 32 + NTOK]), gmix=gmix, wconv=wconv, cvec=cv,
                             yT=np.ascontiguousarray(yT[b, :, :, s0:s0 + NTOK]), wg=inp["w_gate"][l], wb=inp["w_branch"][l],
                             wout=inp["w_out"][l], cst=cstv))
        res = run_bass_kernel_spmd(ncC1, maps, core_ids=list(range(8)))
        x1 = [np.asarray(res.results[c]["x1T"]) for c in range(8)]
        gffn = np.ascontiguousarray(inp["norm_ffn"][l].reshape(8, 128).T)
        if l % 2 == 0:
            ncC2 = _get(("C2", NTOK, 22, False), lambda: build_C2(NTOK, 22, False))
            j = l // 2
            maps = [dict(x1T=x1[c], xaT=x1[c], gffn=gffn, w1=inp["ffn_w1"][j], w3=inp["ffn_w3"][j], w2=inp["ffn_w2"][j], cst=cstv)
                    for c in range(8)]
            res = run_bass_kernel_spmd(ncC2, maps, core_ids=list(range(8)))
            x2 = [np.asarray(res.results[c]["x2T"]) for c in range(8)]
        else:
            ncC2 = _get(("C2moe", NTOK), lambda: build_C2moe(NTOK))
            j = l // 2
            maps = [dict(x1T=x1[c], gffn=gffn, w1=inp["moe_w1"][j], w3=inp["moe_w3"][j], w2=inp["moe_w2"][j],
                         cst=cstv, router=inp["router"][j], ident=ident) for c in range(8)]
            res = run_bass_kernel_spmd(ncC2, maps, core_ids=list(range(8)))
            x2 = [np.asarray(res.results[c]["x2T"]) for c in range(8)]
        xT = np.stack([np.concatenate(x2[b * SH:(b + 1) * SH], axis=1) for b in range(B)])
    return np.ascontiguousarray(np.transpose(xT, (0, 2, 1))).astype(np.float32)


def build_C2moe(NTOK, NE=8, NF=11):
    nc = bass.Bass("TRN2", target_bir_lowering=False)
    NT = NTOK // 512
    DFF = NF * 128
    din = lambda n, s, d=F32: nc.dram_tensor(n, list(s), d, kind="ExternalInput").ap()
    x1_d = din("x1T", [1024, NTOK])
    gffn_d = din("gffn", [128, 8])
    w1_d = din("w1", [NE, 1024, DFF]); w3_d = din("w3", [NE, 1024, DFF]); w2_d = din("w2", [NE, DFF, 1024])
    cst_d = din("cst", [128, 8])
    rt_d = din("router", [1024, 8])
    idn_d = din("ident", [128, 128])
    x2_d = nc.dram_tensor("x2T", [1024, NTOK], F32, kind="ExternalOutput").ap()
    with contextlib.ExitStack() as st:
        A = Alloc(nc, st); P = Prog(nc)
        gffn = A.sb("gffn_s", [128, 8]); P.dma(gffn[:], gffn_d)
        cst = A.sb("cst_s", [128, 8]); P.dma(cst[:], cst_d)
        ones_bf = A.sb("ones_bf", [128, 128], BF16); P.memset(ones_bf[:], 1.0)
        stg = A.sb("stg", [128, 2816])
        w1 = A.sb("w1_b", [128, 8, DFF], BF16); w3 = A.sb("w3_b", [128, 8, DFF], BF16); w2 = A.sb("w2_b", [128, NF, 1024], BF16)
        xt = A.sb("xt", [128, 8, 512])
        sq = A.sb("sq", [128, 2, 512], BF16)
        hT_all = A.sb("hT_all", [128, NT * 8, 512], BF16)
        rstd = A.sb("rstd", [128, 512])
        actT = A.sb("actT", [128, NF, 512], BF16)
        sil = [A.sb("sil%d" % i, [128, 512]) for i in range(2)]
        pa = [A.ps("pa%d" % i, [128, 512]) for i in range(2)]
        pb = [A.ps("pb%d" % i, [128, 512]) for i in range(2)]
        pn = A.ps("pn", [128, 512]); po = A.ps("po", [128, 512]); pr = A.ps("pr", [128, 512])
        rtf = A.sb("rtf", [128, 8, 8]); P.dma(rtf[:], rt_d.rearrange("(c p) n -> p c n", p=128))
        rtb = A.sb("rtb", [128, 8, 8], BF16); P.copy(rtb[:], rtf[:])
        idf = A.sb("idf", [128, 128]); P.dma(idf[:], idn_d)
        idb = A.sb("idb", [128, 128], BF16); P.copy(idb[:], idf[:])
        lg = A.sb("lg", [128, 8]); mx = A.sb("mx", [128, 8]); ex = A.sb("ex", [128, 8]); msk = A.sb("msk", [128, 8])
        s1 = A.sb("s1", [128, 4]); wrep = A.sb("wrep", [128, 128], BF16); wbc = A.sb("wbc", [128, 512])
        wgt_all = A.sb("wgt_all", [128, NT * 4, 8])
        x1v = x1_d.rearrange("(c p) t -> p c t", p=128)
        x2v = x2_d.rearrange("(c p) t -> p c t", p=128)

        class _HT:
            def __init__(self, g):
                self.g = g

            def __getitem__(self, key):
                p, c, t = key
                return hT_all[p, self.g * 8 + c, t]

        for e in range(NE):
            load_cast(P, w1[:], w1_d[e].rearrange("(c p) n -> p c n", p=128), stg)
            load_cast(P, w3[:], w3_d[e].rearrange("(c p) n -> p c n", p=128), stg)
            load_cast(P, w2[:], w2_d[e].rearrange("(c p) n -> p c n", p=128), stg)
            for g in range(NT):
                t0 = g * 512
                P.dma(xt[:], (x1v if e == 0 else x2v)[:, :, t0:t0 + 512])
                hT = _HT(g)
                if e == 0:
                    rms_tile(P, xt[:], sq[:], ones_bf[:], pn[:], rstd[:], cst, hT, gffn)
                    for blk in range(4):
                        bc = slice(128 * blk, 128 * blk + 128)
                        for c in range(8):
                            P.mm(pr[:, 0:8], hT[:, c, bc], rtb[:, c, :], start=(c == 0), stop=(c == 7))
                        P.copy(lg[:], pr[:, 0:8], eng='act')
                        P.add('dve', lambda e_: e_.max(mx[:], lg[:]), reads=[lg[:]], writes=[mx[:]])
                        P.tt(s1[:, 0:1], mx[:, 1:2], mx[:, 0:1], ALU.subtract)
                        P.act(s1[:, 0:1], s1[:, 0:1], AF.Exp)
                        P.ts(s1[:, 0:1], s1[:, 0:1], 1.0, None, ALU.add)
                        P.add('dve', lambda e_: e_.reciprocal(s1[:, 1:2], s1[:, 0:1]), reads=[s1[:, 0:1]], writes=[s1[:, 1:2]])
                        P.ts(s1[:, 2:3], mx[:, 0:1], -1.0, None, ALU.mult)
                        P.act(ex[:], lg[:], AF.Exp, bias=s1[:, 2:3], scale=1.0)
                        P.ts(msk[:], lg[:], mx[:, 1:2], None, ALU.is_ge)
                        P.stt(wgt_all[:, g * 4 + blk, :], ex[:], s1[:, 1:2], msk[:], ALU.mult, ALU.mult)
                for blk in range(4):
                    bc = slice(128 * blk, 128 * blk + 128)
                    P.copy(wrep[:], wgt_all[:, g * 4 + blk, e:e + 1].to_broadcast([128, 128]), eng='dve')
                    P.mm(pr[:, 128:256], wrep[:], idb[:])
                    P.copy(wbc[:, bc], pr[:, 128:256], eng='act')
                for f in range(NF):
                    fc = slice(128 * f, 128 * f + 128)
                    pa_, pb_ = pa[f % 2], pb[f % 2]
                    for c in range(8):
                        P.mm(pa_[:], w1[:, c, fc], hT[:, c, :], start=(c == 0), stop=(c == 7))
                    for c in range(8):
                        P.mm(pb_[:], w3[:, c, fc], hT[:, c, :], start=(c == 0), stop=(c == 7))
                    P.act(sil[f % 2][:], pa_[:], AF.Silu)
                    P.tt(sil[f % 2][:], sil[f % 2][:], pb_[:], ALU.mult)
                    P.tt(actT[:, f, :], sil[f % 2][:], wbc[:], ALU.mult, eng='pool')
                for m in range(8):
                    mc = slice(128 * m, 128 * m + 128)
                    for f in range(NF):
                        P.mm(po[:], w2[:, f, mc], actT[:, f, :], start=(f == 0), stop=(f == NF - 1))
                    P.tt(xt[:, m, :], xt[:, m, :], po[:], ALU.add)
                P.dma(x2v[:, :, t0:t0 + 512], xt[:])
        P.emit()
        print("build_C2moe: sbuf bytes/partition", A.bytes, "ops", P.nops)
    return nc
```

```python
import numpy as np
import concourse.bass as bass
import concourse.mybir as mybir

F32 = mybir.dt.float32
BF16 = mybir.dt.bfloat16
AF = mybir.ActivationFunctionType
ALU = mybir.AluOpType
AX = mybir.AxisListType

N_DMA_SEMS = 40
PSUM_NAMES = set()


def _region(ap):
    t = ap.tensor
    name = ap.name
    dims = ap.ap
    off = ap.offset
    space = str(ap.space)
    if 'DRAM' in space.upper() or 'HBM' in space.upper() or not hasattr(ap, 'base_partition') or len(dims) == 0:
        lo = off
        hi = off + sum((c - 1) * abs(s) for s, c in dims) + 1
        return (name, 0, 1, lo, hi)
    pstride = dims[0][0]
    pcount = dims[0][1]
    if pstride == 0:
        pstride = 1 << 30
    p0 = off // pstride if pstride < (1 << 30) else 0
    f0 = off - p0 * pstride if pstride < (1 << 30) else off
    f1 = f0 + sum((c - 1) * abs(s) for s, c in dims[1:]) + 1
    if name in PSUM_NAMES:
        return (name, (p0 // 32) * 32, ((p0 + pcount + 31) // 32) * 32, 0, 1 << 30)
    return (name, p0, p0 + pcount, f0, f1)


class Prog:
    def __init__(self, nc):
        self.nc = nc
        self.ops = {e: [] for e in ('pe', 'act', 'dve', 'pool', 'sp')}
        self.cnt = {e: 0 for e in ('pe', 'act', 'dve', 'pool')}
        self.recs = {}
        self.events = []
        self.known = {e: {} for e in self.ops}
        self.dma_uses = [0] * N_DMA_SEMS
        self.dma_next = 0
        self.nops = 0
        self.out_events = []

    def _is_dram(self, ap):
        s = str(ap.space).upper()
        return 'DRAM' in s or 'HBM' in s

    def add(self, eng, fn, reads=(), writes=(), dma=False):
        waits = {}

        def need(ev):
            semkey, val, _, _ = ev
            if waits.get(semkey, 0) < val:
                waits[semkey] = val

        rregs = [_region(a) for a in reads]
        wregs = [_region(a) for a in writes]
        idx = len(self.events)
        for r in rregs:
            for rec in self.recs.get(r[0], ()):
                if rec[5] != 'w':
                    continue
                if rec[1] < r[2] and r[1] < rec[2] and rec[3] < r[4] and r[3] < rec[4]:
                    ev = self.events[rec[6]]
                    if ev[2] == eng and not ev[3] and not dma and eng == 'pe':
                        continue
                    need(ev)
        for r in wregs:
            for rec in self.recs.get(r[0], ()):
                if rec[1] < r[2] and r[1] < rec[2] and rec[3] < r[4] and r[3] < rec[4]:
                    ev = self.events[rec[6]]
                    if ev[2] == eng and not ev[3] and not dma:
                        if eng == 'pe':
                            continue
                    need(ev)
        if dma:
            j = self.dma_next
            self.dma_next = (j + 1) % N_DMA_SEMS
            prev = self.dma_uses[j] * 16
            self.dma_uses[j] += 1
            val = prev + 16
            semkey = ('dma', j)
            if prev > 0:
                if waits.get(semkey, 0) < prev:
                    waits[semkey] = prev
            ev = (semkey, val, eng, True)
        else:
            self.cnt[eng] += 1
            ev = ((eng,), self.cnt[eng], eng, False)
        self.events.append(ev)
        kn = self.known[eng]
        wl = []
        for sk, v in waits.items():
            if kn.get(sk, 0) >= v:
                continue
            kn[sk] = v
            wl.append((sk, v))
        self.ops[eng].append((wl, fn, ev))
        evs = self.events
        for r in wregs:
            lst = self.recs.setdefault(r[0], [])
            lst[:] = [rec for rec in lst
                      if not (r[1] <= rec[1] and rec[2] <= r[2] and r[3] <= rec[3] and rec[4] <= r[4])]
            lst.append((r[0], r[1], r[2], r[3], r[4], 'w', idx))
        for r in rregs:
            lst = self.recs.setdefault(r[0], [])
            if not dma:
                lst[:] = [rec for rec in lst
                          if not (rec[5] == 'r' and evs[rec[6]][2] == eng and not evs[rec[6]][3]
                                  and r[1] <= rec[1] and rec[2] <= r[2] and r[3] <= rec[3] and rec[4] <= r[4])]
            lst.append((r[0], r[1], r[2], r[3], r[4], 'r', idx))
        self.nops += 1
        return ev

    def dma(self, out, in_, eng='sp', **kw):
        ev = self.add(eng, lambda e: e.dma_start(out=out, in_=in_, **kw), reads=[in_], writes=[out], dma=True)
        if self._is_dram(out):
            self.out_events.append(ev)
        return ev

    def mm(self, out, lhsT, rhs, start=True, stop=True, **kw):
        return self.add('pe', lambda e: e.matmul(out, lhsT, rhs, start=start, stop=stop, **kw),
                        reads=[lhsT, rhs], writes=[out])

    def transpose(self, out, in_, ident):
        return self.add('pe', lambda e: e.transpose(out, in_, ident), reads=[in_, ident], writes=[out])

    def act(self, out, in_, func, bias=None, scale=None, accum_out=None, eng='act'):
        kw = {}
        reads = [in_]
        writes = [out]
        if bias is not None:
            kw['bias'] = bias
            if not isinstance(bias, (int, float)):
                reads.append(bias)
        if scale is not None:
            kw['scale'] = scale
            if not isinstance(scale, (int, float)):
                reads.append(scale)
        if accum_out is not None:
            kw['accum_out'] = accum_out
            writes.append(accum_out)
        return self.add('act', lambda e: e.activation(out, in_, func, **kw), reads=reads, writes=writes)

    def tt(self, out, in0, in1, op, eng='dve'):
        return self.add(eng, lambda e: e.tensor_tensor(out, in0, in1, op), reads=[in0, in1], writes=[out])

    def ts(self, out, in0, s1, s2, op0, op1=None, eng='dve', accum_out=None):
        reads = [in0] + [s for s in (s1, s2) if s is not None and not isinstance(s, (int, float))]
        writes = [out] + ([accum_out] if accum_out is not None else [])
        kw = {}
        if op1 is not None:
            kw['op1'] = op1
        if accum_out is not None:
            kw['accum_out'] = accum_out
        return self.add(eng, lambda e: e.tensor_scalar(out, in0, s1, s2, op0, **kw), reads=reads, writes=writes)

    def stt(self, out, in0, scalar, in1, op0, op1, eng='dve', accum_out=None):
        reads = [in0, in1] + ([scalar] if not isinstance(scalar, (int, float)) else [])
        writes = [out] + ([accum_out] if accum_out is not None else [])
        kw = {}
        eng = 'dve'
        if accum_out is not None:
            kw['accum_out'] = accum_out
        return self.add(eng, lambda e: e.scalar_tensor_tensor(out, in0, scalar, in1, op0, op1, **kw),
                        reads=reads, writes=writes)

    def copy(self, out, in_, eng='dve'):
        if eng == 'act':
            return self.add('act', lambda e: e.copy(out, in_), reads=[in_], writes=[out])
        return self.add(eng, lambda e: e.tensor_copy(out, in_), reads=[in_], writes=[out])

    def memset(self, out, val, eng='pool'):
        return self.add(eng, lambda e: e.memset(out, val), reads=[], writes=[out])

    def reduce(self, out, in_, op, axis=AX.X, eng='dve'):
        return self.add(eng, lambda e: e.tensor_reduce(out, in_, axis, op), reads=[in_], writes=[out])

    def emit(self):
        nc = self.nc
        import contextlib
        with contextlib.ExitStack() as st:
            sems = {}
            for e in ('pe', 'act', 'dve', 'pool'):
                sems[(e,)] = st.enter_context(nc.semaphore("s_" + e))
            for j in range(N_DMA_SEMS):
                sems[('dma', j)] = st.enter_context(nc.semaphore("s_dma%d" % j))
            final = {}
            for ev in self.out_events:
                if final.get(ev[0], 0) < ev[1]:
                    final[ev[0]] = ev[1]
            for e in ('pe', 'act', 'dve', 'pool'):
                if self.cnt[e] > 0:
                    final[(e,)] = self.cnt[e]
            for j in range(N_DMA_SEMS):
                if self.dma_uses[j] > 0:
                    final[('dma', j)] = self.dma_uses[j] * 16
            block = st.enter_context(nc.Block())
            ops = self.ops

            def run(engname):
                def f(eng):
                    for wl, fn, ev in ops[engname]:
                        for sk, v in wl:
                            eng.wait_ge(sems[sk], v)
                        ins = fn(eng)
                        ins.then_inc(sems[ev[0]], 16 if ev[3] else 1)
                    if engname == 'sp':
                        for sk, v in final.items():
                            eng.wait_ge(sems[sk], v)
                return f

            block.sync(run('sp'))
            block.tensor(run('pe'))
            block.scalar(run('act'))
            block.vector(run('dve'))
            block.gpsimd(run('pool'))


import contextlib
import ml_dtypes
from concourse.bass_utils import run_bass_kernel_spmd

D_MODEL = 1024
NH = 4
HD = 64
RW_SCALE = 0.606531
C_R, C_K, C_V, C_PW, C_PA, C_SQ, C_PG, C_SK, C_SV, C_RQ, C_RK, C_RV, C_RG = \
    0, 64, 128, 192, 256, 320, 384, 512, 576, 640, 704, 768, 832
NCOL_B = 896
B_PARTS = ('rw', 'ret', 'sb')
RET_DBG = 3
(V_MUR, V_MUK, V_MUV, V_MUPW, V_MUPA, V_W0, V_A0, V_KK, V_KA, V_RK, V_LNW, V_LNB,
 V_SBQ, V_SBK, V_GN) = range(15)
CST = [1e-6, 64e-5, 1e-5, 1.0, 1e-12, 0.0]
K_EPS6, K_EPSRW, K_EPS5, K_ONE, K_TINY, K_ZERO = range(6)


class Alloc:
    def __init__(self, nc, st):
        self.nc = nc
        self.st = st
        self.bytes = 0

    def sb(self, name, shape, dt=F32):
        n = 1
        for s in shape[1:]:
            n *= s
        self.bytes += n * (4 if dt == F32 else 2)
        return self.st.enter_context(self.nc.sbuf_tensor(name, list(shape), dt))

    def ps(self, name, shape, dt=F32):
        PSUM_NAMES.add(name)
        return self.st.enter_context(self.nc.psum_tensor(name, list(shape), dt))


def rms_tile(P, xt, sq, ones_bf, pn, rstd, cst, hT, gain, tmpf=None):
    nsq = sq.shape[1]
    if nsq == 8:
        P.act(sq, xt, AF.Square)
    for c in range(8):
        if nsq < 8:
            P.act(sq[:, c % nsq, :], xt[:, c, :], AF.Square)
        P.mm(pn, ones_bf, sq[:, c % nsq, :], start=(c == 0), stop=(c == 7))
    P.act(rstd, pn, AF.Sqrt, scale=1.0 / D_MODEL, bias=cst[:, K_EPS6:K_EPS6 + 1])
    P.add('dve', lambda e: e.reciprocal(rstd, rstd), reads=[rstd], writes=[rstd])
    for c in range(8):
        P.stt(hT[:, c, :], xt[:, c, :], gain[:, c:c + 1], rstd, ALU.mult, ALU.mult,
              eng=('dve' if c % 2 == 0 else 'pool'))


def ln_feat(P, A, y, pn, ones_s, eps_col, tmp1, tmp2, sbf, npart=64, N=512):
    P.copy(sbf, y, eng='pool')
    P.mm(pn[0:npart, 0:N], ones_s, sbf, start=True, stop=True)
    P.tt(y, y, pn[0:npart, 0:N], ALU.subtract)
    P.act(sbf, y, AF.Square)
    P.mm(pn[0:npart, 0:N], ones_s, sbf, start=True, stop=True)
    P.act(tmp2, pn[0:npart, 0:N], AF.Sqrt, bias=eps_col, scale=1.0)
    P.add('dve', lambda e: e.reciprocal(tmp2, tmp2), reads=[tmp2], writes=[tmp2])
    P.tt(y, y, tmp2, ALU.mult)


def build_B(S):
    nc = bass.Bass("TRN2", target_bir_lowering=False)
    NT = S // 512
    NB = S // 128
    din = lambda n, s, d=F32: nc.dram_tensor(n, list(s), d, kind="ExternalInput").ap()
    xT = din("xT", [1024, S])
    gmix_d = din("gmix", [128, 8])
    wh_d = din("wh", [1024, NCOL_B])
    vec64_d = din("vec64", [64, 16])
    mupg_d = din("mupg", [128, 1])
    w2_d = din("w2h", [64, 64])
    a2_d = din("a2h", [64, 64])
    g2_d = din("g2h", [128, 64])
    cos_d = din("cosT", [64, S])
    sin_d = din("sinT", [64, S])
    cst_d = din("cst", [128, 8])
    tab_d = din("tab", [128, 6, 128])
    tab64_d = din("tab64", [64, 5, 512], BF16)
    qdec_d = din("qdec4", [64, 512])
    kdec_d = din("kdec", [128, 2])
    y_d = nc.dram_tensor("yT", [3, 64, S], BF16, kind="ExternalOutput").ap()

    with contextlib.ExitStack() as st:
        A = Alloc(nc, st)
        P = Prog(nc)
        gmix = A.sb("gmix_s", [128, 8]); P.dma(gmix[:], gmix_d)
        vec = A.sb("vec_s", [64, 16]); P.dma(vec[:], vec64_d)
        mupg = A.sb("mupg_s", [128, 1]); P.dma(mupg[:], mupg_d)
        w2f = A.sb("w2_s", [64, 64]); P.dma(w2f[:], w2_d)
        a2f = A.sb("a2_s", [64, 64]); P.dma(a2f[:], a2_d)
        g2f = A.sb("g2_s", [128, 64]); P.dma(g2f[:], g2_d)
        w2 = A.sb("w2_b", [64, 64], BF16); P.copy(w2[:], w2f[:])
        a2 = A.sb("a2_b", [64, 64], BF16); P.copy(a2[:], a2f[:])
        g2 = A.sb("g2_b", [128, 64], BF16); P.copy(g2[:], g2f[:])
        cst = A.sb("cst_s", [128, 8]); P.dma(cst[:], cst_d)
        tab = A.sb("tab_s", [128, 6, 128]); P.dma(tab[:], tab_d)
        tab64 = A.sb("tab64_s", [64, 5, 512], BF16); P.dma(tab64[:], tab64_d)
        qdec4t = A.sb("qdec4_s", [64, 512]); P.dma(qdec4t[:], qdec_d)
        kdec = A.sb("kdec_s", [128, 2]); P.dma(kdec[:], kdec_d)
        ident = A.sb("identb", [128, 128], BF16); P.copy(ident[:], tab[:, 0, :])
        ident = ident[:]
        mask_lt = A.sb("mask_lt", [128, 128], BF16); P.copy(mask_lt[:], tab[:, 1, :])
        negrev = A.sb("negrev", [128, 128], BF16); P.copy(negrev[:], tab[:, 2, :])
        decayT = tab[:, 3, :]
        maskU8, maskL8, maskUI8, I8, rstm = (tab64[:, i, :] for i in range(5))
        qdec4 = qdec4t[:]
        ones_bf = A.sb("ones_bf", [128, 128], BF16); P.memset(ones_bf[:], 1.0)
        negones = A.sb("negones", [128, 128], BF16); P.memset(negones[:], -1.0)
        ones64 = A.sb("ones64", [64, 64], BF16); P.memset(ones64[:], 1.0)
        ones64s = A.sb("ones64s", [64, 64], BF16); P.memset(ones64s[:], 1.0 / 64)
        qn8 = A.sb("qn8", [64, 1]); P.ts(qn8[:], vec[:, V_SBQ:V_SBQ + 1], 0.125, None, ALU.mult)
        V = lambda i: vec[:, i:i + 1]
        C = lambda i, n=64: cst[0:n, i:i + 1]

        xts = [A.sb("xt0", [128, 8, 512])] * 2
        wbf = A.sb("wbf", [128, 8, NCOL_B], BF16)
        sq = A.sb("sq", [128, 2, 512], BF16)
        hT = A.sb("hT", [128, 8, 512], BF16)
        rstd = A.sb("rstd", [128, 512])
        kT_all = A.sb("kT_all", [64, S], BF16)
        v_all = A.sb("v_all", [128, NB, 64], BF16)
        pj = A.ps("pj", [128, 512]); pn = A.ps("pn", [128, 512])
        px0 = A.ps("px0", [128, 512]); px1 = A.ps("px1", [128, 512]); py = A.ps("py", [128, 512])
        pz = A.ps("pz", [128, 512]); pla = A.ps("pla", [128, 512]); po = A.ps("po", [128, 512])
        pzs = [pz, pj]; plas = [pla, py]

        whv = wh_d.rearrange("(c p) n -> p c n", p=128)
        for half in range(2):
            stg = xts[half][:].rearrange("p c t -> p (c t)")[:, 0:4 * NCOL_B].rearrange("p (c n) -> p c n", c=4)
            P.dma(stg, whv[:, 4 * half:4 * half + 4, :])
            P.copy(wbf[:, 4 * half:4 * half + 4, :], stg, eng=('dve' if half == 0 else 'act'))

        names = ["raw_r", "raw_k", "raw_v", "raw_pw", "raw_pa"]
        raws = [A.sb(n, [64, 513]) for n in names]
        rawg = A.sb("raw_pg", [128, 513])
        for r_ in raws:
            P.memset(r_[:, 0:1], 0.0)
            P.memset(r_[:, 512:513], 0.0)
        P.memset(rawg[:, 0:1], 0.0); P.memset(rawg[:, 512:513], 0.0)
        e1 = A.sb("sb_e1", [128, 512]); spb = A.sb("sb_spb", [128, 512], BF16)
        LS = A.sb("sb_LS", [128, 512])
        T = {}
        for n in ["r", "k", "v", "pw", "pa", "d", "logw", "lp", "ep", "em", "epm", "a", "g", "kk", "t1",
                  "kp", "al", "be", "kt", "rb", "bon", "ysb", "tm1", "tm2"]:
            T[n] = A.sb("rw_" + n, [64, 512], BF16 if n in ("al", "be", "kt", "rb") else F32)
        sA = A.sb("rw_sA", [64, 512], BF16); sB = A.sb("rw_sB", [64, 512], BF16); vb = A.sb("rw_vb", [64, 512], BF16)
        dgb = spb
        pgm = LS; dg = e1
        M_ = [A.sb("rw_M%d" % i, [64, 8, 64], BF16) for i in range(2)]
        L_ = [A.sb("rw_L%d" % i, [64, 8, 64], BF16) for i in range(2)]
        Pt = A.sb("rw_Pt", [64, 8, 64], BF16); Pt32 = A.sb("rw_Pt32", [64, 8, 64])
        AakT = A.sb("rw_AakT", [64, 8, 64], BF16); ArbT = A.sb("rw_ArbT", [64, 8, 64], BF16)
        ArkT = A.sb("rw_ArkT", [64, 8, 64], BF16); Vt = A.sb("rw_Vt", [64, 8, 64], BF16); Bt = A.sb("rw_Bt", [64, 8, 64], BF16)
        Kt = A.sb("rw_Kt", [64, 8, 64], BF16); Us = A.sb("rw_Us", [64, 8, 64], BF16); Xs = A.sb("rw_Xs", [64, 64], BF16)
        H32 = A.sb("rw_H32", [64, 64]); H = A.sb("rw_H", [64, 64], BF16); Hd = A.sb("rw_Hd", [64, 64])
        P.memset(H32[:], 0.0); P.memset(H[:], 0.0)
        yout = [A.sb("yout%d" % i, [64, 512], BF16) for i in range(3)]
        cosb = A.sb("cosb", [64, 512]); sinb = A.sb("sinb", [64, 512])
        tq = T["d"]; t1 = T["tm1"]; t2 = T["tm2"]
        qr = A.sb("rt_qr", [64, 512], BF16); kr = A.sb("rt_kr", [64, 512], BF16); qd = A.sb("rt_qd", [64, 512], BF16)
        rv = A.sb("rt_rvb", [64, 512], BF16); rg = T["pa"]
        sc = A.sb("rt_sc", [128, 128], BF16); vtok = A.sb("rt_vtok", [128, 64], BF16); ktok = A.sb("rt_ktok", [128, 64], BF16)
        rst = A.sb("rt_st", [64, 64]); rst_bf = A.sb("rt_stbf", [64, 64], BF16)
        P.memset(rst[:], 0.0); P.memset(rst_bf[:], 0.0)
        ro = A.sb("rt_ro", [64, 512])
        sq_t = T["logw"]; sq_s = T["lp"]; sq_r = T["em"]
        qT = A.sb("sb_qT", [64, 512], BF16); svT = sB
        attn = A.sb("sb_attn", [128, 512], BF16)
        oacc = A.sb("sb_oacc", [64, 512])
        e1s = [e1, A.sb("sb_e1b", [128, 512])]; spbs = [spb, A.sb("sb_spb2", [128, 512], BF16)]
        attns = [attn, A.sb("sb_attn2", [128, 512], BF16)]
        ex2 = A.sb("sb_ex2", [128, 512], BF16)
        LSbs = [A.sb("sb_LSb%d" % i, [128, 512], BF16) for i in range(3)]

        xTv = xT.rearrange("(c p) t -> p c t", p=128)

        def proj(col0, M, ps):
            for c in range(8):
                P.mm(ps, wbf[:, c, col0:col0 + M], hT[:, c, :], start=(c == 0), stop=(c == 7))

        for g in range(NT):
            t0 = g * 512
            gens = []
            xt = xts[g % 2]
            P.dma(xt[:], xTv[:, :, t0:t0 + 512])
            P.dma(cosb[:], cos_d[:, t0:t0 + 512])
            P.dma(sinb[:], sin_d[:, t0:t0 + 512])
            rms_tile(P, xt[:], sq[:], ones_bf[:], pj[:], rstd[:], cst, hT, gmix)

            if 'rw' in B_PARTS:
                mixed = [T["r"], T["k"], T["v"], T["pw"], T["pa"]]
                mus = [V_MUR, V_MUK, V_MUV, V_MUPW, V_MUPA]

                def shift_mix(i, src_ps):
                    raw = raws[i]
                    if g > 0:
                        P.copy(raw[:, 0:1], raw[:, 512:513], eng='pool')
                    P.copy(raw[:, 1:513], src_ps, eng='act')
                    P.tt(T["d"][:], raw[:, 0:512], raw[:, 1:513], ALU.subtract, eng='pool')
                    P.stt(mixed[i][:], T["d"][:], V(mus[i]), raw[:, 1:513], ALU.mult, ALU.add)
                proj(C_R, 128, pj[:, :]); shift_mix(0, pj[0:64, :]); shift_mix(1, pj[64:128, :])
                proj(C_V, 128, pj[:, :]); shift_mix(2, pj[0:64, :]); shift_mix(3, pj[64:128, :])
                proj(C_PA, 128, pj[:, :]); shift_mix(4, pj[0:64, :])
                P.copy(ro[:], pj[64:128, :], eng='act')
                if g > 0:
                    P.copy(rawg[:, 0:1], rawg[:, 512:513], eng='pool')
                proj(C_PG, 128, pj[:, :])
                P.copy(rawg[:, 1:513], pj[:, :], eng='act')
                P.tt(dg[:], rawg[:, 0:512], rawg[:, 1:513], ALU.subtract, eng='pool')
                P.stt(pgm[:], dg[:], mupg[:, 0:1], rawg[:, 1:513], ALU.mult, ALU.add)

                r_, k_, v_ = T["r"], T["k"], T["v"]
                P.act(sA[:], T["pw"][:], AF.Tanh)
                P.mm(pn[0:64, :], w2[:], sA[:])
                P.act(T["logw"][:], pn[0:64, :], AF.Sigmoid, bias=V(V_W0))
                P.ts(T["logw"][:], T["logw"][:], -RW_SCALE, None, ALU.mult)
                P.add('dve', lambda e: e.tensor_tensor_scan(T["lp"][:], rstm, T["logw"][:], 0.0, ALU.mult, ALU.add),
                      reads=[rstm, T["logw"][:]], writes=[T["lp"][:]])
                P.act(T["ep"][:], T["lp"][:], AF.Exp)
                P.act(T["em"][:], T["lp"][:], AF.Exp, scale=-1.0)
                P.tt(T["tm1"][:], T["lp"][:], T["logw"][:], ALU.subtract, eng='pool')
                P.act(T["epm"][:], T["tm1"][:], AF.Exp)
                P.copy(sB[:], T["pa"][:], eng='pool')
                P.mm(pn[0:64, :], a2[:], sB[:])
                P.act(T["a"][:], pn[0:64, :], AF.Sigmoid, bias=V(V_A0))
                P.act(dgb[:], pgm[:], AF.Sigmoid)
                P.mm(pn[0:64, :], g2[:], dgb[:])
                P.copy(T["g"][:], pn[0:64, :], eng='act')
                P.ts(T["kk"][:], k_[:], V(V_KK), None, ALU.mult)
                P.act(sA[:], T["kk"][:], AF.Square)
                P.mm(pn[0:64, :], ones64[:], sA[:])
                P.act(T["tm2"][:], pn[0:64, :], AF.Sqrt)
                P.ts(T["tm2"][:], T["tm2"][:], 1e-12, None, ALU.max)
                P.add('dve', lambda e: e.reciprocal(T["tm2"][:], T["tm2"][:]), reads=[T["tm2"][:]], writes=[T["tm2"][:]])
                P.tt(T["kk"][:], T["kk"][:], T["tm2"][:], ALU.mult)
                P.ts(T["t1"][:], T["a"][:], 1.0, V(V_KA), ALU.subtract, ALU.mult)
                P.stt(T["kp"][:], T["t1"][:], 1.0, k_[:], ALU.add, ALU.mult)
                P.stt(T["al"][:], T["kk"][:], -1.0, T["epm"][:], ALU.mult, ALU.mult)
                P.tt(T["t1"][:], T["kk"][:], T["a"][:], ALU.mult, eng='pool')
                P.tt(T["be"][:], T["t1"][:], T["em"][:], ALU.mult, eng='pool')
                P.tt(T["kt"][:], T["kp"][:], T["em"][:], ALU.mult)
                P.tt(T["rb"][:], r_[:], T["ep"][:], ALU.mult, eng='pool')
                P.stt(sB[:], r_[:], V(V_RK), T["kp"][:], ALU.mult, ALU.mult)
                P.mm(pn[0:64, :], ones64[:], sB[:])
                P.copy(vb[:], v_[:], eng='pool')
                P.tt(T["bon"][:], pn[0:64, :], v_[:], ALU.mult)

                al, be, kt, rb = T["al"], T["be"], T["kt"], T["rb"]
                X0 = px0[0:64, :].rearrange("p (c n) -> p c n", c=8)
                X1 = px1[0:64, :].rearrange("p (c n) -> p c n", c=8)
                X2 = pn[0:64, :].rearrange("p (c n) -> p c n", c=8)
                f3 = lambda ap: ap
                fl = lambda t3: t3.rearrange("p c n -> p (c n)")
                cs = lambda c: slice(64 * c, 64 * c + 64)
                for c in range(8):
                    P.mm(X0[:, c, :], be[:, cs(c)], al[:, cs(c)])
                    P.mm(X1[:, c, :], al[:, cs(c)], be[:, cs(c)])
                P.tt(fl(Pt32[:]), px0[0:64, :], maskU8, ALU.mult)
                P.copy(fl(M_[0][:]), fl(Pt32[:]), eng='pool')
                P.tt(fl(L_[0][:]), px1[0:64, :], maskL8, ALU.mult, eng='pool' if False else 'dve')
                for c in range(8):
                    P.mm(X2[:, c, :], kt[:, cs(c)], al[:, cs(c)])
                P.tt(fl(AakT[:]), pn[0:64, :], maskU8, ALU.mult)
                for c in range(8):
                    P.mm(X0[:, c, :], be[:, cs(c)], rb[:, cs(c)])
                    P.mm(X1[:, c, :], kt[:, cs(c)], rb[:, cs(c)])
                P.tt(fl(ArbT[:]), px0[0:64, :], maskUI8, ALU.mult)
                P.tt(fl(ArkT[:]), px1[0:64, :], maskUI8, ALU.mult)
                for c in range(8):
                    P.mm(X2[:, c, :], vb[:, cs(c)], ident[0:64, 0:64])
                    P.mm(X0[:, c, :], be[:, cs(c)], ident[0:64, 0:64])
                    P.mm(X1[:, c, :], kt[:, cs(c)], ident[0:64, 0:64])
                P.copy(fl(Vt[:]), pn[0:64, :], eng='act')
                P.copy(fl(Bt[:]), px0[0:64, :], eng='dve')
                P.copy(fl(Kt[:]), px1[0:64, :], eng='act')
                P.tt(fl(Pt32[:]), fl(Pt32[:]), I8, ALU.add)
                P.copy(fl(Pt[:]), fl(Pt32[:]), eng='pool')
                for n in range(1, 6):
                    Mp, Lp = M_[(n - 1) % 2], L_[(n - 1) % 2]
                    Mn, Ln = M_[n % 2], L_[n % 2]
                    for c in range(8):
                        if n < 5:
                            P.mm(X0[:, c, :], Lp[:, c, :], Mp[:, c, :])
                        P.mm(X1[:, c, :], Mp[:, c, :], Lp[:, c, :])
                    if n < 5:
                        P.copy(fl(Mn[:]), px0[0:64, :], eng='act')
                    P.copy(fl(Ln[:]), px1[0:64, :], eng='dve')
                    for c in range(8):
                        P.mm(X2[:, c, :], Ln[:, c, :], Pt[:, c, :])
                    P.tt(fl(Pt32[:]), fl(Pt32[:]), pn[0:64, :], ALU.add)
                    P.copy(fl(Pt[:]), fl(Pt32[:]), eng='pool')
                def rw_gen():
                    ysb = T["ysb"]
                    for c in range(8):
                        epC = T["ep"][:, 64 * c + 63:64 * c + 64]
                        P.mm(X0[:, c, :], al[:, cs(c)], H[:], start=True, stop=False)
                        P.mm(X0[:, c, :], AakT[:, c, :], Vt[:, c, :], start=False, stop=True)
                        P.copy(Xs[:], X0[:, c, :], eng='act')
                        P.mm(X1[:, c, :], Pt[:, c, :], Xs[:])
                        P.copy(Us[:, c, :], X1[:, c, :], eng='dve')
                        yield
                        P.mm(X0[:, c, :], H[:], rb[:, cs(c)], start=True, stop=False)
                        P.mm(X0[:, c, :], Us[:, c, :], ArbT[:, c, :], start=False, stop=False)
                        P.mm(X0[:, c, :], Vt[:, c, :], ArkT[:, c, :], start=False, stop=True)
                        P.copy(ysb[:, cs(c)], X0[:, c, :], eng='act')
                        P.ts(Hd[:], H32[:], epC, None, ALU.mult, eng='pool')
                        P.mm(X2[:, c, :], Bt[:, c, :], Us[:, c, :], start=True, stop=False)
                        P.mm(X2[:, c, :], Kt[:, c, :], Vt[:, c, :], start=False, stop=True)
                        P.stt(H32[:], X2[:, c, :], epC, Hd[:], ALU.mult, ALU.add)
                        P.copy(H[:], H32[:], eng='pool')
                        yield
                    ln_feat(P, A, ysb[:], pn, ones64s[:], C(K_EPSRW), T["tm1"][:], T["tm2"][:], sA[:])
                    P.ts(ysb[:], ysb[:], V(V_LNW), V(V_LNB), ALU.mult, ALU.add)
                    P.tt(ysb[:], ysb[:], T["bon"][:], ALU.add)
                    P.tt(yout[0][:], ysb[:], T["g"][:], ALU.mult)
                    P.dma(y_d[0, :, t0:t0 + 512], yout[0][:])
                gens.append(rw_gen())

            if 'ret' in B_PARTS:
                def rotary(src_ps, dst, scale):
                    P.copy(tq[:], src_ps, eng='act')
                    P.stt(t1[0:32, :], tq[32:64, :], scale, sinb[32:64, :], ALU.mult, ALU.mult, eng='pool')
                    P.stt(t1[32:64, :], tq[0:32, :], scale, sinb[0:32, :], ALU.mult, ALU.mult, eng='pool')
                    P.stt(t2[:], tq[:], scale, cosb[:], ALU.mult, ALU.mult)
                    P.tt(dst[0:32, :], t2[0:32, :], t1[0:32, :], ALU.subtract)
                    P.tt(dst[32:64, :], t2[32:64, :], t1[32:64, :], ALU.add)
                proj(C_RQ, 128, pj[:, :]); rotary(pj[0:64, :], qr, 1.0); rotary(pj[64:128, :], kr, 0.125)
                P.tt(qd[:], qr[:], qdec4, ALU.mult, eng='pool')
                proj(C_RV, 128, pj[:, :]); P.copy(rv[:], pj[0:64, :], eng='act'); P.act(rg[:], pj[64:128, :], AF.Silu)
                def ret_gen():
                    for c in range(4):
                        c4 = slice(128 * c, 128 * c + 128)
                        P.mm(px0[:, 0:128], kr[:, c4], qr[:, c4])
                        P.tt(sc[:], px0[:, 0:128], decayT, ALU.mult)
                        P.mm(px1[:, 0:64], rv[:, c4], ident[0:64, 0:64])
                        P.copy(vtok[:], px1[:, 0:64], eng='act')
                        P.mm(px1[:, 64:128], kr[:, c4], ident[0:64, 0:64])
                        P.ts(ktok[:], px1[:, 64:128], kdec[:, 0:1], None, ALU.mult)
                        yield
                        P.mm(px0[0:64, 0:128], vtok[:], sc[:], start=True, stop=False)
                        P.mm(px0[0:64, 0:128], rst_bf[:], qd[:, c4], start=False, stop=True)
                        P.copy(ro[:, c4], px0[0:64, 0:128], eng='act')
                        P.mm(px1[0:64, 128:192], ktok[:], vtok[:])
                        P.stt(rst[:], rst[:], kdec[0:64, 1:2], px1[0:64, 128:192], ALU.mult, ALU.add)
                        P.copy(rst_bf[:], rst[:], eng='pool')
                        yield
                    ln_feat(P, A, ro[:], pn, ones64s[:], C(K_EPS5), t1[:], t2[:], sA[:])
                    P.stt(yout[2][:], ro[:], V(V_GN), rg[:], ALU.mult, ALU.mult)
                    P.dma(y_d[2, :, t0:t0 + 512], yout[2][:])
                gens.append(ret_gen())

            if 'sb' in B_PARTS:
                def qknorm(src, dst, gcol):
                    P.copy(sq_t[:], src, eng='act')
                    P.act(sA[:], sq_t[:], AF.Square)
                    P.mm(pn[0:64, :], ones64s[:], sA[:])
                    P.act(sq_r[:], pn[0:64, :], AF.Sqrt, bias=C(K_EPS6), scale=1.0)
                    P.add('dve', lambda e: e.reciprocal(sq_r[:], sq_r[:]), reads=[sq_r[:]], writes=[sq_r[:]])
                    P.stt(dst, sq_t[:], gcol, sq_r[:], ALU.mult, ALU.mult)
                qknorm(ro[:], qT[:], qn8[:, 0:1])
                proj(C_SK, 128, pj[:, :])
                P.copy(svT[:], pj[64:128, :], eng='act')
                qknorm(pj[0:64, :], kT_all[:, t0:t0 + 512], V(V_SBK))
                for i in range(4):
                    P.mm(px1[:, 0:64], svT[:, 128 * i:128 * i + 128], ident[0:64, 0:64])
                    P.copy(v_all[:, 4 * g + i, :], px1[:, 0:64], eng='act')
                P.memset(LS[:], 0.0); P.memset(LSbs[0][:], 0.0)
                def sb_gen():
                    js = list(range(4 * g + 3, -1, -1))

                    def geom(j):
                        diag = j >= 4 * g
                        c0 = (j - 4 * g) * 128 if diag else 0
                        return diag, c0, slice(c0, 512), kT_all[:, 128 * j:128 * j + 128]

                    def stage1(i):
                        j = js[i]
                        diag, c0, cols, kTj = geom(j)
                        pz_, e1_, spb_ = pzs[i % 2], e1s[i % 2], spbs[i % 2]
                        P.mm(pz_[:, cols], kTj, qT[:, cols])
                        P.act(e1_[:, cols], pz_[:, cols], AF.Exp)
                        P.act(spb_[:, cols], e1_[:, cols], AF.Ln, bias=cst[:, K_ONE:K_ONE + 1], scale=1.0)
                        if diag:
                            P.tt(spb_[:, c0:c0 + 128], spb_[:, c0:c0 + 128], mask_lt[:], ALU.mult, eng='pool')
                        if i + 1 < len(js):
                            ncols = geom(js[i + 1])[2]
                            P.tt(LS[:, cols], LS[:, cols], spb_[:, cols], ALU.add)
                            P.copy(LSbs[(i + 1) % 3][:, ncols], LS[:, ncols], eng='dve')

                    def stage2(i):
                        j = js[i]
                        diag, c0, cols, kTj = geom(j)
                        pla_, spb_, attn_, e1_ = plas[i % 2], spbs[i % 2], attns[i % 2], e1s[i % 2]
                        P.mm(pla_[:, cols], negrev[:], spb_[:, cols], start=True, stop=False)
                        P.mm(pla_[:, cols], negones[:], LSbs[i % 3][:, cols], start=False, stop=True)
                        P.act(ex2[:, cols], pla_[:, cols], AF.Exp)
                        P.tt(attn_[:, cols], e1_[:, cols], ex2[:, cols], ALU.mult)
                        if diag:
                            P.tt(attn_[:, c0:c0 + 128], attn_[:, c0:c0 + 128], mask_lt[:], ALU.mult, eng='pool')
                        P.mm(po[0:64, cols], v_all[:, j, :], attn_[:, cols], start=True, stop=True)
                        if diag:
                            P.copy(oacc[:, c0:c0 + 128], po[0:64, c0:c0 + 128], eng='dve')
                            if c0 + 128 < 512:
                                P.tt(oacc[:, c0 + 128:512], oacc[:, c0 + 128:512], po[0:64, c0 + 128:512], ALU.add)
                        else:
                            P.tt(oacc[:, :], oacc[:, :], po[0:64, :], ALU.add)

                    stage1(0)
                    for i in range(len(js)):
                        if i + 1 < len(js):
                            stage1(i + 1)
                        stage2(i)
                        yield
                    P.copy(yout[1][:], oacc[:], eng='act')
                    P.dma(y_d[1, :, t0:t0 + 512], yout[1][:])
                gens.append(sb_gen())

            while gens:
                for gen in list(gens):
                    try:
                        next(gen)
                    except StopIteration:
                        gens.remove(gen)
        P.emit()
        print("build_B: sbuf bytes/partition", A.bytes, "ops", P.nops)
    return nc


def _const_tables_B(S, h):
    idx = np.arange(128)
    tab = np.zeros((128, 6, 128), np.float32)
    tab[:, 0, :] = np.eye(128, dtype=np.float32)
    tab[:, 1, :] = (idx[:, None] < idx[None, :]).astype(np.float32)
    tab[:, 2, :] = -(idx[:, None] >= idx[None, :]).astype(np.float32)
    lg = np.log(np.float32(1.0) - np.float32(2.0) ** np.float32(-5.0 - h)).astype(np.float32)
    rel = (idx[None, :] - idx[:, None]).astype(np.float32)
    tab[:, 3, :] = np.where(rel >= 0, np.exp(np.maximum(rel, 0) * lg), 0.0).astype(np.float32)
    i64 = np.arange(64)
    t64 = np.zeros((64, 5, 512), np.float32)
    mU = (i64[:, None] < i64[None, :]).astype(np.float32)
    mL = (i64[None, :] < i64[:, None]).astype(np.float32)
    mUI = (i64[:, None] <= i64[None, :]).astype(np.float32)
    t64[:, 0, :] = np.tile(mU, (1, 8))
    t64[:, 1, :] = np.tile(mL, (1, 8))
    t64[:, 2, :] = np.tile(mUI, (1, 8))
    t64[:, 3, :] = np.tile(np.eye(64, dtype=np.float32), (1, 8))
    rs = np.ones(512, np.float32); rs[::64] = 0.0
    t64[:, 4, :] = rs[None, :]
    qdec = np.exp((idx.astype(np.float32) + 1.0) * lg).astype(np.float32)
    qdec4 = np.ascontiguousarray(np.tile(qdec[None, :], (64, 4)).astype(np.float32))
    kdec = np.zeros((128, 2), np.float32)
    kdec[:, 0] = np.exp((127.0 - idx.astype(np.float32)) * lg)
    kdec[:, 1] = np.exp(np.float32(128.0) * lg)
    inv_freq = (np.float32(10000.0) ** (-np.arange(0, 64, 2, dtype=np.float32) / np.float32(64))).astype(np.float32)
    ang = np.arange(S, dtype=np.float32)[:, None] * inv_freq[None, :]
    cosT = np.concatenate([np.cos(ang).T, np.cos(ang).T], axis=0).astype(np.float32)
    sinT = np.concatenate([np.sin(ang).T, np.sin(ang).T], axis=0).astype(np.float32)
    cst = np.zeros((128, 8), np.float32)
    for i, v in enumerate(CST):
        cst[:, i] = v
    return dict(tab=tab, tab64=t64.astype(ml_dtypes.bfloat16), qdec4=qdec4, kdec=kdec, cosT=np.ascontiguousarray(cosT), sinT=np.ascontiguousarray(sinT), cst=cst)


def prep_B(inp, l, b, h, xT_b, tables):
    hs = slice(64 * h, 64 * h + 64)
    w_in = inp["w_in"][l]
    RW, SB0, RT0 = 0, 1024, 1024 + 768
    cols = [w_in[:, RW + 0 + 64 * h: RW + 0 + 64 * h + 64], w_in[:, RW + 256 + 64 * h: RW + 256 + 64 * h + 64],
            w_in[:, RW + 512 + 64 * h: RW + 512 + 64 * h + 64], w_in[:, RW + 768: RW + 832], w_in[:, RW + 832: RW + 896],
            w_in[:, SB0 + 64 * h: SB0 + 64 * h + 64], w_in[:, RW + 896: RW + 1024],
            w_in[:, SB0 + 256 + 64 * h: SB0 + 256 + 64 * h + 64],
            w_in[:, SB0 + 512 + 64 * h: SB0 + 512 + 64 * h + 64],
            w_in[:, RT0 + 64 * h: RT0 + 64 * h + 64], w_in[:, RT0 + 256 + 64 * h: RT0 + 256 + 64 * h + 64],
            w_in[:, RT0 + 512 + 64 * h: RT0 + 512 + 64 * h + 64], w_in[:, RT0 + 768 + 64 * h: RT0 + 768 + 64 * h + 64]]
    wh = np.ascontiguousarray(np.concatenate(cols, axis=1))
    mu = inp["rw_mu"][l]
    vec = np.zeros((64, 16), np.float32)
    vec[:, V_MUR] = mu[0 + 64 * h: 64 * h + 64]
    vec[:, V_MUK] = mu[256 + 64 * h: 256 + 64 * h + 64]
    vec[:, V_MUV] = mu[512 + 64 * h: 512 + 64 * h + 64]
    vec[:, V_MUPW] = mu[768:832]
    vec[:, V_MUPA] = mu[832:896]
    vec[:, V_W0] = inp["rw_w0"][l][hs]
    vec[:, V_A0] = inp["rw_a0"][l][hs]
    vec[:, V_KK] = inp["rw_k_k"][l][hs]
    vec[:, V_KA] = inp["rw_k_a"][l][hs]
    vec[:, V_RK] = inp["rw_r_k"][l][h]
    vec[:, V_LNW] = inp["rw_ln_w"][l][hs]
    vec[:, V_LNB] = inp["rw_ln_b"][l][hs]
    vec[:, V_SBQ] = inp["sb_q_norm"][l]
    vec[:, V_SBK] = inp["sb_k_norm"][l]
    vec[:, V_GN] = inp["ret_gn"][l][hs]
    m = dict(xT=xT_b, gmix=np.ascontiguousarray(inp["norm_mix"][l].reshape(8, 128).T), wh=wh, vec64=vec,
             mupg=np.ascontiguousarray(mu[896:1024].reshape(128, 1)),
             w2h=np.ascontiguousarray(inp["rw_w2"][l][:, hs]), a2h=np.ascontiguousarray(inp["rw_a2"][l][:, hs]),
             g2h=np.ascontiguousarray(inp["rw_g2"][l][:, hs]))
    m.update(tables[h])
    return m


def load_cast(P, dst_bf, src_view, stg, engs=('dve', 'act')):
    C_, N_ = dst_bf.shape[1], dst_bf.shape[2]
    cap = stg.shape[1] // N_
    i = 0
    c = 0
    while c < C_:
        k = min(cap, C_ - c)
        sv = stg[:, 0:k * N_].rearrange("p (c n) -> p c n", c=k)
        P.dma(sv, src_view[:, c:c + k, :])
        P.copy(dst_bf[:, c:c + k, :], sv, eng=engs[i % len(engs)])
        c += k
        i += 1


def build_C1(NTOK):
    nc = bass.Bass("TRN2", target_bir_lowering=False)
    NT = NTOK // 512
    din = lambda n, s, d=F32: nc.dram_tensor(n, list(s), d, kind="ExternalInput").ap()
    xTh = din("xTh", [1024, 32 + NTOK])
    gmix_d = din("gmix", [128, 8])
    wconv_d = din("wconv", [1024, 512])
    cvec_d = din("cvec", [128, 2, 34])
    yT_d = din("yT", [3, 256, NTOK], BF16)
    wg_d = din("wg", [4, 1024, 1024])
    wb_d = din("wb", [4, 256, 1024])
    wout_d = din("wout", [1024, 1024])
    cst_d = din("cst", [128, 8])
    x1_d = nc.dram_tensor("x1T", [1024, NTOK], F32, kind="ExternalOutput").ap()
    with contextlib.ExitStack() as st:
        A = Alloc(nc, st); P = Prog(nc)
        gmix = A.sb("gmix_s", [128, 8]); P.dma(gmix[:], gmix_d)
        cvec = A.sb("cvec_s", [128, 2, 34]); P.dma(cvec[:], cvec_d)
        cst = A.sb("cst_s", [128, 8]); P.dma(cst[:], cst_d)
        ones_bf = A.sb("ones_bf", [128, 128], BF16); P.memset(ones_bf[:], 1.0)
        ones_s = A.sb("ones_s", [128, 128], BF16); P.memset(ones_s[:], 1.0 / 256)
        stg = A.sb("stg", [128, 4096])
        wg = A.sb("wg_b", [128, 32, 1024], BF16)
        wb = A.sb("wb_b", [128, 8, 1024], BF16)
        wout = A.sb("wout_b", [128, 8, 1024], BF16)
        wconv = A.sb("wconv_b", [128, 8, 512], BF16)
        load_cast(P, wconv[:], wconv_d.rearrange("(c p) n -> p c n", p=128), stg)
        load_cast(P, wg[:], wg_d.rearrange("i (c p) n -> p (i c) n", p=128), stg)
        load_cast(P, wb[:], wb_d.rearrange("i (c p) n -> p (i c) n", p=128), stg)
        load_cast(P, wout[:], wout_d.rearrange("(c p) n -> p c n", p=128), stg)
        xt = A.sb("xt", [128, 8, 512]); sq = A.sb("sq", [128, 8, 512], BF16); hT = A.sb("hT", [128, 8, 512], BF16)
        rstd = A.sb("rstd", [128, 512])
        xh = A.sb("xh", [128, 8, 32]); sqh = A.sb("sqh", [128, 8, 32], BF16); hTh = A.sb("hTh", [128, 8, 32], BF16)
        rstdh = A.sb("rstdh", [128, 32])
        ua = A.sb("ua", [128, 512]); sg = A.sb("sg", [128, 512])
        u = A.sb("u", [128, 2, 544])
        acc = A.sb("acc", [128, 2, 512]); accb = A.sb("accb", [128, 2, 512], BF16); tmp2 = A.sb("tmp2", [128, 512])
        ycT = A.sb("ycT", [128, 2, 512], BF16)
        ybr = A.sb("ybr", [128, 6, 512], BF16)
        merged = A.sb("merged", [128, 8, 512], BF16); macc = A.sb("macc", [128, 512]); mtmp = A.sb("mtmp", [128, 512])
        pj = [A.ps("pj%d" % i, [128, 512]) for i in range(2)]
        pg = [A.ps("pg%d" % i, [128, 512]) for i in range(2)]
        pb = [A.ps("pb%d" % i, [128, 512]) for i in range(2)]
        pn = A.ps("pn", [128, 512]); po = A.ps("po", [128, 512])
        xv = xTh.rearrange("(c p) t -> p c t", p=128)
        yv = yT_d.rearrange("i (c p) t -> p i c t", p=128)
        x1v = x1_d.rearrange("(c p) t -> p c t", p=128)

        def conv_u(hT_, N, ucol0):
            for cc in range(2):
                pa_, pb_ = pj[0], pj[1]
                for c in range(8):
                    P.mm(pa_[:, 0:N], wconv[:, c, 128 * cc:128 * cc + 128], hT_[:, c, :], start=(c == 0), stop=(c == 7))
                for c in range(8):
                    P.mm(pb_[:, 0:N], wconv[:, c, 256 + 128 * cc:256 + 128 * cc + 128], hT_[:, c, :], start=(c == 0), stop=(c == 7))
                P.act(sg[:, 0:N], pb_[:, 0:N], AF.Sigmoid)
                P.tt(u[:, cc, ucol0:ucol0 + N], sg[:, 0:N], pa_[:, 0:N], ALU.mult)

        P.dma(xh[:], xv[:, :, 0:32])
        rms_tile(P, xh[:], sqh[:], ones_bf[:], pn[:, 0:32], rstdh[:], cst, hTh, gmix)
        conv_u(hTh, 32, 0)
        for g in range(NT):
            t0 = g * 512
            P.dma(xt[:], xv[:, :, 32 + t0:32 + t0 + 512])
            for i in range(3):
                P.dma(ybr[:, 2 * i:2 * i + 2, :], yv[:, i, :, t0:t0 + 512])
            rms_tile(P, xt[:], sq[:], ones_bf[:], pn[:], rstd[:], cst, hT, gmix)
            if g > 0:
                P.copy(u[:, :, 0:32], u[:, :, 512:544], eng='pool')
            conv_u(hT, 512, 32)
            for cc in range(2):
                P.ts(acc[:, cc, :], u[:, cc, 2:514], cvec[:, cc, 0:1], cvec[:, cc, 31:32], ALU.mult, ALU.add)
                for j in range(1, 31):
                    P.stt(acc[:, cc, :], u[:, cc, 2 + j:514 + j], cvec[:, cc, j:j + 1], acc[:, cc, :], ALU.mult, ALU.add)
            P.copy(accb[:], acc[:], eng='pool')
            for cc in range(2):
                P.mm(pn[:], ones_s[:], accb[:, cc, :], start=(cc == 0), stop=(cc == 1))
            for cc in range(2):
                P.tt(acc[:, cc, :], acc[:, cc, :], pn[:], ALU.subtract)
            P.act(accb[:], acc[:], AF.Square)
            for cc in range(2):
                P.mm(pn[:], ones_s[:], accb[:, cc, :], start=(cc == 0), stop=(cc == 1))
            P.act(tmp2[:], pn[:], AF.Sqrt, bias=cst[:, K_EPS5:K_EPS5 + 1], scale=1.0)
            P.add('dve', lambda e: e.reciprocal(tmp2[:], tmp2[:]), reads=[tmp2[:]], writes=[tmp2[:]])
            for cc in range(2):
                P.tt(acc[:, cc, :], acc[:, cc, :], tmp2[:], ALU.mult)
                P.ts(acc[:, cc, :], acc[:, cc, :], cvec[:, cc, 32:33], cvec[:, cc, 33:34], ALU.mult, ALU.add)
                P.act(ycT[:, cc, :], acc[:, cc, :], AF.Silu)
            k = 0
            for m in range(8):
                mc = slice(128 * m, 128 * m + 128)
                for i in range(4):
                    pg_, pb_ = pg[k % 2], pb[k % 2]
                    k += 1
                    for c in range(8):
                        P.mm(pg_[:], wg[:, 8 * i + c, mc], hT[:, c, :], start=(c == 0), stop=(c == 7))
                    for c2 in range(2):
                        src = ybr[:, 2 * i + c2, :] if i < 3 else ycT[:, c2, :]
                        P.mm(pb_[:], wb[:, 2 * i + c2, mc], src, start=(c2 == 0), stop=(c2 == 1))
                    P.act(sg[:], pg_[:], AF.Sigmoid)
                    if i == 0:
                        P.tt(macc[:], sg[:], pb_[:], ALU.mult)
                    elif i < 3:
                        P.tt(mtmp[:], sg[:], pb_[:], ALU.mult)
                        P.tt(macc[:], macc[:], mtmp[:], ALU.add, eng='pool')
                    else:
                        P.tt(mtmp[:], sg[:], pb_[:], ALU.mult)
                        P.tt(merged[:, m, :], macc[:], mtmp[:], ALU.add, eng='pool')
            for m in range(8):
                mc = slice(128 * m, 128 * m + 128)
                for c in range(8):
                    P.mm(po[:], wout[:, c, mc], merged[:, c, :], start=(c == 0), stop=(c == 7))
                P.tt(xt[:, m, :], xt[:, m, :], po[:], ALU.add)
            P.dma(x1v[:, :, t0:t0 + 512], xt[:])
        P.emit()
        print("build_C1: sbuf bytes/partition", A.bytes, "ops", P.nops)
    return nc


def build_C2(NTOK, NF, moe):
    nc = bass.Bass("TRN2", target_bir_lowering=False)
    NT = NTOK // 512
    DFF = NF * 128
    din = lambda n, s, d=F32: nc.dram_tensor(n, list(s), d, kind="ExternalInput").ap()
    x1_d = din("x1T", [1024, NTOK])
    xa_d = din("xaT", [1024, NTOK])
    gffn_d = din("gffn", [128, 8])
    w1_d = din("w1", [1024, DFF]); w3_d = din("w3", [1024, DFF]); w2_d = din("w2", [DFF, 1024])
    cst_d = din("cst", [128, 8])
    if moe:
        rt_d = din("router", [1024, 8])
        esel_d = din("esel", [128, 8])
        idn_d = din("ident", [128, 128])
    x2_d = nc.dram_tensor("x2T", [1024, NTOK], F32, kind="ExternalOutput").ap()
    with contextlib.ExitStack() as st:
        A = Alloc(nc, st); P = Prog(nc)
        gffn = A.sb("gffn_s", [128, 8]); P.dma(gffn[:], gffn_d)
        cst = A.sb("cst_s", [128, 8]); P.dma(cst[:], cst_d)
        ones_bf = A.sb("ones_bf", [128, 128], BF16); P.memset(ones_bf[:], 1.0)
        stg = A.sb("stg", [128, 2816])
        w1 = A.sb("w1_b", [128, 8, DFF], BF16); w3 = A.sb("w3_b", [128, 8, DFF], BF16); w2 = A.sb("w2_b", [128, NF, 1024], BF16)
        load_cast(P, w1[:], w1_d.rearrange("(c p) n -> p c n", p=128), stg)
        load_cast(P, w3[:], w3_d.rearrange("(c p) n -> p c n", p=128), stg)
        load_cast(P, w2[:], w2_d.rearrange("(c p) n -> p c n", p=128), stg)
        xt = A.sb("xt", [128, 8, 512]); xa = A.sb("xa", [128, 8, 512]) if moe else xt
        sq = A.sb("sq", [128, 8, 512], BF16); hT = A.sb("hT", [128, 8, 512], BF16)
        rstd = A.sb("rstd", [128, 512])
        actT = A.sb("actT", [128, NF, 512], BF16)
        sil = [A.sb("sil%d" % i, [128, 512]) for i in range(2)]
        pa = [A.ps("pa%d" % i, [128, 512]) for i in range(2)]
        pb = [A.ps("pb%d" % i, [128, 512]) for i in range(2)]
        pn = A.ps("pn", [128, 512]); po = A.ps("po", [128, 512]); pr = A.ps("pr", [128, 512])
        if moe:
            rtf = A.sb("rtf", [128, 8, 8]); P.dma(rtf[:], rt_d.rearrange("(c p) n -> p c n", p=128))
            rtb = A.sb("rtb", [128, 8, 8], BF16); P.copy(rtb[:], rtf[:])
            esel = A.sb("esel_s", [128, 8]); P.dma(esel[:], esel_d)
            idf = A.sb("idf", [128, 128]); P.dma(idf[:], idn_d)
            idb = A.sb("idb", [128, 128], BF16); P.copy(idb[:], idf[:])
            lg = A.sb("lg", [128, 8]); mx = A.sb("mx", [128, 8]); ex = A.sb("ex", [128, 8]); msk = A.sb("msk", [128, 8])
            s1 = A.sb("s1", [128, 4]); wrep = A.sb("wrep", [128, 128], BF16); wbc = A.sb("wbc", [128, 512])
        x1v = x1_d.rearrange("(c p) t -> p c t", p=128)
        xav = xa_d.rearrange("(c p) t -> p c t", p=128)
        x2v = x2_d.rearrange("(c p) t -> p c t", p=128)
        for g in range(NT):
            t0 = g * 512
            P.dma(xt[:], x1v[:, :, t0:t0 + 512])
            if moe:
                P.dma(xa[:], xav[:, :, t0:t0 + 512])
            rms_tile(P, xt[:], sq[:], ones_bf[:], pn[:], rstd[:], cst, hT, gffn)
            if moe:
                for blk in range(4):
                    bc = slice(128 * blk, 128 * blk + 128)
                    for c in range(8):
                        P.mm(pr[:, 0:8], hT[:, c, bc], rtb[:, c, :], start=(c == 0), stop=(c == 7))
                    P.copy(lg[:], pr[:, 0:8], eng='act')
                    P.add('dve', lambda e: e.max(mx[:], lg[:]), reads=[lg[:]], writes=[mx[:]])
                    P.tt(s1[:, 0:1], mx[:, 1:2], mx[:, 0:1], ALU.subtract)
                    P.act(s1[:, 0:1], s1[:, 0:1], AF.Exp)
                    P.ts(s1[:, 0:1], s1[:, 0:1], 1.0, None, ALU.add)
                    P.add('dve', lambda e: e.reciprocal(s1[:, 1:2], s1[:, 0:1]), reads=[s1[:, 0:1]], writes=[s1[:, 1:2]])
                    P.ts(s1[:, 2:3], mx[:, 0:1], -1.0, None, ALU.mult)
                    P.act(ex[:], lg[:], AF.Exp, bias=s1[:, 2:3], scale=1.0)
                    P.ts(msk[:], lg[:], mx[:, 1:2], None, ALU.is_ge)
                    P.stt(ex[:], ex[:], s1[:, 1:2], msk[:], ALU.mult, ALU.mult)
                    P.tt(ex[:], ex[:], esel[:], ALU.mult)
                    P.reduce(s1[:, 3:4], ex[:], ALU.add)
                    P.copy(wrep[:], s1[:, 3:4].to_broadcast([128, 128]), eng='dve')
                    P.mm(pr[:, 128:256], wrep[:], idb[:])
                    P.copy(wbc[:, bc], pr[:, 128:256], eng='act')
            for f in range(NF):
                fc = slice(128 * f, 128 * f + 128)
                pa_, pb_ = pa[f % 2], pb[f % 2]
                for c in range(8):
                    P.mm(pa_[:], w1[:, c, fc], hT[:, c, :], start=(c == 0), stop=(c == 7))
                for c in range(8):
                    P.mm(pb_[:], w3[:, c, fc], hT[:, c, :], start=(c == 0), stop=(c == 7))
                P.act(sil[f % 2][:], pa_[:], AF.Silu)
                if moe:
                    P.tt(sil[f % 2][:], sil[f % 2][:], pb_[:], ALU.mult)
                    P.tt(actT[:, f, :], sil[f % 2][:], wbc[:], ALU.mult, eng='pool')
                else:
                    P.tt(actT[:, f, :], sil[f % 2][:], pb_[:], ALU.mult)
            for m in range(8):
                mc = slice(128 * m, 128 * m + 128)
                for f in range(NF):
                    P.mm(po[:], w2[:, f, mc], actT[:, f, :], start=(f == 0), stop=(f == NF - 1))
                P.tt(xa[:, m, :], xa[:, m, :], po[:], ALU.add)
            P.dma(x2v[:, :, t0:t0 + 512], xa[:])
        P.emit()
        print("build_C2: sbuf bytes/partition", A.bytes, "ops", P.nops)
    return nc


_NC_CACHE = {}


def _get(key, fn):
    if key not in _NC_CACHE:
        _NC_CACHE[key] = fn()
    return _NC_CACHE[key]


def kernel(**inp):
    inp = {k: np.asarray(v) for k, v in inp.items()}
    x = inp["x"]
    B, S, D = x.shape
    NTOK = B * S // 8
    SH = S // NTOK
    cstv = np.zeros((128, 8), np.float32)
    for i, v in enumerate(CST):
        cstv[:, i] = v
    tables = [_const_tables_B(S, h) for h in range(4)]
    xT = np.ascontiguousarray(np.transpose(x, (0, 2, 1)))
    L = inp["w_in"].shape[0]
    ident = np.eye(128, dtype=np.float32)
    for l in range(L):
        ncB = _get(("B", S), lambda: build_B(S))
        maps = [prep_B(inp, l, c // 4, c % 4, xT[c // 4], tables) for c in range(8)]
        res = run_bass_kernel_spmd(ncB, maps, core_ids=list(range(8)))
        yT = np.stack([np.asarray(res.results[c]["yT"]) for c in range(8)])
        yT = yT.reshape(B, 4, 3, 64, S).transpose(0, 2, 1, 3, 4).reshape(B, 3, 256, S)
        ncC1 = _get(("C1", NTOK), lambda: build_C1(NTOK))
        xTp = np.concatenate([np.zeros((B, D, 32), np.float32), xT], axis=2)
        cv = np.zeros((128, 2, 34), np.float32)
        for cc in range(2):
            sl = slice(128 * cc, 128 * cc + 128)
            cv[:, cc, 0:31] = inp["conv_dw"][l][:, sl].T
            cv[:, cc, 31] = inp["conv_b"][l][sl]
            cv[:, cc, 32] = inp["conv_ln_w"][l][sl]
            cv[:, cc, 33] = inp["conv_ln_b"][l][sl]
        gmix = np.ascontiguousarray(inp["norm_mix"][l].reshape(8, 128).T)
        wconv = np.ascontiguousarray(inp["w_in"][l][:, 2816:3328])
        maps = []
        for c in range(8):
            b, s0 = c // SH, (c % SH) * NTOK
            maps.append(dict(xTh=np.ascontiguousarray(xTp[b, :, s0:s0 + 32 + NTOK]), gmix=gmix, wconv=wconv, cvec=cv,
                             yT=np.ascontiguousarray(yT[b, :, :, s0:s0 + NTOK]), wg=inp["w_gate"][l], wb=inp["w_branch"][l],
                             wout=inp["w_out"][l], cst=cstv))
        res = run_bass_kernel_spmd(ncC1, maps, core_ids=list(range(8)))
        x1 = [np.asarray(res.results[c]["x1T"]) for c in range(8)]
        gffn = np.ascontiguousarray(inp["norm_ffn"][l].reshape(8, 128).T)
        if l % 2 == 0:
            ncC2 = _get(("C2", NTOK, 22, False), lambda: build_C2(NTOK, 22, False))
            j = l // 2
            maps = [dict(x1T=x1[c], xaT=x1[c], gffn=gffn, w1=inp["ffn_w1"][j], w3=inp["ffn_w3"][j], w2=inp["ffn_w2"][j], cst=cstv)
                    for c in range(8)]
            res = run_bass_kernel_spmd(ncC2, maps, core_ids=list(range(8)))
            x2 = [np.asarray(res.results[c]["x2T"]) for c in range(8)]
        else:
            ncC2 = _get(("C2moe", NTOK), lambda: build_C2moe(NTOK))
            j = l // 2
            maps = [dict(x1T=x1[c], gffn=gffn, w1=inp["moe_w1"][j], w3=inp["moe_w3"][j], w2=inp["moe_w2"][j],
                         cst=cstv, router=inp["router"][j], ident=ident) for c in range(8)]
            res = run_bass_kernel_spmd(ncC2, maps, core_ids=list(range(8)))
            x2 = [np.asarray(res.results[c]["x2T"]) for c in range(8)]
        xT = np.stack([np.concatenate(x2[b * SH:(b + 1) * SH], axis=1) for b in range(B)])
    return np.ascontiguousarray(np.transpose(xT, (0, 2, 1))).astype(np.float32)


def build_C2moe(NTOK, NE=8, NF=11):
    nc = bass.Bass("TRN2", target_bir_lowering=False)
    NT = NTOK // 512
    DFF = NF * 128
    din = lambda n, s, d=F32: nc.dram_tensor(n, list(s), d, kind="ExternalInput").ap()
    x1_d = din("x1T", [1024, NTOK])
    gffn_d = din("gffn", [128, 8])
    w1_d = din("w1", [NE, 1024, DFF]); w3_d = din("w3", [NE, 1024, DFF]); w2_d = din("w2", [NE, DFF, 1024])
    cst_d = din("cst", [128, 8])
    rt_d = din("router", [1024, 8])
    idn_d = din("ident", [128, 128])
    x2_d = nc.dram_tensor("x2T", [1024, NTOK], F32, kind="ExternalOutput").ap()
    with contextlib.ExitStack() as st:
        A = Alloc(nc, st); P = Prog(nc)
        gffn = A.sb("gffn_s", [128, 8]); P.dma(gffn[:], gffn_d)
        cst = A.sb("cst_s", [128, 8]); P.dma(cst[:], cst_d)
        ones_bf = A.sb("ones_bf", [128, 128], BF16); P.memset(ones_bf[:], 1.0)
        stg = A.sb("stg", [128, 2816])
        w1 = A.sb("w1_b", [128, 8, DFF], BF16); w3 = A.sb("w3_b", [128, 8, DFF], BF16); w2 = A.sb("w2_b", [128, NF, 1024], BF16)
        xt = A.sb("xt", [128, 8, 512])
        sq = A.sb("sq", [128, 2, 512], BF16)
        hT_all = A.sb("hT_all", [128, NT * 8, 512], BF16)
        rstd = A.sb("rstd", [128, 512])
        actT = A.sb("actT", [128, NF, 512], BF16)
        sil = [A.sb("sil%d" % i, [128, 512]) for i in range(2)]
        pa = [A.ps("pa%d" % i, [128, 512]) for i in range(2)]
        pb = [A.ps("pb%d" % i, [128, 512]) for i in range(2)]
        pn = A.ps("pn", [128, 512]); po = A.ps("po", [128, 512]); pr = A.ps("pr", [128, 512])
        rtf = A.sb("rtf", [128, 8, 8]); P.dma(rtf[:], rt_d.rearrange("(c p) n -> p c n", p=128))
        rtb = A.sb("rtb", [128, 8, 8], BF16); P.copy(rtb[:], rtf[:])
        idf = A.sb("idf", [128, 128]); P.dma(idf[:], idn_d)
        idb = A.sb("idb", [128, 128], BF16); P.copy(idb[:], idf[:])
        lg = A.sb("lg", [128, 8]); mx = A.sb("mx", [128, 8]); ex = A.sb("ex", [128, 8]); msk = A.sb("msk", [128, 8])
        s1 = A.sb("s1", [128, 4]); wrep = A.sb("wrep", [128, 128], BF16); wbc = A.sb("wbc", [128, 512])
        wgt_all = A.sb("wgt_all", [128, NT * 4, 8])
        x1v = x1_d.rearrange("(c p) t -> p c t", p=128)
        x2v = x2_d.rearrange("(c p) t -> p c t", p=128)

        class _HT:
            def __init__(self, g):
                self.g = g

            def __getitem__(self, key):
                p, c, t = key
                return hT_all[p, self.g * 8 + c, t]

        for e in range(NE):
            load_cast(P, w1[:], w1_d[e].rearrange("(c p) n -> p c n", p=128), stg)
            load_cast(P, w3[:], w3_d[e].rearrange("(c p) n -> p c n", p=128), stg)
            load_cast(P, w2[:], w2_d[e].rearrange("(c p) n -> p c n", p=128), stg)
            for g in range(NT):
                t0 = g * 512
                P.dma(xt[:], (x1v if e == 0 else x2v)[:, :, t0:t0 + 512])
                hT = _HT(g)
                if e == 0:
                    rms_tile(P, xt[:], sq[:], ones_bf[:], pn[:], rstd[:], cst, hT, gffn)
                    for blk in range(4):
                        bc = slice(128 * blk, 128 * blk + 128)
                        for c in range(8):
                            P.mm(pr[:, 0:8], hT[:, c, bc], rtb[:, c, :], start=(c == 0), stop=(c == 7))
                        P.copy(lg[:], pr[:, 0:8], eng='act')
                        P.add('dve', lambda e_: e_.max(mx[:], lg[:]), reads=[lg[:]], writes=[mx[:]])
                        P.tt(s1[:, 0:1], mx[:, 1:2], mx[:, 0:1], ALU.subtract)
                        P.act(s1[:, 0:1], s1[:, 0:1], AF.Exp)
                        P.ts(s1[:, 0:1], s1[:, 0:1], 1.0, None, ALU.add)
                        P.add('dve', lambda e_: e_.reciprocal(s1[:, 1:2], s1[:, 0:1]), reads=[s1[:, 0:1]], writes=[s1[:, 1:2]])
                        P.ts(s1[:, 2:3], mx[:, 0:1], -1.0, None, ALU.mult)
                        P.act(ex[:], lg[:], AF.Exp, bias=s1[:, 2:3], scale=1.0)
                        P.ts(msk[:], lg[:], mx[:, 1:2], None, ALU.is_ge)
                        P.stt(wgt_all[:, g * 4 + blk, :], ex[:], s1[:, 1:2], msk[:], ALU.mult, ALU.mult)
                for blk in range(4):
                    bc = slice(128 * blk, 128 * blk + 128)
                    P.copy(wrep[:], wgt_all[:, g * 4 + blk, e:e + 1].to_broadcast([128, 128]), eng='dve')
                    P.mm(pr[:, 128:256], wrep[:], idb[:])
                    P.copy(wbc[:, bc], pr[:, 128:256], eng='act')
                for f in range(NF):
                    fc = slice(128 * f, 128 * f + 128)
                    pa_, pb_ = pa[f % 2], pb[f % 2]
                    for c in range(8):
                        P.mm(pa_[:], w1[:, c, fc], hT[:, c, :], start=(c == 0), stop=(c == 7))
                    for c in range(8):
                        P.mm(pb_[:], w3[:, c, fc], hT[:, c, :], start=(c == 0), stop=(c == 7))
                    P.act(sil[f % 2][:], pa_[:], AF.Silu)
                    P.tt(sil[f % 2][:], sil[f % 2][:], pb_[:], ALU.mult)
                    P.tt(actT[:, f, :], sil[f % 2][:], wbc[:], ALU.mult, eng='pool')
                for m in range(8):
                    mc = slice(128 * m, 128 * m + 128)
                    for f in range(NF):
                        P.mm(po[:], w2[:, f, mc], actT[:, f, :], start=(f == 0), stop=(f == NF - 1))
                    P.tt(xt[:, m, :], xt[:, m, :], po[:], ALU.add)
                P.dma(x2v[:, :, t0:t0 + 512], xt[:])
        P.emit()
        print("build_C2moe: sbuf bytes/partition", A.bytes, "ops", P.nops)
    return nc
```

```python
import numpy as np
import concourse.bass as bass
import concourse.mybir as mybir

F32 = mybir.dt.float32
BF16 = mybir.dt.bfloat16
AF = mybir.ActivationFunctionType
ALU = mybir.AluOpType
AX = mybir.AxisListType

N_DMA_SEMS = 40
PSUM_NAMES = set()


def _region(ap):
    t = ap.tensor
    name = ap.name
    dims = ap.ap
    off = ap.offset
    space = str(ap.space)
    if 'DRAM' in space.upper() or 'HBM' in space.upper() or not hasattr(ap, 'base_partition') or len(dims) == 0:
        lo = off
        hi = off + sum((c - 1) * abs(s) for s, c in dims) + 1
        return (name, 0, 1, lo, hi)
    pstride = dims[0][0]
    pcount = dims[0][1]
    if pstride == 0:
        pstride = 1 << 30
    p0 = off // pstride if pstride < (1 << 30) else 0
    f0 = off - p0 * pstride if pstride < (1 << 30) else off
    f1 = f0 + sum((c - 1) * abs(s) for s, c in dims[1:]) + 1
    if name in PSUM_NAMES:
        return (name, (p0 // 32) * 32, ((p0 + pcount + 31) // 32) * 32, 0, 1 << 30)
    return (name, p0, p0 + pcount, f0, f1)


class Prog:
    def __init__(self, nc):
        self.nc = nc
        self.ops = {e: [] for e in ('pe', 'act', 'dve', 'pool', 'sp')}
        self.cnt = {e: 0 for e in ('pe', 'act', 'dve', 'pool')}
        self.recs = {}
        self.events = []
        self.known = {e: {} for e in self.ops}
        self.dma_uses = [0] * N_DMA_SEMS
        self.dma_next = 0
        self.nops = 0
        self.out_events = []

    def _is_dram(self, ap):
        s = str(ap.space).upper()
        return 'DRAM' in s or 'HBM' in s

    def add(self, eng, fn, reads=(), writes=(), dma=False):
        waits = {}

        def need(ev):
            semkey, val, _, _ = ev
            if waits.get(semkey, 0) < val:
                waits[semkey] = val

        rregs = [_region(a) for a in reads]
        wregs = [_region(a) for a in writes]
        idx = len(self.events)
        for r in rregs:
            for rec in self.recs.get(r[0], ()):
                if rec[5] != 'w':
                    continue
                if rec[1] < r[2] and r[1] < rec[2] and rec[3] < r[4] and r[3] < rec[4]:
                    ev = self.events[rec[6]]
                    if ev[2] == eng and not ev[3] and not dma and eng == 'pe':
                        continue
                    need(ev)
        for r in wregs:
            for rec in self.recs.get(r[0], ()):
                if rec[1] < r[2] and r[1] < rec[2] and rec[3] < r[4] and r[3] < rec[4]:
                    ev = self.events[rec[6]]
                    if ev[2] == eng and not ev[3] and not dma:
                        if eng == 'pe':
                            continue
                    need(ev)
        if dma:
            j = self.dma_next
            self.dma_next = (j + 1) % N_DMA_SEMS
            prev = self.dma_uses[j] * 16
            self.dma_uses[j] += 1
            val = prev + 16
            semkey = ('dma', j)
            if prev > 0:
                if waits.get(semkey, 0) < prev:
                    waits[semkey] = prev
            ev = (semkey, val, eng, True)
        else:
            self.cnt[eng] += 1
            ev = ((eng,), self.cnt[eng], eng, False)
        self.events.append(ev)
        kn = self.known[eng]
        wl = []
        for sk, v in waits.items():
            if kn.get(sk, 0) >= v:
                continue
            kn[sk] = v
            wl.append((sk, v))
        self.ops[eng].append((wl, fn, ev))
        evs = self.events
        for r in wregs:
            lst = self.recs.setdefault(r[0], [])
            lst[:] = [rec for rec in lst
                      if not (r[1] <= rec[1] and rec[2] <= r[2] and r[3] <= rec[3] and rec[4] <= r[4])]
            lst.append((r[0], r[1], r[2], r[3], r[4], 'w', idx))
        for r in rregs:
            lst = self.recs.setdefault(r[0], [])
            if not dma:
                lst[:] = [rec for rec in lst
                          if not (rec[5] == 'r' and evs[rec[6]][2] == eng and not evs[rec[6]][3]
                                  and r[1] <= rec[1] and rec[2] <= r[2] and r[3] <= rec[3] and rec[4] <= r[4])]
            lst.append((r[0], r[1], r[2], r[3], r[4], 'r', idx))
        self.nops += 1
        return ev

    def dma(self, out, in_, eng='sp', **kw):
        ev = self.add(eng, lambda e: e.dma_start(out=out, in_=in_, **kw), reads=[in_], writes=[out], dma=True)
        if self._is_dram(out):
            self.out_events.append(ev)
        return ev

    def mm(self, out, lhsT, rhs, start=True, stop=True, **kw):
        return self.add('pe', lambda e: e.matmul(out, lhsT, rhs, start=start, stop=stop, **kw),
                        reads=[lhsT, rhs], writes=[out])

    def transpose(self, out, in_, ident):
        return self.add('pe', lambda e: e.transpose(out, in_, ident), reads=[in_, ident], writes=[out])

    def act(self, out, in_, func, bias=None, scale=None, accum_out=None, eng='act'):
        kw = {}
        reads = [in_]
        writes = [out]
        if bias is not None:
            kw['bias'] = bias
            if not isinstance(bias, (int, float)):
                reads.append(bias)
        if scale is not None:
            kw['scale'] = scale
            if not isinstance(scale, (int, float)):
                reads.append(scale)
        if accum_out is not None:
            kw['accum_out'] = accum_out
            writes.append(accum_out)
        return self.add('act', lambda e: e.activation(out, in_, func, **kw), reads=reads, writes=writes)

    def tt(self, out, in0, in1, op, eng='dve'):
        return self.add(eng, lambda e: e.tensor_tensor(out, in0, in1, op), reads=[in0, in1], writes=[out])

    def ts(self, out, in0, s1, s2, op0, op1=None, eng='dve', accum_out=None):
        reads = [in0] + [s for s in (s1, s2) if s is not None and not isinstance(s, (int, float))]
        writes = [out] + ([accum_out] if accum_out is not None else [])
        kw = {}
        if op1 is not None:
            kw['op1'] = op1
        if accum_out is not None:
            kw['accum_out'] = accum_out
        return self.add(eng, lambda e: e.tensor_scalar(out, in0, s1, s2, op0, **kw), reads=reads, writes=writes)

    def stt(self, out, in0, scalar, in1, op0, op1, eng='dve', accum_out=None):
        reads = [in0, in1] + ([scalar] if not isinstance(scalar, (int, float)) else [])
        writes = [out] + ([accum_out] if accum_out is not None else [])
        kw = {}
        eng = 'dve'
        if accum_out is not None:
            kw['accum_out'] = accum_out
        return self.add(eng, lambda e: e.scalar_tensor_tensor(out, in0, scalar, in1, op0, op1, **kw),
                        reads=reads, writes=writes)

    def copy(self, out, in_, eng='dve'):
        if eng == 'act':
            return self.add('act', lambda e: e.copy(out, in_), reads=[in_], writes=[out])
        return self.add(eng, lambda e: e.tensor_copy(out, in_), reads=[in_], writes=[out])

    def memset(self, out, val, eng='pool'):
        return self.add(eng, lambda e: e.memset(out, val), reads=[], writes=[out])

    def reduce(self, out, in_, op, axis=AX.X, eng='dve'):
        return self.add(eng, lambda e: e.tensor_reduce(out, in_, axis, op), reads=[in_], writes=[out])

    def emit(self):
        nc = self.nc
        import contextlib
        with contextlib.ExitStack() as st:
            sems = {}
            for e in ('pe', 'act', 'dve', 'pool'):
                sems[(e,)] = st.enter_context(nc.semaphore("s_" + e))
            for j in range(N_DMA_SEMS):
                sems[('dma', j)] = st.enter_context(nc.semaphore("s_dma%d" % j))
            final = {}
            for ev in self.out_events:
                if final.get(ev[0], 0) < ev[1]:
                    final[ev[0]] = ev[1]
            for e in ('pe', 'act', 'dve', 'pool'):
                if self.cnt[e] > 0:
                    final[(e,)] = self.cnt[e]
            for j in range(N_DMA_SEMS):
                if self.dma_uses[j] > 0:
                    final[('dma', j)] = self.dma_uses[j] * 16
            block = st.enter_context(nc.Block())
            ops = self.ops

            def run(engname):
                def f(eng):
                    for wl, fn, ev in ops[engname]:
                        for sk, v in wl:
                            eng.wait_ge(sems[sk], v)
                        ins = fn(eng)
                        ins.then_inc(sems[ev[0]], 16 if ev[3] else 1)
                    if engname == 'sp':
                        for sk, v in final.items():
                            eng.wait_ge(sems[sk], v)
                return f

            block.sync(run('sp'))
            block.tensor(run('pe'))
            block.scalar(run('act'))
            block.vector(run('dve'))
            block.gpsimd(run('pool'))


import contextlib
import ml_dtypes
from concourse.bass_utils import run_bass_kernel_spmd

D_MODEL = 1024
NH = 4
HD = 64
RW_SCALE = 0.606531
C_R, C_K, C_V, C_PW, C_PA, C_SQ, C_PG, C_SK, C_SV, C_RQ, C_RK, C_RV, C_RG = \
    0, 64, 128, 192, 256, 320, 384, 512, 576, 640, 704, 768, 832
NCOL_B = 896
B_PARTS = ('rw', 'ret', 'sb')
RET_DBG = 3
(V_MUR, V_MUK, V_MUV, V_MUPW, V_MUPA, V_W0, V_A0, V_KK, V_KA, V_RK, V_LNW, V_LNB,
 V_SBQ, V_SBK, V_GN) = range(15)
CST = [1e-6, 64e-5, 1e-5, 1.0, 1e-12, 0.0]
K_EPS6, K_EPSRW, K_EPS5, K_ONE, K_TINY, K_ZERO = range(6)


class Alloc:
    def __init__(self, nc, st):
        self.nc = nc
        self.st = st
        self.bytes = 0

    def sb(self, name, shape, dt=F32):
        n = 1
        for s in shape[1:]:
            n *= s
        self.bytes += n * (4 if dt == F32 else 2)
        return self.st.enter_context(self.nc.sbuf_tensor(name, list(shape), dt))

    def ps(self, name, shape, dt=F32):
        PSUM_NAMES.add(name)
        return self.st.enter_context(self.nc.psum_tensor(name, list(shape), dt))


def rms_tile(P, xt, sq, ones_bf, pn, rstd, cst, hT, gain, tmpf=None):
    nsq = sq.shape[1]
    if nsq == 8:
        P.act(sq, xt, AF.Square)
    for c in range(8):
        if nsq < 8:
            P.act(sq[:, c % nsq, :], xt[:, c, :], AF.Square)
        P.mm(pn, ones_bf, sq[:, c % nsq, :], start=(c == 0), stop=(c == 7))
    P.act(rstd, pn, AF.Sqrt, scale=1.0 / D_MODEL, bias=cst[:, K_EPS6:K_EPS6 + 1])
    P.add('dve', lambda e: e.reciprocal(rstd, rstd), reads=[rstd], writes=[rstd])
    for c in range(8):
        P.stt(hT[:, c, :], xt[:, c, :], gain[:, c:c + 1], rstd, ALU.mult, ALU.mult,
              eng=('dve' if c % 2 == 0 else 'pool'))


def ln_feat(P, A, y, pn, ones_s, eps_col, tmp1, tmp2, sbf, npart=64, N=512):
    P.copy(sbf, y, eng='pool')
    P.mm(pn[0:npart, 0:N], ones_s, sbf, start=True, stop=True)
    P.tt(y, y, pn[0:npart, 0:N], ALU.subtract)
    P.act(sbf, y, AF.Square)
    P.mm(pn[0:npart, 0:N], ones_s, sbf, start=True, stop=True)
    P.act(tmp2, pn[0:npart, 0:N], AF.Sqrt, bias=eps_col, scale=1.0)
    P.add('dve', lambda e: e.reciprocal(tmp2, tmp2), reads=[tmp2], writes=[tmp2])
    P.tt(y, y, tmp2, ALU.mult)


def build_B(S):
    nc = bass.Bass("TRN2", target_bir_lowering=False)
    NT = S // 512
    NB = S // 128
    din = lambda n, s, d=F32: nc.dram_tensor(n, list(s), d, kind="ExternalInput").ap()
    xT = din("xT", [1024, S])
    gmix_d = din("gmix", [128, 8])
    wh_d = din("wh", [1024, NCOL_B])
    vec64_d = din("vec64", [64, 16])
    mupg_d = din("mupg", [128, 1])
    w2_d = din("w2h", [64, 64])
    a2_d = din("a2h", [64, 64])
    g2_d = din("g2h", [128, 64])
    cos_d = din("cosT", [64, S])
    sin_d = din("sinT", [64, S])
    cst_d = din("cst", [128, 8])
    tab_d = din("tab", [128, 6, 128])
    tab64_d = din("tab64", [64, 5, 512], BF16)
    qdec_d = din("qdec4", [64, 512])
    kdec_d = din("kdec", [128, 2])
    y_d = nc.dram_tensor("yT", [3, 64, S], BF16, kind="ExternalOutput").ap()

    with contextlib.ExitStack() as st:
        A = Alloc(nc, st)
        P = Prog(nc)
        gmix = A.sb("gmix_s", [128, 8]); P.dma(gmix[:], gmix_d)
        vec = A.sb("vec_s", [64, 16]); P.dma(vec[:], vec64_d)
        mupg = A.sb("mupg_s", [128, 1]); P.dma(mupg[:], mupg_d)
        w2f = A.sb("w2_s", [64, 64]); P.dma(w2f[:], w2_d)
        a2f = A.sb("a2_s", [64, 64]); P.dma(a2f[:], a2_d)
        g2f = A.sb("g2_s", [128, 64]); P.dma(g2f[:], g2_d)
        w2 = A.sb("w2_b", [64, 64], BF16); P.copy(w2[:], w2f[:])
        a2 = A.sb("a2_b", [64, 64], BF16); P.copy(a2[:], a2f[:])
        g2 = A.sb("g2_b", [128, 64], BF16); P.copy(g2[:], g2f[:])
        cst = A.sb("cst_s", [128, 8]); P.dma(cst[:], cst_d)
        tab = A.sb("tab_s", [128, 6, 128]); P.dma(tab[:], tab_d)
        tab64 = A.sb("tab64_s", [64, 5, 512], BF16); P.dma(tab64[:], tab64_d)
        qdec4t = A.sb("qdec4_s", [64, 512]); P.dma(qdec4t[:], qdec_d)
        kdec = A.sb("kdec_s", [128, 2]); P.dma(kdec[:], kdec_d)
        ident = A.sb("identb", [128, 128], BF16); P.copy(ident[:], tab[:, 0, :])
        ident = ident[:]
        mask_lt = A.sb("mask_lt", [128, 128], BF16); P.copy(mask_lt[:], tab[:, 1, :])
        negrev = A.sb("negrev", [128, 128], BF16); P.copy(negrev[:], tab[:, 2, :])
        decayT = tab[:, 3, :]
        maskU8, maskL8, maskUI8, I8, rstm = (tab64[:, i, :] for i in range(5))
        qdec4 = qdec4t[:]
        ones_bf = A.sb("ones_bf", [128, 128], BF16); P.memset(ones_bf[:], 1.0)
        negones = A.sb("negones", [128, 128], BF16); P.memset(negones[:], -1.0)
        ones64 = A.sb("ones64", [64, 64], BF16); P.memset(ones64[:], 1.0)
        ones64s = A.sb("ones64s", [64, 64], BF16); P.memset(ones64s[:], 1.0 / 64)
        qn8 = A.sb("qn8", [64, 1]); P.ts(qn8[:], vec[:, V_SBQ:V_SBQ + 1], 0.125, None, ALU.mult)
        V = lambda i: vec[:, i:i + 1]
        C = lambda i, n=64: cst[0:n, i:i + 1]

        xts = [A.sb("xt0", [128, 8, 512])] * 2
        wbf = A.sb("wbf", [128, 8, NCOL_B], BF16)
        sq = A.sb("sq", [128, 2, 512], BF16)
        hT = A.sb("hT", [128, 8, 512], BF16)
        rstd = A.sb("rstd", [128, 512])
        kT_all = A.sb("kT_all", [64, S], BF16)
        v_all = A.sb("v_all", [128, NB, 64], BF16)
        pj = A.ps("pj", [128, 512]); pn = A.ps("pn", [128, 512])
        px0 = A.ps("px0", [128, 512]); px1 = A.ps("px1", [128, 512]); py = A.ps("py", [128, 512])
        pz = A.ps("pz", [128, 512]); pla = A.ps("pla", [128, 512]); po = A.ps("po", [128, 512])
        pzs = [pz, pj]; plas = [pla, py]

        whv = wh_d.rearrange("(c p) n -> p c n", p=128)
        for half in range(2):
            stg = xts[half][:].rearrange("p c t -> p (c t)")[:, 0:4 * NCOL_B].rearrange("p (c n) -> p c n", c=4)
            P.dma(stg, whv[:, 4 * half:4 * half + 4, :])
            P.copy(wbf[:, 4 * half:4 * half + 4, :], stg, eng=('dve' if half == 0 else 'act'))

        names = ["raw_r", "raw_k", "raw_v", "raw_pw", "raw_pa"]
        raws = [A.sb(n, [64, 513]) for n in names]
        rawg = A.sb("raw_pg", [128, 513])
        for r_ in raws:
            P.memset(r_[:, 0:1], 0.0)
            P.memset(r_[:, 512:513], 0.0)
        P.memset(rawg[:, 0:1], 0.0); P.memset(rawg[:, 512:513], 0.0)
        e1 = A.sb("sb_e1", [128, 512]); spb = A.sb("sb_spb", [128, 512], BF16)
        LS = A.sb("sb_LS", [128, 512])
        T = {}
        for n in ["r", "k", "v", "pw", "pa", "d", "logw", "lp", "ep", "em", "epm", "a", "g", "kk", "t1",
                  "kp", "al", "be", "kt", "rb", "bon", "ysb", "tm1", "tm2"]:
            T[n] = A.sb("rw_" + n, [64, 512], BF16 if n in ("al", "be", "kt", "rb") else F32)
        sA = A.sb("rw_sA", [64, 512], BF16); sB = A.sb("rw_sB", [64, 512], BF16); vb = A.sb("rw_vb", [64, 512], BF16)
        dgb = spb
        pgm = LS; dg = e1
        M_ = [A.sb("rw_M%d" % i, [64, 8, 64], BF16) for i in range(2)]
        L_ = [A.sb("rw_L%d" % i, [64, 8, 64], BF16) for i in range(2)]
        Pt = A.sb("rw_Pt", [64, 8, 64], BF16); Pt32 = A.sb("rw_Pt32", [64, 8, 64])
        AakT = A.sb("rw_AakT", [64, 8, 64], BF16); ArbT = A.sb("rw_ArbT", [64, 8, 64], BF16)
        ArkT = A.sb("rw_ArkT", [64, 8, 64], BF16); Vt = A.sb("rw_Vt", [64, 8, 64], BF16); Bt = A.sb("rw_Bt", [64, 8, 64], BF16)
        Kt = A.sb("rw_Kt", [64, 8, 64], BF16); Us = A.sb("rw_Us", [64, 8, 64], BF16); Xs = A.sb("rw_Xs", [64, 64], BF16)
        H32 = A.sb("rw_H32", [64, 64]); H = A.sb("rw_H", [64, 64], BF16); Hd = A.sb("rw_Hd", [64, 64])
        P.memset(H32[:], 0.0); P.memset(H[:], 0.0)
        yout = [A.sb("yout%d" % i, [64, 512], BF16) for i in range(3)]
        cosb = A.sb("cosb", [64, 512]); sinb = A.sb("sinb", [64, 512])
        tq = T["d"]; t1 = T["tm1"]; t2 = T["tm2"]
        qr = A.sb("rt_qr", [64, 512], BF16); kr = A.sb("rt_kr", [64, 512], BF16); qd = A.sb("rt_qd", [64, 512], BF16)
        rv = A.sb("rt_rvb", [64, 512], BF16); rg = T["pa"]
        sc = A.sb("rt_sc", [128, 128], BF16); vtok = A.sb("rt_vtok", [128, 64], BF16); ktok = A.sb("rt_ktok", [128, 64], BF16)
        rst = A.sb("rt_st", [64, 64]); rst_bf = A.sb("rt_stbf", [64, 64], BF16)
        P.memset(rst[:], 0.0); P.memset(rst_bf[:], 0.0)
        ro = A.sb("rt_ro", [64, 512])
        sq_t = T["logw"]; sq_s = T["lp"]; sq_r = T["em"]
        qT = A.sb("sb_qT", [64, 512], BF16); svT = sB
        attn = A.sb("sb_attn", [128, 512], BF16)
        oacc = A.sb("sb_oacc", [64, 512])
        e1s = [e1, A.sb("sb_e1b", [128, 512])]; spbs = [spb, A.sb("sb_spb2", [128, 512], BF16)]
        attns = [attn, A.sb("sb_attn2", [128, 512], BF16)]
        ex2 = A.sb("sb_ex2", [128, 512], BF16)
        LSbs = [A.sb("sb_LSb%d" % i, [128, 512], BF16) for i in range(3)]

        xTv = xT.rearrange("(c p) t -> p c t", p=128)

        def proj(col0, M, ps):
            for c in range(8):
                P.mm(ps, wbf[:, c, col0:col0 + M], hT[:, c, :], start=(c == 0), stop=(c == 7))

        for g in range(NT):
            t0 = g * 512
            gens = []
            xt = xts[g % 2]
            P.dma(xt[:], xTv[:, :, t0:t0 + 512])
            P.dma(cosb[:], cos_d[:, t0:t0 + 512])
            P.dma(sinb[:], sin_d[:, t0:t0 + 512])
            rms_tile(P, xt[:], sq[:], ones_bf[:], pj[:], rstd[:], cst, hT, gmix)

            if 'rw' in B_PARTS:
                mixed = [T["r"], T["k"], T["v"], T["pw"], T["pa"]]
                mus = [V_MUR, V_MUK, V_MUV, V_MUPW, V_MUPA]

                def shift_mix(i, src_ps):
                    raw = raws[i]
                    if g > 0:
                        P.copy(raw[:, 0:1], raw[:, 512:513], eng='pool')
                    P.copy(raw[:, 1:513], src_ps, eng='act')
                proj(C_R, 128, pj[:, :]); shift_mix(0, pj[0:64, :]); shift_mix(1, pj[64:128, :])
                proj(C_V, 128, pj[:, :]); shift_mix(2, pj[0:64, :]); shift_mix(3, pj[64:128, :])
                proj(C_PA, 128, pj[:, :]); shift_mix(4, pj[0:64, :])
                P.copy(ro[:], pj[64:128, :], eng='act')
                if g > 0:
                    P.copy(rawg[:, 0:1], rawg[:, 512:513], eng='pool')
                proj(C_PG, 128, pj[:, :])
                P.copy(rawg[:, 1:513], pj[:, :], eng='act')

                for i_ in range(5):
                    P.tt(T["d"][:], raws[i_][:, 0:512], raws[i_][:, 1:513], ALU.subtract, eng='pool')
                    P.stt(mixed[i_][:], T["d"][:], V(mus[i_]), raws[i_][:, 1:513], ALU.mult, ALU.add)
                P.tt(dg[:], rawg[:, 0:512], rawg[:, 1:513], ALU.subtract, eng='pool')
                P.stt(pgm[:], dg[:], mupg[:, 0:1], rawg[:, 1:513], ALU.mult, ALU.add)
                P.copy(sB[:], T["pa"][:], eng='pool')
                P.mm(pn[0:64, :], a2[:], sB[:])
                P.act(T["a"][:], pn[0:64, :], AF.Sigmoid, bias=V(V_A0))
                P.act(dgb[:], pgm[:], AF.Sigmoid)
                P.mm(pn[0:64, :], g2[:], dgb[:])
                P.copy(T["g"][:], pn[0:64, :], eng='act')

                def rw_gen():
                    ysb = T["ysb"]
                    r_, k_, v_ = T["r"], T["k"], T["v"]
                    P.act(sA[:], T["pw"][:], AF.Tanh)
                    P.mm(pn[0:64, :], w2[:], sA[:])
                    P.act(T["logw"][:], pn[0:64, :], AF.Sigmoid, bias=V(V_W0))
                    P.ts(T["logw"][:], T["logw"][:], -RW_SCALE, None, ALU.mult)
                    P.add('dve', lambda e: e.tensor_tensor_scan(T["lp"][:], rstm, T["logw"][:], 0.0, ALU.mult, ALU.add),
                          reads=[rstm, T["logw"][:]], writes=[T["lp"][:]])
                    P.act(T["ep"][:], T["lp"][:], AF.Exp)
                    P.act(T["em"][:], T["lp"][:], AF.Exp, scale=-1.0)
                    P.tt(T["tm1"][:], T["lp"][:], T["logw"][:], ALU.subtract, eng='pool')
                    P.act(T["epm"][:], T["tm1"][:], AF.Exp)
                    yield
                    P.ts(T["kk"][:], k_[:], V(V_KK), None, ALU.mult)
                    P.act(sA[:], T["kk"][:], AF.Square)
                    P.mm(pn[0:64, :], ones64[:], sA[:])
                    P.act(T["tm2"][:], pn[0:64, :], AF.Sqrt)
                    P.ts(T["tm2"][:], T["tm2"][:], 1e-12, None, ALU.max)
                    P.add('dve', lambda e: e.reciprocal(T["tm2"][:], T["tm2"][:]), reads=[T["tm2"][:]], writes=[T["tm2"][:]])
                    P.tt(T["kk"][:], T["kk"][:], T["tm2"][:], ALU.mult)
                    yield
                    P.ts(T["t1"][:], T["a"][:], 1.0, V(V_KA), ALU.subtract, ALU.mult)
                    P.stt(T["kp"][:], T["t1"][:], 1.0, k_[:], ALU.add, ALU.mult)
                    yield
                    P.stt(T["al"][:], T["kk"][:], -1.0, T["epm"][:], ALU.mult, ALU.mult)
                    P.tt(T["t1"][:], T["kk"][:], T["a"][:], ALU.mult, eng='pool')
                    P.tt(T["be"][:], T["t1"][:], T["em"][:], ALU.mult, eng='pool')
                    P.tt(T["kt"][:], T["kp"][:], T["em"][:], ALU.mult)
                    P.tt(T["rb"][:], r_[:], T["ep"][:], ALU.mult, eng='pool')
                    yield
                    P.stt(sB[:], r_[:], V(V_RK), T["kp"][:], ALU.mult, ALU.mult)
                    P.mm(pn[0:64, :], ones64[:], sB[:])
                    P.copy(vb[:], v_[:], eng='pool')
                    P.tt(T["bon"][:], pn[0:64, :], v_[:], ALU.mult)
                    yield

                    al, be, kt, rb = T["al"], T["be"], T["kt"], T["rb"]
                    X0 = px0[0:64, :].rearrange("p (c n) -> p c n", c=8)
                    X1 = px1[0:64, :].rearrange("p (c n) -> p c n", c=8)
                    X2 = pn[0:64, :].rearrange("p (c n) -> p c n", c=8)
                    f3 = lambda ap: ap
                    fl = lambda t3: t3.rearrange("p c n -> p (c n)")
                    cs = lambda c: slice(64 * c, 64 * c + 64)
                    for c in range(8):
                        P.mm(X0[:, c, :], be[:, cs(c)], al[:, cs(c)])
                        P.mm(X1[:, c, :], al[:, cs(c)], be[:, cs(c)])
                    P.tt(fl(Pt32[:]), px0[0:64, :], maskU8, ALU.mult)
                    P.copy(fl(M_[0][:]), fl(Pt32[:]), eng='pool')
                    P.tt(fl(L_[0][:]), px1[0:64, :], maskL8, ALU.mult, eng='pool' if False else 'dve')
                    yield
                    for c in range(8):
                        P.mm(X2[:, c, :], kt[:, cs(c)], al[:, cs(c)])
                    P.tt(fl(AakT[:]), pn[0:64, :], maskU8, ALU.mult)
                    yield
                    for c in range(8):
                        P.mm(X0[:, c, :], be[:, cs(c)], rb[:, cs(c)])
                        P.mm(X1[:, c, :], kt[:, cs(c)], rb[:, cs(c)])
                    P.tt(fl(ArbT[:]), px0[0:64, :], maskUI8, ALU.mult)
                    P.tt(fl(ArkT[:]), px1[0:64, :], maskUI8, ALU.mult)
                    yield
                    for c in range(8):
                        P.mm(X2[:, c, :], vb[:, cs(c)], ident[0:64, 0:64])
                        P.mm(X0[:, c, :], be[:, cs(c)], ident[0:64, 0:64])
                        P.mm(X1[:, c, :], kt[:, cs(c)], ident[0:64, 0:64])
                    P.copy(fl(Vt[:]), pn[0:64, :], eng='act')
                    P.copy(fl(Bt[:]), px0[0:64, :], eng='dve')
                    P.copy(fl(Kt[:]), px1[0:64, :], eng='act')
                    yield
                    P.tt(fl(Pt32[:]), fl(Pt32[:]), I8, ALU.add)
                    P.copy(fl(Pt[:]), fl(Pt32[:]), eng='pool')
                    yield
                    for n in range(1, 6):
                        Mp, Lp = M_[(n - 1) % 2], L_[(n - 1) % 2]
                        Mn, Ln = M_[n % 2], L_[n % 2]
                        for c in range(8):
                            if n < 5:
                                P.mm(X0[:, c, :], Lp[:, c, :], Mp[:, c, :])
                            P.mm(X1[:, c, :], Mp[:, c, :], Lp[:, c, :])
                        if n < 5:
                            P.copy(fl(Mn[:]), px0[0:64, :], eng='act')
                        P.copy(fl(Ln[:]), px1[0:64, :], eng='dve')
                        for c in range(8):
                            P.mm(X2[:, c, :], Ln[:, c, :], Pt[:, c, :])
                        P.tt(fl(Pt32[:]), fl(Pt32[:]), pn[0:64, :], ALU.add)
                        P.copy(fl(Pt[:]), fl(Pt32[:]), eng='pool')
                        yield
                    for c in range(8):
                        epC = T["ep"][:, 64 * c + 63:64 * c + 64]
                        P.mm(X0[:, c, :], al[:, cs(c)], H[:], start=True, stop=False)
                        P.mm(X0[:, c, :], AakT[:, c, :], Vt[:, c, :], start=False, stop=True)
                        P.copy(Xs[:], X0[:, c, :], eng='act')
                        P.mm(X1[:, c, :], Pt[:, c, :], Xs[:])
                        P.copy(Us[:, c, :], X1[:, c, :], eng='dve')
                        yield
                        P.mm(X0[:, c, :], H[:], rb[:, cs(c)], start=True, stop=False)
                        P.mm(X0[:, c, :], Us[:, c, :], ArbT[:, c, :], start=False, stop=False)
                        P.mm(X0[:, c, :], Vt[:, c, :], ArkT[:, c, :], start=False, stop=True)
                        P.copy(ysb[:, cs(c)], X0[:, c, :], eng='act')
                        P.ts(Hd[:], H32[:], epC, None, ALU.mult, eng='pool')
                        P.mm(X2[:, c, :], Bt[:, c, :], Us[:, c, :], start=True, stop=False)
                        P.mm(X2[:, c, :], Kt[:, c, :], Vt[:, c, :], start=False, stop=True)
                        P.stt(H32[:], X2[:, c, :], epC, Hd[:], ALU.mult, ALU.add)
                        P.copy(H[:], H32[:], eng='pool')
                        yield
                    ln_feat(P, A, ysb[:], pn, ones64s[:], C(K_EPSRW), T["tm1"][:], T["tm2"][:], sA[:])
                    P.ts(ysb[:], ysb[:], V(V_LNW), V(V_LNB), ALU.mult, ALU.add)
                    P.tt(ysb[:], ysb[:], T["bon"][:], ALU.add)
                    P.tt(yout[0][:], ysb[:], T["g"][:], ALU.mult)
                    P.dma(y_d[0, :, t0:t0 + 512], yout[0][:])
                gens.append(rw_gen())

            if 'ret' in B_PARTS:
                def rotary(src_ps, dst, scale):
                    P.copy(tq[:], src_ps, eng='act')
                    P.stt(t1[0:32, :], tq[32:64, :], scale, sinb[32:64, :], ALU.mult, ALU.mult, eng='pool')
                    P.stt(t1[32:64, :], tq[0:32, :], scale, sinb[0:32, :], ALU.mult, ALU.mult, eng='pool')
                    P.stt(t2[:], tq[:], scale, cosb[:], ALU.mult, ALU.mult)
                    P.tt(dst[0:32, :], t2[0:32, :], t1[0:32, :], ALU.subtract)
                    P.tt(dst[32:64, :], t2[32:64, :], t1[32:64, :], ALU.add)
                proj(C_RQ, 128, pj[:, :]); rotary(pj[0:64, :], qr, 1.0); rotary(pj[64:128, :], kr, 0.125)
                P.tt(qd[:], qr[:], qdec4, ALU.mult, eng='pool')
                proj(C_RV, 128, pj[:, :]); P.copy(rv[:], pj[0:64, :], eng='act'); P.act(rg[:], pj[64:128, :], AF.Silu)
                def ret_gen():
                    for c in range(4):
                        c4 = slice(128 * c, 128 * c + 128)
                        P.mm(px0[:, 0:128], kr[:, c4], qr[:, c4])
                        P.tt(sc[:], px0[:, 0:128], decayT, ALU.mult)
                        P.mm(px1[:, 0:64], rv[:, c4], ident[0:64, 0:64])
                        P.copy(vtok[:], px1[:, 0:64], eng='act')
                        P.mm(px1[:, 64:128], kr[:, c4], ident[0:64, 0:64])
                        P.ts(ktok[:], px1[:, 64:128], kdec[:, 0:1], None, ALU.mult)
                        yield
                        P.mm(px0[0:64, 0:128], vtok[:], sc[:], start=True, stop=False)
                        P.mm(px0[0:64, 0:128], rst_bf[:], qd[:, c4], start=False, stop=True)
                        P.copy(ro[:, c4], px0[0:64, 0:128], eng='act')
                        P.mm(px1[0:64, 128:192], ktok[:], vtok[:])
                        P.stt(rst[:], rst[:], kdec[0:64, 1:2], px1[0:64, 128:192], ALU.mult, ALU.add)
                        P.copy(rst_bf[:], rst[:], eng='pool')
                        yield
                    ln_feat(P, A, ro[:], pn, ones64s[:], C(K_EPS5), t1[:], t2[:], sA[:])
                    P.stt(yout[2][:], ro[:], V(V_GN), rg[:], ALU.mult, ALU.mult)
                    P.dma(y_d[2, :, t0:t0 + 512], yout[2][:])
                gens.append(ret_gen())

            if 'sb' in B_PARTS:
                def qknorm(src, dst, gcol):
                    P.copy(sq_t[:], src, eng='act')
                    P.act(sA[:], sq_t[:], AF.Square)
                    P.mm(pn[0:64, :], ones64s[:], sA[:])
                    P.act(sq_r[:], pn[0:64, :], AF.Sqrt, bias=C(K_EPS6), scale=1.0)
                    P.add('dve', lambda e: e.reciprocal(sq_r[:], sq_r[:]), reads=[sq_r[:]], writes=[sq_r[:]])
                    P.stt(dst, sq_t[:], gcol, sq_r[:], ALU.mult, ALU.mult)
                qknorm(ro[:], qT[:], qn8[:, 0:1])
                proj(C_SK, 128, pj[:, :])
                P.copy(svT[:], pj[64:128, :], eng='act')
                qknorm(pj[0:64, :], kT_all[:, t0:t0 + 512], V(V_SBK))
                for i in range(4):
                    P.mm(px1[:, 0:64], svT[:, 128 * i:128 * i + 128], ident[0:64, 0:64])
                    P.copy(v_all[:, 4 * g + i, :], px1[:, 0:64], eng='act')
                P.memset(LS[:], 0.0); P.memset(LSbs[0][:], 0.0)
                def sb_gen():
                    js = list(range(4 * g + 3, -1, -1))

                    def geom(j):
                        diag = j >= 4 * g
                        c0 = (j - 4 * g) * 128 if diag else 0
                        return diag, c0, slice(c0, 512), kT_all[:, 128 * j:128 * j + 128]

                    def stage1(i):
                        j = js[i]
                        diag, c0, cols, kTj = geom(j)
                        pz_, e1_, spb_ = pzs[i % 2], e1s[i % 2], spbs[i % 2]
                        P.mm(pz_[:, cols], kTj, qT[:, cols])
                        P.act(e1_[:, cols], pz_[:, cols], AF.Exp)
                        P.act(spb_[:, cols], e1_[:, cols], AF.Ln, bias=cst[:, K_ONE:K_ONE + 1], scale=1.0)
                        if diag:
                            P.tt(spb_[:, c0:c0 + 128], spb_[:, c0:c0 + 128], mask_lt[:], ALU.mult, eng='pool')
                        if i + 1 < len(js):
                            ncols = geom(js[i + 1])[2]
                            P.tt(LS[:, cols], LS[:, cols], spb_[:, cols], ALU.add)
                            P.copy(LSbs[(i + 1) % 3][:, ncols], LS[:, ncols], eng='dve')

                    def stage2(i):
                        j = js[i]
                        diag, c0, cols, kTj = geom(j)
                        pla_, spb_, attn_, e1_ = plas[i % 2], spbs[i % 2], attns[i % 2], e1s[i % 2]
                        P.mm(pla_[:, cols], negrev[:], spb_[:, cols], start=True, stop=False)
                        P.mm(pla_[:, cols], negones[:], LSbs[i % 3][:, cols], start=False, stop=True)
                        P.act(ex2[:, cols], pla_[:, cols], AF.Exp)
                        P.tt(attn_[:, cols], e1_[:, cols], ex2[:, cols], ALU.mult)
                        if diag:
                            P.tt(attn_[:, c0:c0 + 128], attn_[:, c0:c0 + 128], mask_lt[:], ALU.mult, eng='pool')
                        P.mm(po[0:64, cols], v_all[:, j, :], attn_[:, cols], start=True, stop=True)
                        if diag:
                            P.copy(oacc[:, c0:c0 + 128], po[0:64, c0:c0 + 128], eng='dve')
                            if c0 + 128 < 512:
                                P.tt(oacc[:, c0 + 128:512], oacc[:, c0 + 128:512], po[0:64, c0 + 128:512], ALU.add)
                        else:
                            P.tt(oacc[:, :], oacc[:, :], po[0:64, :], ALU.add)

                    stage1(0)
                    for i in range(len(js)):
                        if i + 1 < len(js):
                            stage1(i + 1)
                        stage2(i)
                        yield
                    P.copy(yout[1][:], oacc[:], eng='act')
                    P.dma(y_d[1, :, t0:t0 + 512], yout[1][:])
                gens.append(sb_gen())

            gens = gens[::-1]
            while gens:
                for gen in list(gens):
                    try:
                        next(gen)
                    except StopIteration:
                        gens.remove(gen)
        P.emit()
        print("build_B: sbuf bytes/partition", A.bytes, "ops", P.nops)
    return nc


def _const_tables_B(S, h):
    idx = np.arange(128)
    tab = np.zeros((128, 6, 128), np.float32)
    tab[:, 0, :] = np.eye(128, dtype=np.float32)
    tab[:, 1, :] = (idx[:, None] < idx[None, :]).astype(np.float32)
    tab[:, 2, :] = -(idx[:, None] >= idx[None, :]).astype(np.float32)
    lg = np.log(np.float32(1.0) - np.float32(2.0) ** np.float32(-5.0 - h)).astype(np.float32)
    rel = (idx[None, :] - idx[:, None]).astype(np.float32)
    tab[:, 3, :] = np.where(rel >= 0, np.exp(np.maximum(rel, 0) * lg), 0.0).astype(np.float32)
    i64 = np.arange(64)
    t64 = np.zeros((64, 5, 512), np.float32)
    mU = (i64[:, None] < i64[None, :]).astype(np.float32)
    mL = (i64[None, :] < i64[:, None]).astype(np.float32)
    mUI = (i64[:, None] <= i64[None, :]).astype(np.float32)
    t64[:, 0, :] = np.tile(mU, (1, 8))
    t64[:, 1, :] = np.tile(mL, (1, 8))
    t64[:, 2, :] = np.tile(mUI, (1, 8))
    t64[:, 3, :] = np.tile(np.eye(64, dtype=np.float32), (1, 8))
    rs = np.ones(512, np.float32); rs[::64] = 0.0
    t64[:, 4, :] = rs[None, :]
    qdec = np.exp((idx.astype(np.float32) + 1.0) * lg).astype(np.float32)
    qdec4 = np.ascontiguousarray(np.tile(qdec[None, :], (64, 4)).astype(np.float32))
    kdec = np.zeros((128, 2), np.float32)
    kdec[:, 0] = np.exp((127.0 - idx.astype(np.float32)) * lg)
    kdec[:, 1] = np.exp(np.float32(128.0) * lg)
    inv_freq = (np.float32(10000.0) ** (-np.arange(0, 64, 2, dtype=np.float32) / np.float32(64))).astype(np.float32)
    ang = np.arange(S, dtype=np.float32)[:, None] * inv_freq[None, :]
    cosT = np.concatenate([np.cos(ang).T, np.cos(ang).T], axis=0).astype(np.float32)
    sinT = np.concatenate([np.sin(ang).T, np.sin(ang).T], axis=0).astype(np.float32)
    cst = np.zeros((128, 8), np.float32)
    for i, v in enumerate(CST):
        cst[:, i] = v
    return dict(tab=tab, tab64=t64.astype(ml_dtypes.bfloat16), qdec4=qdec4, kdec=kdec, cosT=np.ascontiguousarray(cosT), sinT=np.ascontiguousarray(sinT), cst=cst)


def prep_B(inp, l, b, h, xT_b, tables):
    hs = slice(64 * h, 64 * h + 64)
    w_in = inp["w_in"][l]
    RW, SB0, RT0 = 0, 1024, 1024 + 768
    cols = [w_in[:, RW + 0 + 64 * h: RW + 0 + 64 * h + 64], w_in[:, RW + 256 + 64 * h: RW + 256 + 64 * h + 64],
            w_in[:, RW + 512 + 64 * h: RW + 512 + 64 * h + 64], w_in[:, RW + 768: RW + 832], w_in[:, RW + 832: RW + 896],
            w_in[:, SB0 + 64 * h: SB0 + 64 * h + 64], w_in[:, RW + 896: RW + 1024],
            w_in[:, SB0 + 256 + 64 * h: SB0 + 256 + 64 * h + 64],
            w_in[:, SB0 + 512 + 64 * h: SB0 + 512 + 64 * h + 64],
            w_in[:, RT0 + 64 * h: RT0 + 64 * h + 64], w_in[:, RT0 + 256 + 64 * h: RT0 + 256 + 64 * h + 64],
            w_in[:, RT0 + 512 + 64 * h: RT0 + 512 + 64 * h + 64], w_in[:, RT0 + 768 + 64 * h: RT0 + 768 + 64 * h + 64]]
    wh = np.ascontiguousarray(np.concatenate(cols, axis=1))
    mu = inp["rw_mu"][l]
    vec = np.zeros((64, 16), np.float32)
    vec[:, V_MUR] = mu[0 + 64 * h: 64 * h + 64]
    vec[:, V_MUK] = mu[256 + 64 * h: 256 + 64 * h + 64]
    vec[:, V_MUV] = mu[512 + 64 * h: 512 + 64 * h + 64]
    vec[:, V_MUPW] = mu[768:832]
    vec[:, V_MUPA] = mu[832:896]
    vec[:, V_W0] = inp["rw_w0"][l][hs]
    vec[:, V_A0] = inp["rw_a0"][l][hs]
    vec[:, V_KK] = inp["rw_k_k"][l][hs]
    vec[:, V_KA] = inp["rw_k_a"][l][hs]
    vec[:, V_RK] = inp["rw_r_k"][l][h]
    vec[:, V_LNW] = inp["rw_ln_w"][l][hs]
    vec[:, V_LNB] = inp["rw_ln_b"][l][hs]
    vec[:, V_SBQ] = inp["sb_q_norm"][l]
    vec[:, V_SBK] = inp["sb_k_norm"][l]
    vec[:, V_GN] = inp["ret_gn"][l][hs]
    m = dict(xT=xT_b, gmix=np.ascontiguousarray(inp["norm_mix"][l].reshape(8, 128).T), wh=wh, vec64=vec,
             mupg=np.ascontiguousarray(mu[896:1024].reshape(128, 1)),
             w2h=np.ascontiguousarray(inp["rw_w2"][l][:, hs]), a2h=np.ascontiguousarray(inp["rw_a2"][l][:, hs]),
             g2h=np.ascontiguousarray(inp["rw_g2"][l][:, hs]))
    m.update(tables[h])
    return m


def load_cast(P, dst_bf, src_view, stg, engs=('dve', 'act')):
    C_, N_ = dst_bf.shape[1], dst_bf.shape[2]
    cap = stg.shape[1] // N_
    i = 0
    c = 0
    while c < C_:
        k = min(cap, C_ - c)
        sv = stg[:, 0:k * N_].rearrange("p (c n) -> p c n", c=k)
        P.dma(sv, src_view[:, c:c + k, :])
        P.copy(dst_bf[:, c:c + k, :], sv, eng=engs[i % len(engs)])
        c += k
        i += 1


def build_C1(NTOK):
    nc = bass.Bass("TRN2", target_bir_lowering=False)
    NT = NTOK // 512
    din = lambda n, s, d=F32: nc.dram_tensor(n, list(s), d, kind="ExternalInput").ap()
    xTh = din("xTh", [1024, 32 + NTOK])
    gmix_d = din("gmix", [128, 8])
    wconv_d = din("wconv", [1024, 512])
    cvec_d = din("cvec", [128, 2, 34])
    yT_d = din("yT", [3, 256, NTOK], BF16)
    wg_d = din("wg", [4, 1024, 1024])
    wb_d = din("wb", [4, 256, 1024])
    wout_d = din("wout", [1024, 1024])
    cst_d = din("cst", [128, 8])
    x1_d = nc.dram_tensor("x1T", [1024, NTOK], F32, kind="ExternalOutput").ap()
    with contextlib.ExitStack() as st:
        A = Alloc(nc, st); P = Prog(nc)
        gmix = A.sb("gmix_s", [128, 8]); P.dma(gmix[:], gmix_d)
        cvec = A.sb("cvec_s", [128, 2, 34]); P.dma(cvec[:], cvec_d)
        cst = A.sb("cst_s", [128, 8]); P.dma(cst[:], cst_d)
        ones_bf = A.sb("ones_bf", [128, 128], BF16); P.memset(ones_bf[:], 1.0)
        ones_s = A.sb("ones_s", [128, 128], BF16); P.memset(ones_s[:], 1.0 / 256)
        stg = A.sb("stg", [128, 4096])
        wg = A.sb("wg_b", [128, 32, 1024], BF16)
        wb = A.sb("wb_b", [128, 8, 1024], BF16)
        wout = A.sb("wout_b", [128, 8, 1024], BF16)
        wconv = A.sb("wconv_b", [128, 8, 512], BF16)
        load_cast(P, wconv[:], wconv_d.rearrange("(c p) n -> p c n", p=128), stg)
        load_cast(P, wg[:], wg_d.rearrange("i (c p) n -> p (i c) n", p=128), stg)
        load_cast(P, wb[:], wb_d.rearrange("i (c p) n -> p (i c) n", p=128), stg)
        load_cast(P, wout[:], wout_d.rearrange("(c p) n -> p c n", p=128), stg)
        xt = A.sb("xt", [128, 8, 512]); sq = A.sb("sq", [128, 8, 512], BF16); hT = A.sb("hT", [128, 8, 512], BF16)
        rstd = A.sb("rstd", [128, 512])
        xh = A.sb("xh", [128, 8, 32]); sqh = A.sb("sqh", [128, 8, 32], BF16); hTh = A.sb("hTh", [128, 8, 32], BF16)
        rstdh = A.sb("rstdh", [128, 32])
        ua = A.sb("ua", [128, 512]); sg = A.sb("sg", [128, 512])
        u = A.sb("u", [128, 2, 544])
        acc = A.sb("acc", [128, 2, 512]); accb = A.sb("accb", [128, 2, 512], BF16); tmp2 = A.sb("tmp2", [128, 512])
        ycT = A.sb("ycT", [128, 2, 512], BF16)
        ybr = A.sb("ybr", [128, 6, 512], BF16)
        merged = A.sb("merged", [128, 8, 512], BF16); macc = A.sb("macc", [128, 512]); mtmp = A.sb("mtmp", [128, 512])
        pj = [A.ps("pj%d" % i, [128, 512]) for i in range(2)]
        pg = [A.ps("pg%d" % i, [128, 512]) for i in range(2)]
        pb = [A.ps("pb%d" % i, [128, 512]) for i in range(2)]
        pn = A.ps("pn", [128, 512]); po = A.ps("po", [128, 512])
        xv = xTh.rearrange("(c p) t -> p c t", p=128)
        yv = yT_d.rearrange("i (c p) t -> p i c t", p=128)
        x1v = x1_d.rearrange("(c p) t -> p c t", p=128)

        def conv_u(hT_, N, ucol0):
            for cc in range(2):
                pa_, pb_ = pj[0], pj[1]
                for c in range(8):
                    P.mm(pa_[:, 0:N], wconv[:, c, 128 * cc:128 * cc + 128], hT_[:, c, :], start=(c == 0), stop=(c == 7))
                for c in range(8):
                    P.mm(pb_[:, 0:N], wconv[:, c, 256 + 128 * cc:256 + 128 * cc + 128], hT_[:, c, :], start=(c == 0), stop=(c == 7))
                P.act(sg[:, 0:N], pb_[:, 0:N], AF.Sigmoid)
                P.tt(u[:, cc, ucol0:ucol0 + N], sg[:, 0:N], pa_[:, 0:N], ALU.mult)

        P.dma(xh[:], xv[:, :, 0:32])
        rms_tile(P, xh[:], sqh[:], ones_bf[:], pn[:, 0:32], rstdh[:], cst, hTh, gmix)
        conv_u(hTh, 32, 0)
        for g in range(NT):
            t0 = g * 512
            P.dma(xt[:], xv[:, :, 32 + t0:32 + t0 + 512])
            for i in range(3):
                P.dma(ybr[:, 2 * i:2 * i + 2, :], yv[:, i, :, t0:t0 + 512])
            rms_tile(P, xt[:], sq[:], ones_bf[:], pn[:], rstd[:], cst, hT, gmix)
            if g > 0:
                P.copy(u[:, :, 0:32], u[:, :, 512:544], eng='pool')
            conv_u(hT, 512, 32)
            for cc in range(2):
                P.ts(acc[:, cc, :], u[:, cc, 2:514], cvec[:, cc, 0:1], cvec[:, cc, 31:32], ALU.mult, ALU.add)
                for j in range(1, 31):
                    P.stt(acc[:, cc, :], u[:, cc, 2 + j:514 + j], cvec[:, cc, j:j + 1], acc[:, cc, :], ALU.mult, ALU.add)
            P.copy(accb[:], acc[:], eng='pool')
            for cc in range(2):
                P.mm(pn[:], ones_s[:], accb[:, cc, :], start=(cc == 0), stop=(cc == 1))
            for cc in range(2):
                P.tt(acc[:, cc, :], acc[:, cc, :], pn[:], ALU.subtract)
            P.act(accb[:], acc[:], AF.Square)
            for cc in range(2):
                P.mm(pn[:], ones_s[:], accb[:, cc, :], start=(cc == 0), stop=(cc == 1))
            P.act(tmp2[:], pn[:], AF.Sqrt, bias=cst[:, K_EPS5:K_EPS5 + 1], scale=1.0)
            P.add('dve', lambda e: e.reciprocal(tmp2[:], tmp2[:]), reads=[tmp2[:]], writes=[tmp2[:]])
            for cc in range(2):
                P.tt(acc[:, cc, :], acc[:, cc, :], tmp2[:], ALU.mult)
                P.ts(acc[:, cc, :], acc[:, cc, :], cvec[:, cc, 32:33], cvec[:, cc, 33:34], ALU.mult, ALU.add)
                P.act(ycT[:, cc, :], acc[:, cc, :], AF.Silu)
            k = 0
            for m in range(8):
                mc = slice(128 * m, 128 * m + 128)
                for i in range(4):
                    pg_, pb_ = pg[k % 2], pb[k % 2]
                    k += 1
                    for c in range(8):
                        P.mm(pg_[:], wg[:, 8 * i + c, mc], hT[:, c, :], start=(c == 0), stop=(c == 7))
                    for c2 in range(2):
                        src = ybr[:, 2 * i + c2, :] if i < 3 else ycT[:, c2, :]
                        P.mm(pb_[:], wb[:, 2 * i + c2, mc], src, start=(c2 == 0), stop=(c2 == 1))
                    P.act(sg[:], pg_[:], AF.Sigmoid)
                    if i == 0:
                        P.tt(macc[:], sg[:], pb_[:], ALU.mult)
                    elif i < 3:
                        P.tt(mtmp[:], sg[:], pb_[:], ALU.mult)
                        P.tt(macc[:], macc[:], mtmp[:], ALU.add, eng='pool')
                    else:
                        P.tt(mtmp[:], sg[:], pb_[:], ALU.mult)
                        P.tt(merged[:, m, :], macc[:], mtmp[:], ALU.add, eng='pool')
            for m in range(8):
                mc = slice(128 * m, 128 * m + 128)
                for c in range(8):
                    P.mm(po[:], wout[:, c, mc], merged[:, c, :], start=(c == 0), stop=(c == 7))
                P.tt(xt[:, m, :], xt[:, m, :], po[:], ALU.add)
            P.dma(x1v[:, :, t0:t0 + 512], xt[:])
        P.emit()
        print("build_C1: sbuf bytes/partition", A.bytes, "ops", P.nops)
    return nc


def build_C2(NTOK, NF, moe):
    nc = bass.Bass("TRN2", target_bir_lowering=False)
    NT = NTOK // 512
    DFF = NF * 128
    din = lambda n, s, d=F32: nc.dram_tensor(n, list(s), d, kind="ExternalInput").ap()
    x1_d = din("x1T", [1024, NTOK])
    xa_d = din("xaT", [1024, NTOK])
    gffn_d = din("gffn", [128, 8])
    w1_d = din("w1", [1024, DFF]); w3_d = din("w3", [1024, DFF]); w2_d = din("w2", [DFF, 1024])
    cst_d = din("cst", [128, 8])
    if moe:
        rt_d = din("router", [1024, 8])
        esel_d = din("esel", [128, 8])
        idn_d = din("ident", [128, 128])
    x2_d = nc.dram_tensor("x2T", [1024, NTOK], F32, kind="ExternalOutput").ap()
    with contextlib.ExitStack() as st:
        A = Alloc(nc, st); P = Prog(nc)
        gffn = A.sb("gffn_s", [128, 8]); P.dma(gffn[:], gffn_d)
        cst = A.sb("cst_s", [128, 8]); P.dma(cst[:], cst_d)
        ones_bf = A.sb("ones_bf", [128, 128], BF16); P.memset(ones_bf[:], 1.0)
        stg = A.sb("stg", [128, 2816])
        w1 = A.sb("w1_b", [128, 8, DFF], BF16); w3 = A.sb("w3_b", [128, 8, DFF], BF16); w2 = A.sb("w2_b", [128, NF, 1024], BF16)
        load_cast(P, w1[:], w1_d.rearrange("(c p) n -> p c n", p=128), stg)
        load_cast(P, w3[:], w3_d.rearrange("(c p) n -> p c n", p=128), stg)
        load_cast(P, w2[:], w2_d.rearrange("(c p) n -> p c n", p=128), stg)
        xt = A.sb("xt", [128, 8, 512]); xa = A.sb("xa", [128, 8, 512]) if moe else xt
        sq = A.sb("sq", [128, 8, 512], BF16); hT = A.sb("hT", [128, 8, 512], BF16)
        rstd = A.sb("rstd", [128, 512])
        actT = A.sb("actT", [128, NF, 512], BF16)
        sil = [A.sb("sil%d" % i, [128, 512]) for i in range(2)]
        pa = [A.ps("pa%d" % i, [128, 512]) for i in range(2)]
        pb = [A.ps("pb%d" % i, [128, 512]) for i in range(2)]
        pn = A.ps("pn", [128, 512]); po = A.ps("po", [128, 512]); pr = A.ps("pr", [128, 512])
        if moe:
            rtf = A.sb("rtf", [128, 8, 8]); P.dma(rtf[:], rt_d.rearrange("(c p) n -> p c n", p=128))
            rtb = A.sb("rtb", [128, 8, 8], BF16); P.copy(rtb[:], rtf[:])
            esel = A.sb("esel_s", [128, 8]); P.dma(esel[:], esel_d)
            idf = A.sb("idf", [128, 128]); P.dma(idf[:], idn_d)
            idb = A.sb("idb", [128, 128], BF16); P.copy(idb[:], idf[:])
            lg = A.sb("lg", [128, 8]); mx = A.sb("mx", [128, 8]); ex = A.sb("ex", [128, 8]); msk = A.sb("msk", [128, 8])
            s1 = A.sb("s1", [128, 4]); wrep = A.sb("wrep", [128, 128], BF16); wbc = A.sb("wbc", [128, 512])
        x1v = x1_d.rearrange("(c p) t -> p c t", p=128)
        xav = xa_d.rearrange("(c p) t -> p c t", p=128)
        x2v = x2_d.rearrange("(c p) t -> p c t", p=128)
        for g in range(NT):
            t0 = g * 512
            P.dma(xt[:], x1v[:, :, t0:t0 + 512])
            if moe:
                P.dma(xa[:], xav[:, :, t0:t0 + 512])
            rms_tile(P, xt[:], sq[:], ones_bf[:], pn[:], rstd[:], cst, hT, gffn)
            if moe:
                for blk in range(4):
                    bc = slice(128 * blk, 128 * blk + 128)
                    for c in range(8):
                        P.mm(pr[:, 0:8], hT[:, c, bc], rtb[:, c, :], start=(c == 0), stop=(c == 7))
                    P.copy(lg[:], pr[:, 0:8], eng='act')
                    P.add('dve', lambda e: e.max(mx[:], lg[:]), reads=[lg[:]], writes=[mx[:]])
                    P.tt(s1[:, 0:1], mx[:, 1:2], mx[:, 0:1], ALU.subtract)
                    P.act(s1[:, 0:1], s1[:, 0:1], AF.Exp)
                    P.ts(s1[:, 0:1], s1[:, 0:1], 1.0, None, ALU.add)
                    P.add('dve', lambda e: e.reciprocal(s1[:, 1:2], s1[:, 0:1]), reads=[s1[:, 0:1]], writes=[s1[:, 1:2]])
                    P.ts(s1[:, 2:3], mx[:, 0:1], -1.0, None, ALU.mult)
                    P.act(ex[:], lg[:], AF.Exp, bias=s1[:, 2:3], scale=1.0)
                    P.ts(msk[:], lg[:], mx[:, 1:2], None, ALU.is_ge)
                    P.stt(ex[:], ex[:], s1[:, 1:2], msk[:], ALU.mult, ALU.mult)
                    P.tt(ex[:], ex[:], esel[:], ALU.mult)
                    P.reduce(s1[:, 3:4], ex[:], ALU.add)
                    P.copy(wrep[:], s1[:, 3:4].to_broadcast([128, 128]), eng='dve')
                    P.mm(pr[:, 128:256], wrep[:], idb[:])
                    P.copy(wbc[:, bc], pr[:, 128:256], eng='act')
            for f in range(NF):
                fc = slice(128 * f, 128 * f + 128)
                pa_, pb_ = pa[f % 2], pb[f % 2]
                for c in range(8):
                    P.mm(pa_[:], w1[:, c, fc], hT[:, c, :], start=(c == 0), stop=(c == 7))
                for c in range(8):
                    P.mm(pb_[:], w3[:, c, fc], hT[:, c, :], start=(c == 0), stop=(c == 7))
                P.act(sil[f % 2][:], pa_[:], AF.Silu)
                if moe:
                    P.tt(sil[f % 2][:], sil[f % 2][:], pb_[:], ALU.mult)
                    P.tt(actT[:, f, :], sil[f % 2][:], wbc[:], ALU.mult, eng='pool')
                else:
                    P.tt(actT[:, f, :], sil[f % 2][:], pb_[:], ALU.mult)
            for m in range(8):
                mc = slice(128 * m, 128 * m + 128)
                for f in range(NF):
                    P.mm(po[:], w2[:, f, mc], actT[:, f, :], start=(f == 0), stop=(f == NF - 1))
                P.tt(xa[:, m, :], xa[:, m, :], po[:], ALU.add)
            P.dma(x2v[:, :, t0:t0 + 512], xa[:])
        P.emit()
        print("build_C2: sbuf bytes/partition", A.bytes, "ops", P.nops)
    return nc


_NC_CACHE = {}


def _get(key, fn):
    if key not in _NC_CACHE:
        _NC_CACHE[key] = fn()
    return _NC_CACHE[key]


def kernel(**inp):
    inp = {k: np.asarray(v) for k, v in inp.items()}
    x = inp["x"]
    B, S, D = x.shape
    NTOK = B * S // 8
    SH = S // NTOK
    cstv = np.zeros((128, 8), np.float32)
    for i, v in enumerate(CST):
        cstv[:, i] = v
    tables = [_const_tables_B(S, h) for h in range(4)]
    xT = np.ascontiguousarray(np.transpose(x, (0, 2, 1)))
    L = inp["w_in"].shape[0]
    ident = np.eye(128, dtype=np.float32)
    for l in range(L):
        ncB = _get(("B", S), lambda: build_B(S))
        maps = [prep_B(inp, l, c // 4, c % 4, xT[c // 4], tables) for c in range(8)]
        res = run_bass_kernel_spmd(ncB, maps, core_ids=list(range(8)))
        yT = np.stack([np.asarray(res.results[c]["yT"]) for c in range(8)])
        yT = yT.reshape(B, 4, 3, 64, S).transpose(0, 2, 1, 3, 4).reshape(B, 3, 256, S)
        ncC1 = _get(("C1", NTOK), lambda: build_C1(NTOK))
        xTp = np.concatenate([np.zeros((B, D, 32), np.float32), xT], axis=2)
        cv = np.zeros((128, 2, 34), np.float32)
        for cc in range(2):
            sl = slice(128 * cc, 128 * cc + 128)
            cv[:, cc, 0:31] = inp["conv_dw"][l][:, sl].T
            cv[:, cc, 31] = inp["conv_b"][l][sl]
            cv[:, cc, 32] = inp["conv_ln_w"][l][sl]
            cv[:, cc, 33] = inp["conv_ln_b"][l][sl]
        gmix = np.ascontiguousarray(inp["norm_mix"][l].reshape(8, 128).T)
        wconv = np.ascontiguousarray(inp["w_in"][l][:, 2816:3328])
        maps = []
        for c in range(8):
            b, s0 = c // SH, (c % SH) * NTOK
            maps.append(dict(xTh=np.ascontiguousarray(xTp[b, :, s0:s0 + 32 + NTOK]), gmix=gmix, wconv=wconv, cvec=cv,
                             yT=np.ascontiguousarray(yT[b, :, :, s0:s0 + NTOK]), wg=inp["w_gate"][l], wb=inp["w_branch"][l],
                             wout=inp["w_out"][l], cst=cstv))
        res = run_bass_kernel_spmd(ncC1, maps, core_ids=list(range(8)))
        x1 = [np.asarray(res.results[c]["x1T"]) for c in range(8)]
        gffn = np.ascontiguousarray(inp["norm_ffn"][l].reshape(8, 128).T)
        if l % 2 == 0:
            ncC2 = _get(("C2", NTOK, 22, False), lambda: build_C2(NTOK, 22, False))
            j = l // 2
            maps = [dict(x1T=x1[c], xaT=x1[c], gffn=gffn, w1=inp["ffn_w1"][j], w3=inp["ffn_w3"][j], w2=inp["ffn_w2"][j], cst=cstv)
                    for c in range(8)]
            res = run_bass_kernel_spmd(ncC2, maps, core_ids=list(range(8)))
            x2 = [np.asarray(res.results[c]["x2T"]) for c in range(8)]
        else:
            ncC2 = _get(("C2moe", NTOK), lambda: build_C2moe(NTOK))
            j = l // 2
            maps = [dict(x1T=x1[c], gffn=gffn, w1=inp["moe_w1"][j], w3=inp["moe_w3"][j], w2=inp["moe_w2"][j],
                         cst=cstv, router=inp["router"][j], ident=ident) for c in range(8)]
            res = run_bass_kernel_spmd(ncC2, maps, core_ids=list(range(8)))
            x2 = [np.asarray(res.results[c]["x2T"]) for c in range(8)]
        xT = np.stack([np.concatenate(x2[b * SH:(b + 1) * SH], axis=1) for b in range(B)])
    return np.ascontiguousarray(np.transpose(xT, (0, 2, 1))).astype(np.float32)


def build_C2moe(NTOK, NE=8, NF=11):
    nc = bass.Bass("TRN2", target_bir_lowering=False)
    NT = NTOK // 512
    DFF = NF * 128
    din = lambda n, s, d=F32: nc.dram_tensor(n, list(s), d, kind="ExternalInput").ap()
    x1_d = din("x1T", [1024, NTOK])
    gffn_d = din("gffn", [128, 8])
    w1_d = din("w1", [NE, 1024, DFF]); w3_d = din("w3", [NE, 1024, DFF]); w2_d = din("w2", [NE, DFF, 1024])
    cst_d = din("cst", [128, 8])
    rt_d = din("router", [1024, 8])
    idn_d = din("ident", [128, 128])
    x2_d = nc.dram_tensor("x2T", [1024, NTOK], F32, kind="ExternalOutput").ap()
    with contextlib.ExitStack() as st:
        A = Alloc(nc, st); P = Prog(nc)
        gffn = A.sb("gffn_s", [128, 8]); P.dma(gffn[:], gffn_d)
        cst = A.sb("cst_s", [128, 8]); P.dma(cst[:], cst_d)
        ones_bf = A.sb("ones_bf", [128, 128], BF16); P.memset(ones_bf[:], 1.0)
        stg = A.sb("stg", [128, 2816])
        w1 = A.sb("w1_b", [128, 8, DFF], BF16); w3 = A.sb("w3_b", [128, 8, DFF], BF16); w2 = A.sb("w2_b", [128, NF, 1024], BF16)
        xt = A.sb("xt", [128, 8, 512])
        sq = A.sb("sq", [128, 2, 512], BF16)
        hT_all = A.sb("hT_all", [128, NT * 8, 512], BF16)
        rstd = A.sb("rstd", [128, 512])
        actT = A.sb("actT", [128, NF, 512], BF16)
        sil = [A.sb("sil%d" % i, [128, 512]) for i in range(2)]
        pa = [A.ps("pa%d" % i, [128, 512]) for i in range(2)]
        pb = [A.ps("pb%d" % i, [128, 512]) for i in range(2)]
        pn = A.ps("pn", [128, 512]); po = A.ps("po", [128, 512]); pr = A.ps("pr", [128, 512])
        rtf = A.sb("rtf", [128, 8, 8]); P.dma(rtf[:], rt_d.rearrange("(c p) n -> p c n", p=128))
        rtb = A.sb("rtb", [128, 8, 8], BF16); P.copy(rtb[:], rtf[:])
        idf = A.sb("idf", [128, 128]); P.dma(idf[:], idn_d)
        idb = A.sb("idb", [128, 128], BF16); P.copy(idb[:], idf[:])
        lg = A.sb("lg", [128, 8]); mx = A.sb("mx", [128, 8]); ex = A.sb("ex", [128, 8]); msk = A.sb("msk", [128, 8])
        s1 = A.sb("s1", [128, 4]); wrep = A.sb("wrep", [128, 128], BF16); wbc = A.sb("wbc", [128, 512])
        wgt_all = A.sb("wgt_all", [128, NT * 4, 8])
        x1v = x1_d.rearrange("(c p) t -> p c t", p=128)
        x2v = x2_d.rearrange("(c p) t -> p c t", p=128)

        class _HT:
            def __init__(self, g):
                self.g = g

            def __getitem__(self, key):
                p, c, t = key
                return hT_all[p, self.g * 8 + c, t]

        for e in range(NE):
            load_cast(P, w1[:], w1_d[e].rearrange("(c p) n -> p c n", p=128), stg)
            load_cast(P, w3[:], w3_d[e].rearrange("(c p) n -> p c n", p=128), stg)
            load_cast(P, w2[:], w2_d[e].rearrange("(c p) n -> p c n", p=128), stg)
            for g in range(NT):
                t0 = g * 512
                P.dma(xt[:], (x1v if e == 0 else x2v)[:, :, t0:t0 + 512])
                hT = _HT(g)
                if e == 0:
                    rms_tile(P, xt[:], sq[:], ones_bf[:], pn[:], rstd[:], cst, hT, gffn)
                    for blk in range(4):
                        bc = slice(128 * blk, 128 * blk + 128)
                        for c in range(8):
                            P.mm(pr[:, 0:8], hT[:, c, bc], rtb[:, c, :], start=(c == 0), stop=(c == 7))
                        P.copy(lg[:], pr[:, 0:8], eng='act')
                        P.add('dve', lambda e_: e_.max(mx[:], lg[:]), reads=[lg[:]], writes=[mx[:]])
                        P.tt(s1[:, 0:1], mx[:, 1:2], mx[:, 0:1], ALU.subtract)
                        P.act(s1[:, 0:1], s1[:, 0:1], AF.Exp)
                        P.ts(s1[:, 0:1], s1[:, 0:1], 1.0, None, ALU.add)
                        P.add('dve', lambda e_: e_.reciprocal(s1[:, 1:2], s1[:, 0:1]), reads=[s1[:, 0:1]], writes=[s1[:, 1:2]])
                        P.ts(s1[:, 2:3], mx[:, 0:1], -1.0, None, ALU.mult)
                        P.act(ex[:], lg[:], AF.Exp, bias=s1[:, 2:3], scale=1.0)
                        P.ts(msk[:], lg[:], mx[:, 1:2], None, ALU.is_ge)
                        P.stt(wgt_all[:, g * 4 + blk, :], ex[:], s1[:, 1:2], msk[:], ALU.mult, ALU.mult)
                for blk in range(4):
                    bc = slice(128 * blk, 128 * blk + 128)
                    P.copy(wrep[:], wgt_all[:, g * 4 + blk, e:e + 1].to_broadcast([128, 128]), eng='dve')
                    P.mm(pr[:, 128:256], wrep[:], idb[:])
                    P.copy(wbc[:, bc], pr[:, 128:256], eng='act')
                for f in range(NF):
                    fc = slice(128 * f, 128 * f + 128)
                    pa_, pb_ = pa[f % 2], pb[f % 2]
                    for c in range(8):
                        P.mm(pa_[:], w1[:, c, fc], hT[:, c, :], start=(c == 0), stop=(c == 7))
                    for c in range(8):
                        P.mm(pb_[:], w3[:, c, fc], hT[:, c, :], start=(c == 0), stop=(c == 7))
                    P.act(sil[f % 2][:], pa_[:], AF.Silu)
                    P.tt(sil[f % 2][:], sil[f % 2][:], pb_[:], ALU.mult)
                    P.tt(actT[:, f, :], sil[f % 2][:], wbc[:], ALU.mult, eng='pool')
                for m in range(8):
                    mc = slice(128 * m, 128 * m + 128)
                    for f in range(NF):
                        P.mm(po[:], w2[:, f, mc], actT[:, f, :], start=(f == 0), stop=(f == NF - 1))
                    P.tt(xt[:, m, :], xt[:, m, :], po[:], ALU.add)
                P.dma(x2v[:, :, t0:t0 + 512], xt[:])
        P.emit()
        print("build_C2moe: sbuf bytes/partition", A.bytes, "ops", P.nops)
    return nc
```

```python
import numpy as np
import concourse.bass as bass
import concourse.mybir as mybir

F32 = mybir.dt.float32
BF16 = mybir.dt.bfloat16
AF = mybir.ActivationFunctionType
ALU = mybir.AluOpType
AX = mybir.AxisListType

N_DMA_SEMS = 40
PSUM_NAMES = set()


def _region(ap):
    t = ap.tensor
    name = ap.name
    dims = ap.ap
    off = ap.offset
    space = str(ap.space)
    if 'DRAM' in space.upper() or 'HBM' in space.upper() or not hasattr(ap, 'base_partition') or len(dims) == 0:
        lo = off
        hi = off + sum((c - 1) * abs(s) for s, c in dims) + 1
        return (name, 0, 1, lo, hi)
    pstride = dims[0][0]
    pcount = dims[0][1]
    if pstride == 0:
        pstride = 1 << 30
    p0 = off // pstride if pstride < (1 << 30) else 0
    f0 = off - p0 * pstride if pstride < (1 << 30) else off
    f1 = f0 + sum((c - 1) * abs(s) for s, c in dims[1:]) + 1
    if name in PSUM_NAMES:
        return (name, (p0 // 32) * 32, ((p0 + pcount + 31) // 32) * 32, 0, 1 << 30)
    return (name, p0, p0 + pcount, f0, f1)


class Prog:
    def __init__(self, nc):
        self.nc = nc
        self.ops = {e: [] for e in ('pe', 'act', 'dve', 'pool', 'sp')}
        self.cnt = {e: 0 for e in ('pe', 'act', 'dve', 'pool')}
        self.recs = {}
        self.events = []
        self.known = {e: {} for e in self.ops}
        self.dma_uses = [0] * N_DMA_SEMS
        self.dma_next = 0
        self.nops = 0
        self.out_events = []

    def _is_dram(self, ap):
        s = str(ap.space).upper()
        return 'DRAM' in s or 'HBM' in s

    def add(self, eng, fn, reads=(), writes=(), dma=False):
        waits = {}

        def need(ev):
            semkey, val, _, _ = ev
            if waits.get(semkey, 0) < val:
                waits[semkey] = val

        rregs = [_region(a) for a in reads]
        wregs = [_region(a) for a in writes]
        idx = len(self.events)
        for r in rregs:
            for rec in self.recs.get(r[0], ()):
                if rec[5] != 'w':
                    continue
                if rec[1] < r[2] and r[1] < rec[2] and rec[3] < r[4] and r[3] < rec[4]:
                    ev = self.events[rec[6]]
                    if ev[2] == eng and not ev[3] and not dma and eng == 'pe':
                        continue
                    need(ev)
        for r in wregs:
            for rec in self.recs.get(r[0], ()):
                if rec[1] < r[2] and r[1] < rec[2] and rec[3] < r[4] and r[3] < rec[4]:
                    ev = self.events[rec[6]]
                    if ev[2] == eng and not ev[3] and not dma:
                        if eng == 'pe':
                            continue
                    need(ev)
        if dma:
            j = self.dma_next
            self.dma_next = (j + 1) % N_DMA_SEMS
            prev = self.dma_uses[j] * 16
            self.dma_uses[j] += 1
            val = prev + 16
            semkey = ('dma', j)
            if prev > 0:
                if waits.get(semkey, 0) < prev:
                    waits[semkey] = prev
            ev = (semkey, val, eng, True)
        else:
            self.cnt[eng] += 1
            ev = ((eng,), self.cnt[eng], eng, False)
        self.events.append(ev)
        kn = self.known[eng]
        wl = []
        for sk, v in waits.items():
            if kn.get(sk, 0) >= v:
                continue
            kn[sk] = v
            wl.append((sk, v))
        self.ops[eng].append((wl, fn, ev))
        evs = self.events
        for r in wregs:
            lst = self.recs.setdefault(r[0], [])
            lst[:] = [rec for rec in lst
                      if not (r[1] <= rec[1] and rec[2] <= r[2] and r[3] <= rec[3] and rec[4] <= r[4])]
            lst.append((r[0], r[1], r[2], r[3], r[4], 'w', idx))
        for r in rregs:
            lst = self.recs.setdefault(r[0], [])
            if not dma:
                lst[:] = [rec for rec in lst
                          if not (rec[5] == 'r' and evs[rec[6]][2] == eng and not evs[rec[6]][3]
                                  and r[1] <= rec[1] and rec[2] <= r[2] and r[3] <= rec[3] and rec[4] <= r[4])]
            lst.append((r[0], r[1], r[2], r[3], r[4], 'r', idx))
        self.nops += 1
        return ev

    def dma(self, out, in_, eng='sp', **kw):
        ev = self.add(eng, lambda e: e.dma_start(out=out, in_=in_, **kw), reads=[in_], writes=[out], dma=True)
        if self._is_dram(out):
            self.out_events.append(ev)
        return ev

    def mm(self, out, lhsT, rhs, start=True, stop=True, **kw):
        return self.add('pe', lambda e: e.matmul(out, lhsT, rhs, start=start, stop=stop, **kw),
                        reads=[lhsT, rhs], writes=[out])

    def transpose(self, out, in_, ident):
        return self.add('pe', lambda e: e.transpose(out, in_, ident), reads=[in_, ident], writes=[out])

    def act(self, out, in_, func, bias=None, scale=None, accum_out=None, eng='act'):
        kw = {}
        reads = [in_]
        writes = [out]
        if bias is not None:
            kw['bias'] = bias
            if not isinstance(bias, (int, float)):
                reads.append(bias)
        if scale is not None:
            kw['scale'] = scale
            if not isinstance(scale, (int, float)):
                reads.append(scale)
        if accum_out is not None:
            kw['accum_out'] = accum_out
            writes.append(accum_out)
        return self.add('act', lambda e: e.activation(out, in_, func, **kw), reads=reads, writes=writes)

    def tt(self, out, in0, in1, op, eng='dve'):
        return self.add(eng, lambda e: e.tensor_tensor(out, in0, in1, op), reads=[in0, in1], writes=[out])

    def ts(self, out, in0, s1, s2, op0, op1=None, eng='dve', accum_out=None):
        reads = [in0] + [s for s in (s1, s2) if s is not None and not isinstance(s, (int, float))]
        writes = [out] + ([accum_out] if accum_out is not None else [])
        kw = {}
        if op1 is not None:
            kw['op1'] = op1
        if accum_out is not None:
            kw['accum_out'] = accum_out
        return self.add(eng, lambda e: e.tensor_scalar(out, in0, s1, s2, op0, **kw), reads=reads, writes=writes)

    def stt(self, out, in0, scalar, in1, op0, op1, eng='dve', accum_out=None):
        reads = [in0, in1] + ([scalar] if not isinstance(scalar, (int, float)) else [])
        writes = [out] + ([accum_out] if accum_out is not None else [])
        kw = {}
        eng = 'dve'
        if accum_out is not None:
            kw['accum_out'] = accum_out
        return self.add(eng, lambda e: e.scalar_tensor_tensor(out, in0, scalar, in1, op0, op1, **kw),
                        reads=reads, writes=writes)

    def copy(self, out, in_, eng='dve'):
        if eng == 'act':
            return self.add('act', lambda e: e.copy(out, in_), reads=[in_], writes=[out])
        return self.add(eng, lambda e: e.tensor_copy(out, in_), reads=[in_], writes=[out])

    def memset(self, out, val, eng='pool'):
        return self.add(eng, lambda e: e.memset(out, val), reads=[], writes=[out])

    def reduce(self, out, in_, op, axis=AX.X, eng='dve'):
        return self.add(eng, lambda e: e.tensor_reduce(out, in_, axis, op), reads=[in_], writes=[out])

    def emit(self):
        nc = self.nc
        import contextlib
        with contextlib.ExitStack() as st:
            sems = {}
            for e in ('pe', 'act', 'dve', 'pool'):
                sems[(e,)] = st.enter_context(nc.semaphore("s_" + e))
            for j in range(N_DMA_SEMS):
                sems[('dma', j)] = st.enter_context(nc.semaphore("s_dma%d" % j))
            final = {}
            for ev in self.out_events:
                if final.get(ev[0], 0) < ev[1]:
                    final[ev[0]] = ev[1]
            for e in ('pe', 'act', 'dve', 'pool'):
                if self.cnt[e] > 0:
                    final[(e,)] = self.cnt[e]
            for j in range(N_DMA_SEMS):
                if self.dma_uses[j] > 0:
                    final[('dma', j)] = self.dma_uses[j] * 16
            block = st.enter_context(nc.Block())
            ops = self.ops

            def run(engname):
                def f(eng):
                    for wl, fn, ev in ops[engname]:
                        for sk, v in wl:
                            eng.wait_ge(sems[sk], v)
                        ins = fn(eng)
                        ins.then_inc(sems[ev[0]], 16 if ev[3] else 1)
                    if engname == 'sp':
                        for sk, v in final.items():
                            eng.wait_ge(sems[sk], v)
                return f

            block.sync(run('sp'))
            block.tensor(run('pe'))
            block.scalar(run('act'))
            block.vector(run('dve'))
            block.gpsimd(run('pool'))


import contextlib
import ml_dtypes
from concourse.bass_utils import run_bass_kernel_spmd

D_MODEL = 1024
NH = 4
HD = 64
RW_SCALE = 0.606531
C_R, C_K, C_V, C_PW, C_PA, C_SQ, C_PG, C_SK, C_SV, C_RQ, C_RK, C_RV, C_RG = \
    0, 64, 128, 192, 256, 320, 384, 512, 576, 640, 704, 768, 832
NCOL_B = 896
B_PARTS = ('rw', 'ret', 'sb')
RET_DBG = 3
(V_MUR, V_MUK, V_MUV, V_MUPW, V_MUPA, V_W0, V_A0, V_KK, V_KA, V_RK, V_LNW, V_LNB,
 V_SBQ, V_SBK, V_GN) = range(15)
CST = [1e-6, 64e-5, 1e-5, 1.0, 1e-12, 0.0]
K_EPS6, K_EPSRW, K_EPS5, K_ONE, K_TINY, K_ZERO = range(6)


class Alloc:
    def __init__(self, nc, st):
        self.nc = nc
        self.st = st
        self.bytes = 0

    def sb(self, name, shape, dt=F32):
        n = 1
        for s in shape[1:]:
            n *= s
        self.bytes += n * (4 if dt == F32 else 2)
        return self.st.enter_context(self.nc.sbuf_tensor(name, list(shape), dt))

    def ps(self, name, shape, dt=F32):
        PSUM_NAMES.add(name)
        return self.st.enter_context(self.nc.psum_tensor(name, list(shape), dt))


def rms_tile(P, xt, sq, ones_bf, pn, rstd, cst, hT, gain, tmpf=None):
    nsq = sq.shape[1]
    if nsq == 8:
        P.act(sq, xt, AF.Square)
    for c in range(8):
        if nsq < 8:
            P.act(sq[:, c % nsq, :], xt[:, c, :], AF.Square)
        P.mm(pn, ones_bf, sq[:, c % nsq, :], start=(c == 0), stop=(c == 7))
    P.act(rstd, pn, AF.Sqrt, scale=1.0 / D_MODEL, bias=cst[:, K_EPS6:K_EPS6 + 1])
    P.add('dve', lambda e: e.reciprocal(rstd, rstd), reads=[rstd], writes=[rstd])
    for c in range(8):
        P.stt(hT[:, c, :], xt[:, c, :], gain[:, c:c + 1], rstd, ALU.mult, ALU.mult,
              eng=('dve' if c % 2 == 0 else 'pool'))


def ln_feat(P, A, y, pn, ones_s, eps_col, tmp1, tmp2, sbf, npart=64, N=512):
    P.copy(sbf, y, eng='pool')
    P.mm(pn[0:npart, 0:N], ones_s, sbf, start=True, stop=True)
    P.tt(y, y, pn[0:npart, 0:N], ALU.subtract)
    P.act(sbf, y, AF.Square)
    P.mm(pn[0:npart, 0:N], ones_s, sbf, start=True, stop=True)
    P.act(tmp2, pn[0:npart, 0:N], AF.Sqrt, bias=eps_col, scale=1.0)
    P.add('dve', lambda e: e.reciprocal(tmp2, tmp2), reads=[tmp2], writes=[tmp2])
    P.tt(y, y, tmp2, ALU.mult)


def build_B(S):
    nc = bass.Bass("TRN2", target_bir_lowering=False)
    NT = S // 512
    NB = S // 128
    din = lambda n, s, d=F32: nc.dram_tensor(n, list(s), d, kind="ExternalInput").ap()
    xT = din("xT", [1024, S])
    gmix_d = din("gmix", [128, 8])
    wh_d = din("wh", [1024, NCOL_B])
    vec64_d = din("vec64", [64, 16])
    mupg_d = din("mupg", [128, 1])
    w2_d = din("w2h", [64, 64])
    a2_d = din("a2h", [64, 64])
    g2_d = din("g2h", [128, 64])
    cos_d = din("cosT", [64, S])
    sin_d = din("sinT", [64, S])
    cst_d = din("cst", [128, 8])
    tab_d = din("tab", [128, 6, 128])
    tab64_d = din("tab64", [64, 5, 512], BF16)
    qdec_d = din("qdec4", [64, 512])
    kdec_d = din("kdec", [128, 2])
    y_d = nc.dram_tensor("yT", [3, 64, S], BF16, kind="ExternalOutput").ap()

    with contextlib.ExitStack() as st:
        A = Alloc(nc, st)
        P = Prog(nc)
        gmix = A.sb("gmix_s", [128, 8]); P.dma(gmix[:], gmix_d)
        vec = A.sb("vec_s", [64, 16]); P.dma(vec[:], vec64_d)
        mupg = A.sb("mupg_s", [128, 1]); P.dma(mupg[:], mupg_d)
        w2f = A.sb("w2_s", [64, 64]); P.dma(w2f[:], w2_d)
        a2f = A.sb("a2_s", [64, 64]); P.dma(a2f[:], a2_d)
        g2f = A.sb("g2_s", [128, 64]); P.dma(g2f[:], g2_d)
        w2 = A.sb("w2_b", [64, 64], BF16); P.copy(w2[:], w2f[:])
        a2 = A.sb("a2_b", [64, 64], BF16); P.copy(a2[:], a2f[:])
        g2 = A.sb("g2_b", [128, 64], BF16); P.copy(g2[:], g2f[:])
        cst = A.sb("cst_s", [128, 8]); P.dma(cst[:], cst_d)
        tab = A.sb("tab_s", [128, 6, 128]); P.dma(tab[:], tab_d)
        tab64 = A.sb("tab64_s", [64, 5, 512], BF16); P.dma(tab64[:], tab64_d)
        qdec4t = A.sb("qdec4_s", [64, 512]); P.dma(qdec4t[:], qdec_d)
        kdec = A.sb("kdec_s", [128, 2]); P.dma(kdec[:], kdec_d)
        ident = A.sb("identb", [128, 128], BF16); P.copy(ident[:], tab[:, 0, :])
        ident = ident[:]
        mask_lt = A.sb("mask_lt", [128, 128], BF16); P.copy(mask_lt[:], tab[:, 1, :])
        negrev = A.sb("negrev", [128, 128], BF16); P.copy(negrev[:], tab[:, 2, :])
        decayT = tab[:, 3, :]
        maskU8, maskL8, maskUI8, I8, rstm = (tab64[:, i, :] for i in range(5))
        qdec4 = qdec4t[:]
        ones_bf = A.sb("ones_bf", [128, 128], BF16); P.memset(ones_bf[:], 1.0)
        negones = A.sb("negones", [128, 128], BF16); P.memset(negones[:], -1.0)
        ones64 = A.sb("ones64", [64, 64], BF16); P.memset(ones64[:], 1.0)
        ones64s = A.sb("ones64s", [64, 64], BF16); P.memset(ones64s[:], 1.0 / 64)
        qn8 = A.sb("qn8", [64, 1]); P.ts(qn8[:], vec[:, V_SBQ:V_SBQ + 1], 0.125, None, ALU.mult)
        V = lambda i: vec[:, i:i + 1]
        C = lambda i, n=64: cst[0:n, i:i + 1]

        xts = [A.sb("xt0", [128, 8, 512])] * 2
        wbf = A.sb("wbf", [128, 8, NCOL_B], BF16)
        sq = A.sb("sq", [128, 2, 512], BF16)
        hT = A.sb("hT", [128, 8, 512], BF16)
        rstd = A.sb("rstd", [128, 512])
        kT_all = A.sb("kT_all", [64, S], BF16)
        v_all = A.sb("v_all", [128, NB, 64], BF16)
        pj = A.ps("pj", [128, 512]); pn = A.ps("pn", [128, 512])
        px0 = A.ps("px0", [128, 512]); px1 = A.ps("px1", [128, 512]); py = A.ps("py", [128, 512])
        pz = A.ps("pz", [128, 512]); pla = A.ps("pla", [128, 512]); po = A.ps("po", [128, 512])
        pzs = [pz, pj]; plas = [pla, py]

        whv = wh_d.rearrange("(c p) n -> p c n", p=128)
        for half in range(2):
            stg = xts[half][:].rearrange("p c t -> p (c t)")[:, 0:4 * NCOL_B].rearrange("p (c n) -> p c n", c=4)
            P.dma(stg, whv[:, 4 * half:4 * half + 4, :])
            P.copy(wbf[:, 4 * half:4 * half + 4, :], stg, eng=('dve' if half == 0 else 'act'))

        names = ["raw_r", "raw_k", "raw_v", "raw_pw", "raw_pa"]
        raws = [A.sb(n, [64, 513]) for n in names]
        rawg = A.sb("raw_pg", [128, 513])
        for r_ in raws:
            P.memset(r_[:, 0:1], 0.0)
            P.memset(r_[:, 512:513], 0.0)
        P.memset(rawg[:, 0:1], 0.0); P.memset(rawg[:, 512:513], 0.0)
        e1 = A.sb("sb_e1", [128, 512]); spb = A.sb("sb_spb", [128, 512], BF16)
        LS = A.sb("sb_LS", [128, 512])
        T = {}
        for n in ["r", "k", "v", "pw", "pa", "d", "logw", "lp", "ep", "em", "epm", "a", "g", "kk", "t1",
                  "kp", "al", "be", "kt", "rb", "bon", "ysb", "tm1", "tm2"]:
            T[n] = A.sb("rw_" + n, [64, 512], BF16 if n in ("al", "be", "kt", "rb") else F32)
        sA = A.sb("rw_sA", [64, 512], BF16); sB = A.sb("rw_sB", [64, 512], BF16); vb = A.sb("rw_vb", [64, 512], BF16)
        dgb = spb
        pgm = LS; dg = e1
        M_ = [A.sb("rw_M%d" % i, [64, 8, 64], BF16) for i in range(2)]
        L_ = [A.sb("rw_L%d" % i, [64, 8, 64], BF16) for i in range(2)]
        Pt = A.sb("rw_Pt", [64, 8, 64], BF16); Pt32 = A.sb("rw_Pt32", [64, 8, 64])
        AakT = A.sb("rw_AakT", [64, 8, 64], BF16); ArbT = A.sb("rw_ArbT", [64, 8, 64], BF16)
        ArkT = A.sb("rw_ArkT", [64, 8, 64], BF16); Vt = A.sb("rw_Vt", [64, 8, 64], BF16); Bt = A.sb("rw_Bt", [64, 8, 64], BF16)
        Kt = A.sb("rw_Kt", [64, 8, 64], BF16); Us = A.sb("rw_Us", [64, 8, 64], BF16); Xs = A.sb("rw_Xs", [64, 64], BF16)
        H32 = A.sb("rw_H32", [64, 64]); H = A.sb("rw_H", [64, 64], BF16); Hd = A.sb("rw_Hd", [64, 64])
        P.memset(H32[:], 0.0); P.memset(H[:], 0.0)
        yout = [A.sb("yout%d" % i, [64, 512], BF16) for i in range(3)]
        cosb = A.sb("cosb", [64, 512]); sinb = A.sb("sinb", [64, 512])
        tq = T["d"]; t1 = T["tm1"]; t2 = T["tm2"]
        qr = A.sb("rt_qr", [64, 512], BF16); kr = A.sb("rt_kr", [64, 512], BF16); qd = A.sb("rt_qd", [64, 512], BF16)
        rv = A.sb("rt_rvb", [64, 512], BF16); rg = T["pa"]
        sc = A.sb("rt_sc", [128, 128], BF16); vtok = A.sb("rt_vtok", [128, 64], BF16); ktok = A.sb("rt_ktok", [128, 64], BF16)
        rst = A.sb("rt_st", [64, 64]); rst_bf = A.sb("rt_stbf", [64, 64], BF16)
        P.memset(rst[:], 0.0); P.memset(rst_bf[:], 0.0)
        ro = A.sb("rt_ro", [64, 512])
        sq_t = T["logw"]; sq_s = T["lp"]; sq_r = T["em"]
        qT = A.sb("sb_qT", [64, 512], BF16); svT = sB
        attn = A.sb("sb_attn", [128, 512], BF16)
        oacc = A.sb("sb_oacc", [64, 512])
        e1s = [e1, A.sb("sb_e1b", [128, 512])]; spbs = [spb, A.sb("sb_spb2", [128, 512], BF16)]
        attns = [attn, A.sb("sb_attn2", [128, 512], BF16)]
        ex2 = A.sb("sb_ex2", [128, 512], BF16)
        LSbs = [A.sb("sb_LSb%d" % i, [128, 512], BF16) for i in range(3)]

        xTv = xT.rearrange("(c p) t -> p c t", p=128)

        def proj(col0, M, ps):
            for c in range(8):
                P.mm(ps, wbf[:, c, col0:col0 + M], hT[:, c, :], start=(c == 0), stop=(c == 7))

        for g in range(NT):
            t0 = g * 512
            gens = []
            xt = xts[g % 2]
            P.dma(xt[:], xTv[:, :, t0:t0 + 512])
            P.dma(cosb[:], cos_d[:, t0:t0 + 512])
            P.dma(sinb[:], sin_d[:, t0:t0 + 512])
            rms_tile(P, xt[:], sq[:], ones_bf[:], pj[:], rstd[:], cst, hT, gmix)

            if 'rw' in B_PARTS:
                mixed = [T["r"], T["k"], T["v"], T["pw"], T["pa"]]
                mus = [V_MUR, V_MUK, V_MUV, V_MUPW, V_MUPA]

                def shift_mix(i, src_ps):
                    raw = raws[i]
                    if g > 0:
                        P.copy(raw[:, 0:1], raw[:, 512:513], eng='pool')
                    P.copy(raw[:, 1:513], src_ps, eng='act')
                proj(C_R, 128, pj[:, :]); shift_mix(0, pj[0:64, :]); shift_mix(1, pj[64:128, :])
                proj(C_V, 128, pj[:, :]); shift_mix(2, pj[0:64, :]); shift_mix(3, pj[64:128, :])
                proj(C_PA, 128, pj[:, :]); shift_mix(4, pj[0:64, :])
                P.copy(ro[:], pj[64:128, :], eng='act')
                if g > 0:
                    P.copy(rawg[:, 0:1], rawg[:, 512:513], eng='pool')
                proj(C_PG, 128, pj[:, :])
                P.copy(rawg[:, 1:513], pj[:, :], eng='act')

                for i_ in range(5):
                    P.tt(T["d"][:], raws[i_][:, 0:512], raws[i_][:, 1:513], ALU.subtract, eng='pool')
                    P.stt(mixed[i_][:], T["d"][:], V(mus[i_]), raws[i_][:, 1:513], ALU.mult, ALU.add)
                P.tt(dg[:], rawg[:, 0:512], rawg[:, 1:513], ALU.subtract, eng='pool')
                P.stt(pgm[:], dg[:], mupg[:, 0:1], rawg[:, 1:513], ALU.mult, ALU.add)
                P.copy(sB[:], T["pa"][:], eng='pool')
                P.mm(pn[0:64, :], a2[:], sB[:])
                P.act(T["a"][:], pn[0:64, :], AF.Sigmoid, bias=V(V_A0))
                P.act(dgb[:], pgm[:], AF.Sigmoid)
                P.mm(pn[0:64, :], g2[:], dgb[:])
                P.copy(T["g"][:], pn[0:64, :], eng='act')

                def rw_gen():
                    ysb = T["ysb"]
                    r_, k_, v_ = T["r"], T["k"], T["v"]
                    P.act(sA[:], T["pw"][:], AF.Tanh)
                    P.mm(pn[0:64, :], w2[:], sA[:])
                    P.act(T["logw"][:], pn[0:64, :], AF.Sigmoid, bias=V(V_W0))
                    P.ts(T["logw"][:], T["logw"][:], -RW_SCALE, None, ALU.mult)
                    P.add('dve', lambda e: e.tensor_tensor_scan(T["lp"][:], rstm, T["logw"][:], 0.0, ALU.mult, ALU.add),
                          reads=[rstm, T["logw"][:]], writes=[T["lp"][:]])
                    P.act(T["ep"][:], T["lp"][:], AF.Exp)
                    P.act(T["em"][:], T["lp"][:], AF.Exp, scale=-1.0)
                    P.tt(T["tm1"][:], T["lp"][:], T["logw"][:], ALU.subtract, eng='pool')
                    P.act(T["epm"][:], T["tm1"][:], AF.Exp)
                    yield
                    P.ts(T["kk"][:], k_[:], V(V_KK), None, ALU.mult)
                    P.act(sA[:], T["kk"][:], AF.Square)
                    P.mm(pn[0:64, :], ones64[:], sA[:])
                    P.act(T["tm2"][:], pn[0:64, :], AF.Sqrt)
                    P.ts(T["tm2"][:], T["tm2"][:], 1e-12, None, ALU.max)
                    P.add('dve', lambda e: e.reciprocal(T["tm2"][:], T["tm2"][:]), reads=[T["tm2"][:]], writes=[T["tm2"][:]])
                    P.tt(T["kk"][:], T["kk"][:], T["tm2"][:], ALU.mult)
                    yield
                    P.ts(T["t1"][:], T["a"][:], 1.0, V(V_KA), ALU.subtract, ALU.mult)
                    P.stt(T["kp"][:], T["t1"][:], 1.0, k_[:], ALU.add, ALU.mult)
                    yield
                    P.stt(T["al"][:], T["kk"][:], -1.0, T["epm"][:], ALU.mult, ALU.mult)
                    P.tt(T["t1"][:], T["kk"][:], T["a"][:], ALU.mult, eng='pool')
                    P.tt(T["be"][:], T["t1"][:], T["em"][:], ALU.mult, eng='pool')
                    P.tt(T["kt"][:], T["kp"][:], T["em"][:], ALU.mult)
                    P.tt(T["rb"][:], r_[:], T["ep"][:], ALU.mult, eng='pool')
                    yield
                    P.stt(sB[:], r_[:], V(V_RK), T["kp"][:], ALU.mult, ALU.mult)
                    P.mm(pn[0:64, :], ones64[:], sB[:])
                    P.copy(vb[:], v_[:], eng='pool')
                    P.tt(T["bon"][:], pn[0:64, :], v_[:], ALU.mult)
                    yield

                    al, be, kt, rb = T["al"], T["be"], T["kt"], T["rb"]
                    X0 = px0[0:64, :].rearrange("p (c n) -> p c n", c=8)
                    X1 = px1[0:64, :].rearrange("p (c n) -> p c n", c=8)
                    X2 = pn[0:64, :].rearrange("p (c n) -> p c n", c=8)
                    f3 = lambda ap: ap
                    fl = lambda t3: t3.rearrange("p c n -> p (c n)")
                    cs = lambda c: slice(64 * c, 64 * c + 64)
                    for c in range(8):
                        P.mm(X0[:, c, :], be[:, cs(c)], al[:, cs(c)])
                        P.mm(X1[:, c, :], al[:, cs(c)], be[:, cs(c)])
                    P.tt(fl(Pt32[:]), px0[0:64, :], maskU8, ALU.mult)
                    P.copy(fl(M_[0][:]), fl(Pt32[:]), eng='pool')
                    P.tt(fl(L_[0][:]), px1[0:64, :], maskL8, ALU.mult, eng='pool' if False else 'dve')
                    yield
                    for c in range(8):
                        P.mm(X2[:, c, :], kt[:, cs(c)], al[:, cs(c)])
                    P.tt(fl(AakT[:]), pn[0:64, :], maskU8, ALU.mult)
                    yield
                    for c in range(8):
                        P.mm(X0[:, c, :], be[:, cs(c)], rb[:, cs(c)])
                        P.mm(X1[:, c, :], kt[:, cs(c)], rb[:, cs(c)])
                    P.tt(fl(ArbT[:]), px0[0:64, :], maskUI8, ALU.mult)
                    P.tt(fl(ArkT[:]), px1[0:64, :], maskUI8, ALU.mult)
                    yield
                    for c in range(8):
                        P.mm(X2[:, c, :], vb[:, cs(c)], ident[0:64, 0:64])
                        P.mm(X0[:, c, :], be[:, cs(c)], ident[0:64, 0:64])
                        P.mm(X1[:, c, :], kt[:, cs(c)], ident[0:64, 0:64])
                    P.copy(fl(Vt[:]), pn[0:64, :], eng='act')
                    P.copy(fl(Bt[:]), px0[0:64, :], eng='dve')
                    P.copy(fl(Kt[:]), px1[0:64, :], eng='act')
                    yield
                    P.tt(fl(Pt32[:]), fl(Pt32[:]), I8, ALU.add)
                    P.copy(fl(Pt[:]), fl(Pt32[:]), eng='pool')
                    yield
                    for n in range(1, 6):
                        Mp, Lp = M_[(n - 1) % 2], L_[(n - 1) % 2]
                        Mn, Ln = M_[n % 2], L_[n % 2]
                        for c in range(8):
                            if n < 5:
                                P.mm(X0[:, c, :], Lp[:, c, :], Mp[:, c, :])
                            P.mm(X1[:, c, :], Mp[:, c, :], Lp[:, c, :])
                        if n < 5:
                            P.copy(fl(Mn[:]), px0[0:64, :], eng='act')
                        P.copy(fl(Ln[:]), px1[0:64, :], eng='dve')
                        for c in range(8):
                            P.mm(X2[:, c, :], Ln[:, c, :], Pt[:, c, :])
                        P.tt(fl(Pt32[:]), fl(Pt32[:]), pn[0:64, :], ALU.add)
                        P.copy(fl(Pt[:]), fl(Pt32[:]), eng='pool')
                        yield
                    for c in range(8):
                        epC = T["ep"][:, 64 * c + 63:64 * c + 64]
                        P.mm(X0[:, c, :], al[:, cs(c)], H[:], start=True, stop=False)
                        P.mm(X0[:, c, :], AakT[:, c, :], Vt[:, c, :], start=False, stop=True)
                        P.copy(Xs[:], X0[:, c, :], eng='act')
                        P.mm(X1[:, c, :], Pt[:, c, :], Xs[:])
                        P.copy(Us[:, c, :], X1[:, c, :], eng='dve')
                        yield
                        P.mm(X0[:, c, :], H[:], rb[:, cs(c)], start=True, stop=False)
                        P.mm(X0[:, c, :], Us[:, c, :], ArbT[:, c, :], start=False, stop=False)
                        P.mm(X0[:, c, :], Vt[:, c, :], ArkT[:, c, :], start=False, stop=True)
                        P.copy(ysb[:, cs(c)], X0[:, c, :], eng='act')
                        P.ts(Hd[:], H32[:], epC, None, ALU.mult, eng='pool')
                        P.mm(X2[:, c, :], Bt[:, c, :], Us[:, c, :], start=True, stop=False)
                        P.mm(X2[:, c, :], Kt[:, c, :], Vt[:, c, :], start=False, stop=True)
                        P.stt(H32[:], X2[:, c, :], epC, Hd[:], ALU.mult, ALU.add)
                        P.copy(H[:], H32[:], eng='pool')
                        yield
                    ln_feat(P, A, ysb[:], pn, ones64s[:], C(K_EPSRW), T["tm1"][:], T["tm2"][:], sA[:])
                    P.ts(ysb[:], ysb[:], V(V_LNW), V(V_LNB), ALU.mult, ALU.add)
                    P.tt(ysb[:], ysb[:], T["bon"][:], ALU.add)
                    P.tt(yout[0][:], ysb[:], T["g"][:], ALU.mult)
                    P.dma(y_d[0, :, t0:t0 + 512], yout[0][:])
                gens.append(rw_gen())

            if 'ret' in B_PARTS:
                def rotary(src_ps, dst, scale):
                    P.copy(tq[:], src_ps, eng='act')
                    P.stt(t1[0:32, :], tq[32:64, :], scale, sinb[32:64, :], ALU.mult, ALU.mult, eng='pool')
                    P.stt(t1[32:64, :], tq[0:32, :], scale, sinb[0:32, :], ALU.mult, ALU.mult, eng='pool')
                    P.stt(t2[:], tq[:], scale, cosb[:], ALU.mult, ALU.mult)
                    P.tt(dst[0:32, :], t2[0:32, :], t1[0:32, :], ALU.subtract)
                    P.tt(dst[32:64, :], t2[32:64, :], t1[32:64, :], ALU.add)
                proj(C_RQ, 128, pj[:, :]); rotary(pj[0:64, :], qr, 1.0); rotary(pj[64:128, :], kr, 0.125)
                P.tt(qd[:], qr[:], qdec4, ALU.mult, eng='pool')
                proj(C_RV, 128, pj[:, :]); P.copy(rv[:], pj[0:64, :], eng='act'); P.act(rg[:], pj[64:128, :], AF.Silu)
                def ret_gen():
                    for c in range(4):
                        c4 = slice(128 * c, 128 * c + 128)
                        P.mm(px0[:, 0:128], kr[:, c4], qr[:, c4])
                        P.tt(sc[:], px0[:, 0:128], decayT, ALU.mult)
                        P.mm(px1[:, 0:64], rv[:, c4], ident[0:64, 0:64])
                        P.copy(vtok[:], px1[:, 0:64], eng='act')
                        P.mm(px1[:, 64:128], kr[:, c4], ident[0:64, 0:64])
                        P.ts(ktok[:], px1[:, 64:128], kdec[:, 0:1], None, ALU.mult)
                        yield
                        P.mm(px0[0:64, 0:128], vtok[:], sc[:], start=True, stop=False)
                        P.mm(px0[0:64, 0:128], rst_bf[:], qd[:, c4], start=False, stop=True)
                        P.copy(ro[:, c4], px0[0:64, 0:128], eng='act')
                        P.mm(px1[0:64, 128:192], ktok[:], vtok[:])
                        P.stt(rst[:], rst[:], kdec[0:64, 1:2], px1[0:64, 128:192], ALU.mult, ALU.add)
                        P.copy(rst_bf[:], rst[:], eng='pool')
                        yield
                    ln_feat(P, A, ro[:], pn, ones64s[:], C(K_EPS5), t1[:], t2[:], sA[:])
                    P.stt(yout[2][:], ro[:], V(V_GN), rg[:], ALU.mult, ALU.mult)
                    P.dma(y_d[2, :, t0:t0 + 512], yout[2][:])
                gens.append(ret_gen())

            if 'sb' in B_PARTS:
                def qknorm(src, dst, gcol):
                    P.copy(sq_t[:], src, eng='act')
                    P.act(sA[:], sq_t[:], AF.Square)
                    P.mm(pn[0:64, :], ones64s[:], sA[:])
                    P.act(sq_r[:], pn[0:64, :], AF.Sqrt, bias=C(K_EPS6), scale=1.0)
                    P.add('dve', lambda e: e.reciprocal(sq_r[:], sq_r[:]), reads=[sq_r[:]], writes=[sq_r[:]])
                    P.stt(dst, sq_t[:], gcol, sq_r[:], ALU.mult, ALU.mult)
                qknorm(ro[:], qT[:], qn8[:, 0:1])
                proj(C_SK, 128, pj[:, :])
                P.copy(svT[:], pj[64:128, :], eng='act')
                qknorm(pj[0:64, :], kT_all[:, t0:t0 + 512], V(V_SBK))
                for i in range(4):
                    P.mm(px1[:, 0:64], svT[:, 128 * i:128 * i + 128], ident[0:64, 0:64])
                    P.copy(v_all[:, 4 * g + i, :], px1[:, 0:64], eng='act')
                P.memset(LS[:], 0.0); P.memset(LSbs[0][:], 0.0)
                def sb_gen():
                    js = list(range(4 * g + 3, -1, -1))

                    def geom(j):
                        diag = j >= 4 * g
                        c0 = (j - 4 * g) * 128 if diag else 0
                        return diag, c0, slice(c0, 512), kT_all[:, 128 * j:128 * j + 128]

                    def stage1(i):
                        j = js[i]
                        diag, c0, cols, kTj = geom(j)
                        pz_, e1_, spb_ = pzs[i % 2], e1s[i % 2], spbs[i % 2]
                        P.mm(pz_[:, cols], kTj, qT[:, cols])
                        P.act(e1_[:, cols], pz_[:, cols], AF.Exp)
                        P.act(spb_[:, cols], e1_[:, cols], AF.Ln, bias=cst[:, K_ONE:K_ONE + 1], scale=1.0)
                        if diag:
                            P.tt(spb_[:, c0:c0 + 128], spb_[:, c0:c0 + 128], mask_lt[:], ALU.mult, eng='pool')
                        if i + 1 < len(js):
                            ncols = geom(js[i + 1])[2]
                            P.tt(LS[:, cols], LS[:, cols], spb_[:, cols], ALU.add)
                            P.copy(LSbs[(i + 1) % 3][:, ncols], LS[:, ncols], eng='dve')

                    def stage2(i):
                        j = js[i]
                        diag, c0, cols, kTj = geom(j)
                        pla_, spb_, attn_, e1_ = plas[i % 2], spbs[i % 2], attns[i % 2], e1s[i % 2]
                        P.mm(pla_[:, cols], kTj, qT[:, cols], start=True, stop=False)
                        P.mm(pla_[:, cols], negrev[:], spb_[:, cols], start=False, stop=False)
                        P.mm(pla_[:, cols], negones[:], LSbs[i % 3][:, cols], start=False, stop=True)
                        P.act(attn_[:, cols], pla_[:, cols], AF.Exp)
                        if diag:
                            P.tt(attn_[:, c0:c0 + 128], attn_[:, c0:c0 + 128], mask_lt[:], ALU.mult, eng='pool')
                        P.mm(po[0:64, cols], v_all[:, j, :], attn_[:, cols], start=True, stop=True)
                        if diag:
                            P.copy(oacc[:, c0:c0 + 128], po[0:64, c0:c0 + 128], eng='dve')
                            if c0 + 128 < 512:
                                P.tt(oacc[:, c0 + 128:512], oacc[:, c0 + 128:512], po[0:64, c0 + 128:512], ALU.add)
                        else:
                            P.tt(oacc[:, :], oacc[:, :], po[0:64, :], ALU.add)

                    stage1(0)
                    for i in range(len(js)):
                        if i + 1 < len(js):
                            stage1(i + 1)
                        stage2(i)
                        yield
                    P.copy(yout[1][:], oacc[:], eng='act')
                    P.dma(y_d[1, :, t0:t0 + 512], yout[1][:])
                gens.append(sb_gen())

            gens = gens[::-1]
            while gens:
                for gen in list(gens):
                    try:
                        next(gen)
                    except StopIteration:
                        gens.remove(gen)
        P.emit()
        print("build_B: sbuf bytes/partition", A.bytes, "ops", P.nops)
    return nc


def _const_tables_B(S, h):
    idx = np.arange(128)
    tab = np.zeros((128, 6, 128), np.float32)
    tab[:, 0, :] = np.eye(128, dtype=np.float32)
    tab[:, 1, :] = (idx[:, None] < idx[None, :]).astype(np.float32)
    tab[:, 2, :] = -(idx[:, None] >= idx[None, :]).astype(np.float32)
    lg = np.log(np.float32(1.0) - np.float32(2.0) ** np.float32(-5.0 - h)).astype(np.float32)
    rel = (idx[None, :] - idx[:, None]).astype(np.float32)
    tab[:, 3, :] = np.where(rel >= 0, np.exp(np.maximum(rel, 0) * lg), 0.0).astype(np.float32)
    i64 = np.arange(64)
    t64 = np.zeros((64, 5, 512), np.float32)
    mU = (i64[:, None] < i64[None, :]).astype(np.float32)
    mL = (i64[None, :] < i64[:, None]).astype(np.float32)
    mUI = (i64[:, None] <= i64[None, :]).astype(np.float32)
    t64[:, 0, :] = np.tile(mU, (1, 8))
    t64[:, 1, :] = np.tile(mL, (1, 8))
    t64[:, 2, :] = np.tile(mUI, (1, 8))
    t64[:, 3, :] = np.tile(np.eye(64, dtype=np.float32), (1, 8))
    rs = np.ones(512, np.float32); rs[::64] = 0.0
    t64[:, 4, :] = rs[None, :]
    qdec = np.exp((idx.astype(np.float32) + 1.0) * lg).astype(np.float32)
    qdec4 = np.ascontiguousarray(np.tile(qdec[None, :], (64, 4)).astype(np.float32))
    kdec = np.zeros((128, 2), np.float32)
    kdec[:, 0] = np.exp((127.0 - idx.astype(np.float32)) * lg)
    kdec[:, 1] = np.exp(np.float32(128.0) * lg)
    inv_freq = (np.float32(10000.0) ** (-np.arange(0, 64, 2, dtype=np.float32) / np.float32(64))).astype(np.float32)
    ang = np.arange(S, dtype=np.float32)[:, None] * inv_freq[None, :]
    cosT = np.concatenate([np.cos(ang).T, np.cos(ang).T], axis=0).astype(np.float32)
    sinT = np.concatenate([np.sin(ang).T, np.sin(ang).T], axis=0).astype(np.float32)
    cst = np.zeros((128, 8), np.float32)
    for i, v in enumerate(CST):
        cst[:, i] = v
    return dict(tab=tab, tab64=t64.astype(ml_dtypes.bfloat16), qdec4=qdec4, kdec=kdec, cosT=np.ascontiguousarray(cosT), sinT=np.ascontiguousarray(sinT), cst=cst)


def prep_B(inp, l, b, h, xT_b, tables):
    hs = slice(64 * h, 64 * h + 64)
    w_in = inp["w_in"][l]
    RW, SB0, RT0 = 0, 1024, 1024 + 768
    cols = [w_in[:, RW + 0 + 64 * h: RW + 0 + 64 * h + 64], w_in[:, RW + 256 + 64 * h: RW + 256 + 64 * h + 64],
            w_in[:, RW + 512 + 64 * h: RW + 512 + 64 * h + 64], w_in[:, RW + 768: RW + 832], w_in[:, RW + 832: RW + 896],
            w_in[:, SB0 + 64 * h: SB0 + 64 * h + 64], w_in[:, RW + 896: RW + 1024],
            w_in[:, SB0 + 256 + 64 * h: SB0 + 256 + 64 * h + 64],
            w_in[:, SB0 + 512 + 64 * h: SB0 + 512 + 64 * h + 64],
            w_in[:, RT0 + 64 * h: RT0 + 64 * h + 64], w_in[:, RT0 + 256 + 64 * h: RT0 + 256 + 64 * h + 64],
            w_in[:, RT0 + 512 + 64 * h: RT0 + 512 + 64 * h + 64], w_in[:, RT0 + 768 + 64 * h: RT0 + 768 + 64 * h + 64]]
    wh = np.ascontiguousarray(np.concatenate(cols, axis=1))
    mu = inp["rw_mu"][l]
    vec = np.zeros((64, 16), np.float32)
    vec[:, V_MUR] = mu[0 + 64 * h: 64 * h + 64]
    vec[:, V_MUK] = mu[256 + 64 * h: 256 + 64 * h + 64]
    vec[:, V_MUV] = mu[512 + 64 * h: 512 + 64 * h + 64]
    vec[:, V_MUPW] = mu[768:832]
    vec[:, V_MUPA] = mu[832:896]
    vec[:, V_W0] = inp["rw_w0"][l][hs]
    vec[:, V_A0] = inp["rw_a0"][l][hs]
    vec[:, V_KK] = inp["rw_k_k"][l][hs]
    vec[:, V_KA] = inp["rw_k_a"][l][hs]
    vec[:, V_RK] = inp["rw_r_k"][l][h]
    vec[:, V_LNW] = inp["rw_ln_w"][l][hs]
    vec[:, V_LNB] = inp["rw_ln_b"][l][hs]
    vec[:, V_SBQ] = inp["sb_q_norm"][l]
    vec[:, V_SBK] = inp["sb_k_norm"][l]
    vec[:, V_GN] = inp["ret_gn"][l][hs]
    m = dict(xT=xT_b, gmix=np.ascontiguousarray(inp["norm_mix"][l].reshape(8, 128).T), wh=wh, vec64=vec,
             mupg=np.ascontiguousarray(mu[896:1024].reshape(128, 1)),
             w2h=np.ascontiguousarray(inp["rw_w2"][l][:, hs]), a2h=np.ascontiguousarray(inp["rw_a2"][l][:, hs]),
             g2h=np.ascontiguousarray(inp["rw_g2"][l][:, hs]))
    m.update(tables[h])
    return m


def load_cast(P, dst_bf, src_view, stg, engs=('dve', 'act')):
    C_, N_ = dst_bf.shape[1], dst_bf.shape[2]
    cap = stg.shape[1] // N_
    i = 0
    c = 0
    while c < C_:
        k = min(cap, C_ - c)
        sv = stg[:, 0:k * N_].rearrange("p (c n) -> p c n", c=k)
        P.dma(sv, src_view[:, c:c + k, :])
        P.copy(dst_bf[:, c:c + k, :], sv, eng=engs[i % len(engs)])
        c += k
        i += 1


def build_C1(NTOK):
    nc = bass.Bass("TRN2", target_bir_lowering=False)
    NT = NTOK // 512
    din = lambda n, s, d=F32: nc.dram_tensor(n, list(s), d, kind="ExternalInput").ap()
    xTh = din("xTh", [1024, 32 + NTOK])
    gmix_d = din("gmix", [128, 8])
    wconv_d = din("wconv", [1024, 512])
    cvec_d = din("cvec", [128, 2, 34])
    yT_d = din("yT", [3, 256, NTOK], BF16)
    wg_d = din("wg", [4, 1024, 1024])
    wb_d = din("wb", [4, 256, 1024])
    wout_d = din("wout", [1024, 1024])
    cst_d = din("cst", [128, 8])
    x1_d = nc.dram_tensor("x1T", [1024, NTOK], F32, kind="ExternalOutput").ap()
    with contextlib.ExitStack() as st:
        A = Alloc(nc, st); P = Prog(nc)
        gmix = A.sb("gmix_s", [128, 8]); P.dma(gmix[:], gmix_d)
        cvec = A.sb("cvec_s", [128, 2, 34]); P.dma(cvec[:], cvec_d)
        cst = A.sb("cst_s", [128, 8]); P.dma(cst[:], cst_d)
        ones_bf = A.sb("ones_bf", [128, 128], BF16); P.memset(ones_bf[:], 1.0)
        ones_s = A.sb("ones_s", [128, 128], BF16); P.memset(ones_s[:], 1.0 / 256)
        stg = A.sb("stg", [128, 4096])
        wg = A.sb("wg_b", [128, 32, 1024], BF16)
        wb = A.sb("wb_b", [128, 8, 1024], BF16)
        wout = A.sb("wout_b", [128, 8, 1024], BF16)
        wconv = A.sb("wconv_b", [128, 8, 512], BF16)
        load_cast(P, wconv[:], wconv_d.rearrange("(c p) n -> p c n", p=128), stg)
        load_cast(P, wg[:], wg_d.rearrange("i (c p) n -> p (i c) n", p=128), stg)
        load_cast(P, wb[:], wb_d.rearrange("i (c p) n -> p (i c) n", p=128), stg)
        load_cast(P, wout[:], wout_d.rearrange("(c p) n -> p c n", p=128), stg)
        xt = A.sb("xt", [128, 8, 512]); sq = A.sb("sq", [128, 8, 512], BF16); hT = A.sb("hT", [128, 8, 512], BF16)
        rstd = A.sb("rstd", [128, 512])
        xh = A.sb("xh", [128, 8, 32]); sqh = A.sb("sqh", [128, 8, 32], BF16); hTh = A.sb("hTh", [128, 8, 32], BF16)
        rstdh = A.sb("rstdh", [128, 32])
        ua = A.sb("ua", [128, 512]); sg = A.sb("sg", [128, 512])
        u = A.sb("u", [128, 2, 544])
        acc = A.sb("acc", [128, 2, 512]); accb = A.sb("accb", [128, 2, 512], BF16); tmp2 = A.sb("tmp2", [128, 512])
        ycT = A.sb("ycT", [128, 2, 512], BF16)
        ybr = A.sb("ybr", [128, 6, 512], BF16)
        merged = A.sb("merged", [128, 8, 512], BF16); macc = A.sb("macc", [128, 512]); mtmp = A.sb("mtmp", [128, 512])
        pj = [A.ps("pj%d" % i, [128, 512]) for i in range(2)]
        pg = [A.ps("pg%d" % i, [128, 512]) for i in range(2)]
        pb = [A.ps("pb%d" % i, [128, 512]) for i in range(2)]
        pn = A.ps("pn", [128, 512]); po = A.ps("po", [128, 512])
        xv = xTh.rearrange("(c p) t -> p c t", p=128)
        yv = yT_d.rearrange("i (c p) t -> p i c t", p=128)
        x1v = x1_d.rearrange("(c p) t -> p c t", p=128)

        def conv_u(hT_, N, ucol0):
            for cc in range(2):
                pa_, pb_ = pj[0], pj[1]
                for c in range(8):
                    P.mm(pa_[:, 0:N], wconv[:, c, 128 * cc:128 * cc + 128], hT_[:, c, :], start=(c == 0), stop=(c == 7))
                for c in range(8):
                    P.mm(pb_[:, 0:N], wconv[:, c, 256 + 128 * cc:256 + 128 * cc + 128], hT_[:, c, :], start=(c == 0), stop=(c == 7))
                P.act(sg[:, 0:N], pb_[:, 0:N], AF.Sigmoid)
                P.tt(u[:, cc, ucol0:ucol0 + N], sg[:, 0:N], pa_[:, 0:N], ALU.mult)

        P.dma(xh[:], xv[:, :, 0:32])
        rms_tile(P, xh[:], sqh[:], ones_bf[:], pn[:, 0:32], rstdh[:], cst, hTh, gmix)
        conv_u(hTh, 32, 0)
        for g in range(NT):
            t0 = g * 512
            P.dma(xt[:], xv[:, :, 32 + t0:32 + t0 + 512])
            for i in range(3):
                P.dma(ybr[:, 2 * i:2 * i + 2, :], yv[:, i, :, t0:t0 + 512])
            rms_tile(P, xt[:], sq[:], ones_bf[:], pn[:], rstd[:], cst, hT, gmix)
            if g > 0:
                P.copy(u[:, :, 0:32], u[:, :, 512:544], eng='pool')
            conv_u(hT, 512, 32)
            for cc in range(2):
                P.ts(acc[:, cc, :], u[:, cc, 2:514], cvec[:, cc, 0:1], cvec[:, cc, 31:32], ALU.mult, ALU.add)
                for j in range(1, 31):
                    P.stt(acc[:, cc, :], u[:, cc, 2 + j:514 + j], cvec[:, cc, j:j + 1], acc[:, cc, :], ALU.mult, ALU.add)
            P.copy(accb[:], acc[:], eng='pool')
            for cc in range(2):
                P.mm(pn[:], ones_s[:], accb[:, cc, :], start=(cc == 0), stop=(cc == 1))
            for cc in range(2):
                P.tt(acc[:, cc, :], acc[:, cc, :], pn[:], ALU.subtract)
            P.act(accb[:], acc[:], AF.Square)
            for cc in range(2):
                P.mm(pn[:], ones_s[:], accb[:, cc, :], start=(cc == 0), stop=(cc == 1))
            P.act(tmp2[:], pn[:], AF.Sqrt, bias=cst[:, K_EPS5:K_EPS5 + 1], scale=1.0)
            P.add('dve', lambda e: e.reciprocal(tmp2[:], tmp2[:]), reads=[tmp2[:]], writes=[tmp2[:]])
            for cc in range(2):
                P.tt(acc[:, cc, :], acc[:, cc, :], tmp2[:], ALU.mult)
                P.ts(acc[:, cc, :], acc[:, cc, :], cvec[:, cc, 32:33], cvec[:, cc, 33:34], ALU.mult, ALU.add)
                P.act(ycT[:, cc, :], acc[:, cc, :], AF.Silu)
            k = 0
            for m in range(8):
                mc = slice(128 * m, 128 * m + 128)
                for i in range(4):
                    pg_, pb_ = pg[k % 2], pb[k % 2]
                    k += 1
                    for c in range(8):
                        P.mm(pg_[:], wg[:, 8 * i + c, mc], hT[:, c, :], start=(c == 0), stop=(c == 7))
                    for c2 in range(2):
                        src = ybr[:, 2 * i + c2, :] if i < 3 else ycT[:, c2, :]
                        P.mm(pb_[:], wb[:, 2 * i + c2, mc], src, start=(c2 == 0), stop=(c2 == 1))
                    P.act(sg[:], pg_[:], AF.Sigmoid)
                    if i == 0:
                        P.tt(macc[:], sg[:], pb_[:], ALU.mult)
                    elif i < 3:
                        P.tt(mtmp[:], sg[:], pb_[:], ALU.mult)
                        P.tt(macc[:], macc[:], mtmp[:], ALU.add, eng='pool')
                    else:
                        P.tt(mtmp[:], sg[:], pb_[:], ALU.mult)
                        P.tt(merged[:, m, :], macc[:], mtmp[:], ALU.add, eng='pool')
            for m in range(8):
                mc = slice(128 * m, 128 * m + 128)
                for c in range(8):
                    P.mm(po[:], wout[:, c, mc], merged[:, c, :], start=(c == 0), stop=(c == 7))
                P.tt(xt[:, m, :], xt[:, m, :], po[:], ALU.add)
            P.dma(x1v[:, :, t0:t0 + 512], xt[:])
        P.emit()
        print("build_C1: sbuf bytes/partition", A.bytes, "ops", P.nops)
    return nc


def build_C2(NTOK, NF, moe):
    nc = bass.Bass("TRN2", target_bir_lowering=False)
    NT = NTOK // 512
    DFF = NF * 128
    din = lambda n, s, d=F32: nc.dram_tensor(n, list(s), d, kind="ExternalInput").ap()
    x1_d = din("x1T", [1024, NTOK])
    xa_d = din("xaT", [1024, NTOK])
    gffn_d = din("gffn", [128, 8])
    w1_d = din("w1", [1024, DFF]); w3_d = din("w3", [1024, DFF]); w2_d = din("w2", [DFF, 1024])
    cst_d = din("cst", [128, 8])
    if moe:
        rt_d = din("router", [1024, 8])
        esel_d = din("esel", [128, 8])
        idn_d = din("ident", [128, 128])
    x2_d = nc.dram_tensor("x2T", [1024, NTOK], F32, kind="ExternalOutput").ap()
    with contextlib.ExitStack() as st:
        A = Alloc(nc, st); P = Prog(nc)
        gffn = A.sb("gffn_s", [128, 8]); P.dma(gffn[:], gffn_d)
        cst = A.sb("cst_s", [128, 8]); P.dma(cst[:], cst_d)
        ones_bf = A.sb("ones_bf", [128, 128], BF16); P.memset(ones_bf[:], 1.0)
        stg = A.sb("stg", [128, 2816])
        w1 = A.sb("w1_b", [128, 8, DFF], BF16); w3 = A.sb("w3_b", [128, 8, DFF], BF16); w2 = A.sb("w2_b", [128, NF, 1024], BF16)
        load_cast(P, w1[:], w1_d.rearrange("(c p) n -> p c n", p=128), stg)
        load_cast(P, w3[:], w3_d.rearrange("(c p) n -> p c n", p=128), stg)
        load_cast(P, w2[:], w2_d.rearrange("(c p) n -> p c n", p=128), stg)
        xt = A.sb("xt", [128, 8, 512]); xa = A.sb("xa", [128, 8, 512]) if moe else xt
        sq = A.sb("sq", [128, 8, 512], BF16); hT = A.sb("hT", [128, 8, 512], BF16)
        rstd = A.sb("rstd", [128, 512])
        actT = A.sb("actT", [128, NF, 512], BF16)
        sil = [A.sb("sil%d" % i, [128, 512]) for i in range(2)]
        pa = [A.ps("pa%d" % i, [128, 512]) for i in range(2)]
        pb = [A.ps("pb%d" % i, [128, 512]) for i in range(2)]
        pn = A.ps("pn", [128, 512]); po = A.ps("po", [128, 512]); pr = A.ps("pr", [128, 512])
        if moe:
            rtf = A.sb("rtf", [128, 8, 8]); P.dma(rtf[:], rt_d.rearrange("(c p) n -> p c n", p=128))
            rtb = A.sb("rtb", [128, 8, 8], BF16); P.copy(rtb[:], rtf[:])
            esel = A.sb("esel_s", [128, 8]); P.dma(esel[:], esel_d)
            idf = A.sb("idf", [128, 128]); P.dma(idf[:], idn_d)
            idb = A.sb("idb", [128, 128], BF16); P.copy(idb[:], idf[:])
            lg = A.sb("lg", [128, 8]); mx = A.sb("mx", [128, 8]); ex = A.sb("ex", [128, 8]); msk = A.sb("msk", [128, 8])
            s1 = A.sb("s1", [128, 4]); wrep = A.sb("wrep", [128, 128], BF16); wbc = A.sb("wbc", [128, 512])
        x1v = x1_d.rearrange("(c p) t -> p c t", p=128)
        xav = xa_d.rearrange("(c p) t -> p c t", p=128)
        x2v = x2_d.rearrange("(c p) t -> p c t", p=128)
        for g in range(NT):
            t0 = g * 512
            P.dma(xt[:], x1v[:, :, t0:t0 + 512])
            if moe:
                P.dma(xa[:], xav[:, :, t0:t0 + 512])
            rms_tile(P, xt[:], sq[:], ones_bf[:], pn[:], rstd[:], cst, hT, gffn)
            if moe:
                for blk in range(4):
                    bc = slice(128 * blk, 128 * blk + 128)
                    for c in range(8):
                        P.mm(pr[:, 0:8], hT[:, c, bc], rtb[:, c, :], start=(c == 0), stop=(c == 7))
                    P.copy(lg[:], pr[:, 0:8], eng='act')
                    P.add('dve', lambda e: e.max(mx[:], lg[:]), reads=[lg[:]], writes=[mx[:]])
                    P.tt(s1[:, 0:1], mx[:, 1:2], mx[:, 0:1], ALU.subtract)
                    P.act(s1[:, 0:1], s1[:, 0:1], AF.Exp)
                    P.ts(s1[:, 0:1], s1[:, 0:1], 1.0, None, ALU.add)
                    P.add('dve', lambda e: e.reciprocal(s1[:, 1:2], s1[:, 0:1]), reads=[s1[:, 0:1]], writes=[s1[:, 1:2]])
                    P.ts(s1[:, 2:3], mx[:, 0:1], -1.0, None, ALU.mult)
                    P.act(ex[:], lg[:], AF.Exp, bias=s1[:, 2:3], scale=1.0)
                    P.ts(msk[:], lg[:], mx[:, 1:2], None, ALU.is_ge)
                    P.stt(ex[:], ex[:], s1[:, 1:2], msk[:], ALU.mult, ALU.mult)
                    P.tt(ex[:], ex[:], esel[:], ALU.mult)
                    P.reduce(s1[:, 3:4], ex[:], ALU.add)
                    P.copy(wrep[:], s1[:, 3:4].to_broadcast([128, 128]), eng='dve')
                    P.mm(pr[:, 128:256], wrep[:], idb[:])
                    P.copy(wbc[:, bc], pr[:, 128:256], eng='act')
            for f in range(NF):
                fc = slice(128 * f, 128 * f + 128)
                pa_, pb_ = pa[f % 2], pb[f % 2]
                for c in range(8):
                    P.mm(pa_[:], w1[:, c, fc], hT[:, c, :], start=(c == 0), stop=(c == 7))
                for c in range(8):
                    P.mm(pb_[:], w3[:, c, fc], hT[:, c, :], start=(c == 0), stop=(c == 7))
                P.act(sil[f % 2][:], pa_[:], AF.Silu)
                if moe:
                    P.tt(sil[f % 2][:], sil[f % 2][:], pb_[:], ALU.mult)
                    P.tt(actT[:, f, :], sil[f % 2][:], wbc[:], ALU.mult, eng='pool')
                else:
                    P.tt(actT[:, f, :], sil[f % 2][:], pb_[:], ALU.mult)
            for m in range(8):
                mc = slice(128 * m, 128 * m + 128)
                for f in range(NF):
                    P.mm(po[:], w2[:, f, mc], actT[:, f, :], start=(f == 0), stop=(f == NF - 1))
                P.tt(xa[:, m, :], xa[:, m, :], po[:], ALU.add)
            P.dma(x2v[:, :, t0:t0 + 512], xa[:])
        P.emit()
        print("build_C2: sbuf bytes/partition", A.bytes, "ops", P.nops)
    return nc


_NC_CACHE = {}


def _get(key, fn):
    if key not in _NC_CACHE:
        _NC_CACHE[key] = fn()
    return _NC_CACHE[key]


def kernel(**inp):
    inp = {k: np.asarray(v) for k, v in inp.items()}
    x = inp["x"]
    B, S, D = x.shape
    NTOK = B * S // 8
    SH = S // NTOK
    cstv = np.zeros((128, 8), np.float32)
    for i, v in enumerate(CST):
        cstv[:, i] = v
    tables = [_const_tables_B(S, h) for h in range(4)]
    xT = np.ascontiguousarray(np.transpose(x, (0, 2, 1)))
    L = inp["w_in"].shape[0]
    ident = np.eye(128, dtype=np.float32)
    for l in range(L):
        ncB = _get(("B", S), lambda: build_B(S))
        maps = [prep_B(inp, l, c // 4, c % 4, xT[c // 4], tables) for c in range(8)]
        res = run_bass_kernel_spmd(ncB, maps, core_ids=list(range(8)))
        yT = np.stack([np.asarray(res.results[c]["yT"]) for c in range(8)])
        yT = yT.reshape(B, 4, 3, 64, S).transpose(0, 2, 1, 3, 4).reshape(B, 3, 256, S)
        ncC1 = _get(("C1", NTOK), lambda: build_C1(NTOK))
        xTp = np.concatenate([np.zeros((B, D, 32), np.float32), xT], axis=2)
        cv = np.zeros((128, 2, 34), np.float32)
        for cc in range(2):
            sl = slice(128 * cc, 128 * cc + 128)
            cv[:, cc, 0:31] = inp["conv_dw"][l][:, sl].T
            cv[:, cc, 31] = inp["conv_b"][l][sl]
            cv[:, cc, 32] = inp["conv_ln_w"][l][sl]
            cv[:, cc, 33] = inp["conv_ln_b"][l][sl]
        gmix = np.ascontiguousarray(inp["norm_mix"][l].reshape(8, 128).T)
        wconv = np.ascontiguousarray(inp["w_in"][l][:, 2816:3328])
        maps = []
        for c in range(8):
            b, s0 = c // SH, (c % SH) * NTOK
            maps.append(dict(xTh=np.ascontiguousarray(xTp[b, :, s0:s0 + 32 + NTOK]), gmix=gmix, wconv=wconv, cvec=cv,
                             yT=np.ascontiguousarray(yT[b, :, :, s0:s0 + NTOK]), wg=inp["w_gate"][l], wb=inp["w_branch"][l],
                             wout=inp["w_out"][l], cst=cstv))
        res = run_bass_kernel_spmd(ncC1, maps, core_ids=list(range(8)))
        x1 = [np.asarray(res.results[c]["x1T"]) for c in range(8)]
        gffn = np.ascontiguousarray(inp["norm_ffn"][l].reshape(8, 128).T)
        if l % 2 == 0:
            ncC2 = _get(("C2", NTOK, 22, False), lambda: build_C2(NTOK, 22, False))
            j = l // 2
            maps = [dict(x1T=x1[c], xaT=x1[c], gffn=gffn, w1=inp["ffn_w1"][j], w3=inp["ffn_w3"][j], w2=inp["ffn_w2"][j], cst=cstv)
                    for c in range(8)]
            res = run_bass_kernel_spmd(ncC2, maps, core_ids=list(range(8)))
            x2 = [np.asarray(res.results[c]["x2T"]) for c in range(8)]
        else:
            ncC2 = _get(("C2moe", NTOK), lambda: build_C2moe(NTOK))
            j = l // 2
            maps = [dict(x1T=x1[c], gffn=gffn, w1=inp["moe_w1"][j], w3=inp["moe_w3"][j], w2=inp["moe_w2"][j],
                         cst=cstv, router=inp["router"][j], ident=ident) for c in range(8)]
            res = run_bass_kernel_spmd(ncC2, maps, core_ids=list(range(8)))
            x2 = [np.asarray(res.results[c]["x2T"]) for c in range(8)]
        xT = np.stack([np.concatenate(x2[b * SH:(b + 1) * SH], axis=1) for b in range(B)])
    return np.ascontiguousarray(np.transpose(xT, (0, 2, 1))).astype(np.float32)


def build_C2moe(NTOK, NE=8, NF=11):
    nc = bass.Bass("TRN2", target_bir_lowering=False)
    NT = NTOK // 512
    DFF = NF * 128
    din = lambda n, s, d=F32: nc.dram_tensor(n, list(s), d, kind="ExternalInput").ap()
    x1_d = din("x1T", [1024, NTOK])
    gffn_d = din("gffn", [128, 8])
    w1_d = din("w1", [NE, 1024, DFF]); w3_d = din("w3", [NE, 1024, DFF]); w2_d = din("w2", [NE, DFF, 1024])
    cst_d = din("cst", [128, 8])
    rt_d = din("router", [1024, 8])
    idn_d = din("ident", [128, 128])
    x2_d = nc.dram_tensor("x2T", [1024, NTOK], F32, kind="ExternalOutput").ap()
    with contextlib.ExitStack() as st:
        A = Alloc(nc, st); P = Prog(nc)
        gffn = A.sb("gffn_s", [128, 8]); P.dma(gffn[:], gffn_d)
        cst = A.sb("cst_s", [128, 8]); P.dma(cst[:], cst_d)
        ones_bf = A.sb("ones_bf", [128, 128], BF16); P.memset(ones_bf[:], 1.0)
        stg = A.sb("stg", [128, 2816])
        w1 = A.sb("w1_b", [128, 8, DFF], BF16); w3 = A.sb("w3_b", [128, 8, DFF], BF16); w2 = A.sb("w2_b", [128, NF, 1024], BF16)
        xt = A.sb("xt", [128, 8, 512])
        sq = A.sb("sq", [128, 2, 512], BF16)
        hT_all = A.sb("hT_all", [128, NT * 8, 512], BF16)
        rstd = A.sb("rstd", [128, 512])
        actT = A.sb("actT", [128, NF, 512], BF16)
        sil = [A.sb("sil%d" % i, [128, 512]) for i in range(2)]
        pa = [A.ps("pa%d" % i, [128, 512]) for i in range(2)]
        pb = [A.ps("pb%d" % i, [128, 512]) for i in range(2)]
        pn = A.ps("pn", [128, 512]); po = A.ps("po", [128, 512]); pr = A.ps("pr", [128, 512])
        rtf = A.sb("rtf", [128, 8, 8]); P.dma(rtf[:], rt_d.rearrange("(c p) n -> p c n", p=128))
        rtb = A.sb("rtb", [128, 8, 8], BF16); P.copy(rtb[:], rtf[:])
        idf = A.sb("idf", [128, 128]); P.dma(idf[:], idn_d)
        idb = A.sb("idb", [128, 128], BF16); P.copy(idb[:], idf[:])
        lg = A.sb("lg", [128, 8]); mx = A.sb("mx", [128, 8]); ex = A.sb("ex", [128, 8]); msk = A.sb("msk", [128, 8])
        s1 = A.sb("s1", [128, 4]); wrep = A.sb("wrep", [128, 128], BF16); wbc = A.sb("wbc", [128, 512])
        wgt_all = A.sb("wgt_all", [128, NT * 4, 8])
        x1v = x1_d.rearrange("(c p) t -> p c t", p=128)
        x2v = x2_d.rearrange("(c p) t -> p c t", p=128)

        class _HT:
            def __init__(self, g):
                self.g = g

            def __getitem__(self, key):
                p, c, t = key
                return hT_all[p, self.g * 8 + c, t]

        for e in range(NE):
            load_cast(P, w1[:], w1_d[e].rearrange("(c p) n -> p c n", p=128), stg)
            load_cast(P, w3[:], w3_d[e].rearrange("(c p) n -> p c n", p=128), stg)
            load_cast(P, w2[:], w2_d[e].rearrange("(c p) n -> p c n", p=128), stg)
            for g in range(NT):
                t0 = g * 512
                P.dma(xt[:], (x1v if e == 0 else x2v)[:, :, t0:t0 + 512])
                hT = _HT(g)
                if e == 0:
                    rms_tile(P, xt[:], sq[:], ones_bf[:], pn[:], rstd[:], cst, hT, gffn)
                    for blk in range(4):
                        bc = slice(128 * blk, 128 * blk + 128)
                        for c in range(8):
                            P.mm(pr[:, 0:8], hT[:, c, bc], rtb[:, c, :], start=(c == 0), stop=(c == 7))
                        P.copy(lg[:], pr[:, 0:8], eng='act')
                        P.add('dve', lambda e_: e_.max(mx[:], lg[:]), reads=[lg[:]], writes=[mx[:]])
                        P.tt(s1[:, 0:1], mx[:, 1:2], mx[:, 0:1], ALU.subtract)
                        P.act(s1[:, 0:1], s1[:, 0:1], AF.Exp)
                        P.ts(s1[:, 0:1], s1[:, 0:1], 1.0, None, ALU.add)
                        P.add('dve', lambda e_: e_.reciprocal(s1[:, 1:2], s1[:, 0:1]), reads=[s1[:, 0:1]], writes=[s1[:, 1:2]])
                        P.ts(s1[:, 2:3], mx[:, 0:1], -1.0, None, ALU.mult)
                        P.act(ex[:], lg[:], AF.Exp, bias=s1[:, 2:3], scale=1.0)
                        P.ts(msk[:], lg[:], mx[:, 1:2], None, ALU.is_ge)
                        P.stt(wgt_all[:, g * 4 + blk, :], ex[:], s1[:, 1:2], msk[:], ALU.mult, ALU.mult)
                for blk in range(4):
                    bc = slice(128 * blk, 128 * blk + 128)
                    P.copy(wrep[:], wgt_all[:, g * 4 + blk, e:e + 1].to_broadcast([128, 128]), eng='dve')
                    P.mm(pr[:, 128:256], wrep[:], idb[:])
                    P.copy(wbc[:, bc], pr[:, 128:256], eng='act')
                for f in range(NF):
                    fc = slice(128 * f, 128 * f + 128)
                    pa_, pb_ = pa[f % 2], pb[f % 2]
                    for c in range(8):
                        P.mm(pa_[:], w1[:, c, fc], hT[:, c, :], start=(c == 0), stop=(c == 7))
                    for c in range(8):
                        P.mm(pb_[:], w3[:, c, fc], hT[:, c, :], start=(c == 0), stop=(c == 7))
                    P.act(sil[f % 2][:], pa_[:], AF.Silu)
                    P.tt(sil[f % 2][:], sil[f % 2][:], pb_[:], ALU.mult)
                    P.tt(actT[:, f, :], sil[f % 2][:], wbc[:], ALU.mult, eng='pool')
                for m in range(8):
                    mc = slice(128 * m, 128 * m + 128)
                    for f in range(NF):
                        P.mm(po[:], w2[:, f, mc], actT[:, f, :], start=(f == 0), stop=(f == NF - 1))
                    P.tt(xt[:, m, :], xt[:, m, :], po[:], ALU.add)
                P.dma(x2v[:, :, t0:t0 + 512], xt[:])
        P.emit()
        print("build_C2moe: sbuf bytes/partition", A.bytes, "ops", P.nops)
    return nc
```
